# Optimizing a Trainium2 kernel written in Bass

```python
import math
import jax
import jax.numpy as jnp
from jax import lax
import numpy as np

D_MODEL = 1024
BATCH = 8
SEQ = 4096
DEPTH = 2

CTX_LEN = 256
GRID_W = 64
EPS = 1e-6
MOD_CHUNKS = 6

DIFF_HEADS = 4
DIFF_QK = 32
DIFF_V = 2 * DIFF_QK
DIFF_W = DIFF_HEADS * DIFF_V
ROPE_BASE = 10000.0
Q_BLOCK = 128

SSM_HEADS = 8
SSM_P = 64
SSM_GROUPS = 2
SSM_HPG = SSM_HEADS // SSM_GROUPS
SSM_N = 64
SSM_W = SSM_HEADS * SSM_P
SSM_XBC = SSM_W + 2 * SSM_GROUPS * SSM_N
SSD_CHUNK = 128
CONV_K = 5

GDN_HEADS = 4
GDN_DK = 64
GDN_DV = 64
GDN_QKV = GDN_HEADS * (2 * GDN_DK + GDN_DV)
GDN_W = GDN_HEADS * GDN_DV
GDN_CHUNK = 64

D_MIX = DIFF_W + SSM_W + GDN_W
SPLIT_SIZES = (3 * DIFF_W, SSM_W, SSM_XBC, SSM_HEADS, GDN_QKV, GDN_W, 2 * GDN_HEADS, 2 * GDN_HEADS)
IN_DIM = sum(SPLIT_SIZES)

N_EGROUPS = 4
EXPERTS_PER_GROUP = 8
N_EXPERTS = N_EGROUPS * EXPERTS_PER_GROUP
TOP_K = 2
D_EXPERT = 512
MOE_BLOCK = 256

kernel_name = 'hybrid_diffusion_trunk'


def rmsnorm(x, w):
    x32 = x.astype(jnp.float32)
    y = x32 * lax.rsqrt(jnp.mean(x32 * x32, axis=-1, keepdims=True) + EPS)
    return y.astype(x.dtype) * w


def l2norm(x):
    x32 = x.astype(jnp.float32)
    return x32 * lax.rsqrt(jnp.sum(x32 * x32, axis=-1, keepdims=True) + EPS)


def modulate(x, shift, scale):
    return x * (1.0 + scale) + shift


def flip(t):
    return jnp.flip(t, axis=1)


def centred_dwconv(x, w):
    pad = CONV_K // 2
    return lax.conv_general_dilated(
        x, w[:, None, :].astype(x.dtype), window_strides=(1,), padding=[(pad, pad)],
        dimension_numbers=('NWC', 'WIO', 'NWC'), feature_group_count=x.shape[-1])


def axial_rope_tables(rows):
    row = jnp.repeat(jnp.arange(rows, dtype=jnp.float32), GRID_W)
    col = jnp.tile(jnp.arange(GRID_W, dtype=jnp.float32), rows)
    half = DIFF_QK // 2
    inv = ROPE_BASE ** (-jnp.arange(0, half, 2, dtype=jnp.float32) / half)
    ang_r = row[:, None] * inv
    ang_c = col[:, None] * inv
    ang = jnp.concatenate([ang_r, ang_r, ang_c, ang_c], axis=-1)
    return jnp.cos(ang), jnp.sin(ang)


def apply_axial_rope(x, cos, sin):
    a1, a2, b1, b2 = jnp.split(x, 4, axis=-1)
    rot = jnp.concatenate([-a2, a1, -b2, b1], axis=-1)
    cs = cos[None, :, None, None, :]
    sn = sin[None, :, None, None, :]
    return (x * cs + rot * sn).astype(x.dtype)


def diff_attention(hc, hl, cos, sin, lam_init, with_ctx, qn_w, kn_w, lq1, lk1, lq2, lk2, out_w):
    def qkv(h):
        b, n = h.shape[:2]
        q, k, v = jnp.split(h, 3, axis=-1)
        q = rmsnorm(q.reshape(b, n, DIFF_HEADS, 2, DIFF_QK), qn_w)
        k = rmsnorm(k.reshape(b, n, DIFF_HEADS, 2, DIFF_QK), kn_w)
        return q, k, v.reshape(b, n, DIFF_HEADS, DIFF_V)

    qc, kc, vc = qkv(hc)
    ql, kl, vl = qkv(hl)
    ql = apply_axial_rope(ql, cos, sin)
    kl = apply_axial_rope(kl, cos, sin)
    lam = (jnp.exp(jnp.sum(lq1 * lk1).astype(jnp.float32))
           - jnp.exp(jnp.sum(lq2 * lk2).astype(jnp.float32)) + lam_init)
    scale = DIFF_QK ** -0.5

    def attend(q, k, v):
        s = jnp.einsum('bqhmd,bkhmd->bhmqk', q, k, preferred_element_type=jnp.float32) * scale
        p = jax.nn.softmax(s, axis=-1)
        a = p[:, :, 0] - lam * p[:, :, 1]
        return jnp.einsum('bhqk,bkhv->bqhv', a.astype(v.dtype), v)

    def finish(o):
        b, n = o.shape[:2]
        return (rmsnorm(o, out_w) * (1.0 - lam_init)).reshape(b, n, DIFF_W)

    k_all = jnp.concatenate([kc, kl], axis=1)
    v_all = jnp.concatenate([vc, vl], axis=1)
    b, n = hl.shape[:2]
    nb = n // Q_BLOCK
    qb = jnp.moveaxis(ql.reshape(b, nb, Q_BLOCK, DIFF_HEADS, 2, DIFF_QK), 1, 0)
    ol = lax.map(lambda q: attend(q, k_all, v_all), qb)
    ol = finish(jnp.moveaxis(ol, 0, 1).reshape(b, n, DIFF_HEADS, DIFF_V))
    oc = finish(attend(qc, kc, vc)) if with_ctx else None
    return oc, ol


def ssd_chunked(xs, dt, a, bm, cm, h0):
    b, n = xs.shape[:2]
    nc = n // SSD_CHUNK
    xdt = (xs * dt[..., None]).reshape(b, nc, SSD_CHUNK, SSM_GROUPS, SSM_HPG, SSM_P)
    acum = jnp.cumsum((dt * a).reshape(b, nc, SSD_CHUNK, SSM_GROUPS, SSM_HPG), axis=2)
    bc = bm.reshape(b, nc, SSD_CHUNK, SSM_GROUPS, SSM_N)
    cc = cm.reshape(b, nc, SSD_CHUNK, SSM_GROUPS, SSM_N)
    incl = jnp.tril(jnp.ones((SSD_CHUNK, SSD_CHUNK), bool))[:, :, None, None]
    seg = acum[:, :, :, None] - acum[:, :, None, :]
    lmat = jnp.exp(jnp.where(incl, seg, -jnp.inf))
    cb = jnp.einsum('bclgn,bcsgn->bclsg', cc, bc)
    y_diag = jnp.einsum('bclsg,bclsgr,bcsgrp->bclgrp', cb, lmat, xdt)
    states = jnp.einsum('bclgn,bclgr,bclgrp->bcgrpn', bc, jnp.exp(acum[:, :, -1:] - acum), xdt)
    chunk_decay = jnp.exp(acum[:, :, -1])

    def step(h, inp):
        st, dec = inp
        return h * dec[..., None, None] + st, h

    h_final, h_in = lax.scan(step, h0, (jnp.moveaxis(states, 1, 0), jnp.moveaxis(chunk_decay, 1, 0)))
    y_off = jnp.einsum('bclgn,cbgrpn,bclgr->bclgrp', cc, h_in, jnp.exp(acum))
    return (y_diag + y_off).reshape(b, n, SSM_GROUPS, SSM_HPG, SSM_P), h_final


def ssm_mixer(zc, xbc_c, dt_c, zl, xbc_l, dt_l, with_ctx, conv_w, conv_b, dt_bias, a_log, d_skip, norm_w):
    def prep(xbc, dt):
        xbc = jax.nn.silu(centred_dwconv(xbc, conv_w) + conv_b)
        b, n = xbc.shape[:2]
        xs, bm, cm = jnp.split(xbc, [SSM_W, SSM_W + SSM_GROUPS * SSM_N], axis=-1)
        xs = xs.reshape(b, n, SSM_GROUPS, SSM_HPG, SSM_P).astype(jnp.float32)
        bm = bm.reshape(b, n, SSM_GROUPS, SSM_N).astype(jnp.float32)
        cm = cm.reshape(b, n, SSM_GROUPS, SSM_N).astype(jnp.float32)
        dt = dt.astype(jnp.float32)
        dts = [jax.nn.softplus(dt + dt_bias[d].astype(jnp.float32)).reshape(b, n, SSM_GROUPS, SSM_HPG)
               for d in range(2)]
        return xs, bm, cm, dts

    a = -jnp.exp(a_log.astype(jnp.float32)).reshape(2, SSM_GROUPS, SSM_HPG)
    xc, bc, cc, dtc = prep(xbc_c, dt_c)
    xl, bl, cl, dtl = prep(xbc_l, dt_l)
    h0 = jnp.zeros((xl.shape[0], SSM_GROUPS, SSM_HPG, SSM_P, SSM_N), jnp.float32)
    yc_f, hc_f = ssd_chunked(xc, dtc[0], a[0], bc, cc, h0)
    yc_b, hc_b = ssd_chunked(flip(xc), flip(dtc[1]), a[1], flip(bc), flip(cc), h0)
    yl_f, _ = ssd_chunked(xl, dtl[0], a[0], bl, cl, hc_f)
    yl_b, _ = ssd_chunked(flip(xl), flip(dtl[1]), a[1], flip(bl), flip(cl), hc_b)
    d_h = d_skip.astype(jnp.float32).reshape(SSM_GROUPS, SSM_HPG)[..., None]

    def finish(y_f, y_b, xs, z):
        b, n = z.shape[:2]
        y = (y_f + flip(y_b) + d_h * xs).reshape(b, n, SSM_W).astype(z.dtype) * jax.nn.silu(z)
        y = rmsnorm(y.reshape(b, n, SSM_GROUPS, SSM_W // SSM_GROUPS), norm_w.reshape(SSM_GROUPS, -1))
        return y.reshape(b, n, SSM_W)

    oc = finish(yc_f, yc_b, xc, zc) if with_ctx else None
    return oc, finish(yl_f, yl_b, xl, zl)


def gated_delta_chunked(q, k, v, g, beta, s0):
    b, n = q.shape[:2]
    nc = n // GDN_CHUNK

    def chunks(t):
        return jnp.moveaxis(t.reshape((b, nc, GDN_CHUNK) + t.shape[2:]), 2, 3)

    q, k, v, g, beta = (chunks(t) for t in (q, k, v, g, beta))
    gc = jnp.cumsum(g, axis=-1)
    kb = k * beta[..., None]
    vb = v * beta[..., None]
    incl = jnp.tril(jnp.ones((GDN_CHUNK, GDN_CHUNK), bool))
    strict = jnp.tril(jnp.ones((GDN_CHUNK, GDN_CHUNK), bool), -1)
    dec = jnp.exp(jnp.where(incl, gc[..., :, None] - gc[..., None, :], -jnp.inf))
    a_strict = jnp.where(strict, jnp.einsum('bchld,bchsd->bchls', kb, k) * dec, 0.0)
    eye = jnp.eye(GDN_CHUNK, dtype=a_strict.dtype)
    t_inv = lax.linalg.triangular_solve(a_strict + eye, jnp.broadcast_to(eye, a_strict.shape),
                                        left_side=True, lower=True)
    u = jnp.einsum('bchls,bchsv->bchlv', t_inv, vb)
    w = jnp.einsum('bchls,bchsd->bchld', t_inv, kb * jnp.exp(gc)[..., None])
    qk = jnp.where(incl, jnp.einsum('bchld,bchsd->bchls', q, k) * dec, 0.0)
    q_dec = q * jnp.exp(gc)[..., None]
    k_to_end = k * jnp.exp(gc[..., -1:] - gc)[..., None]
    g_end = jnp.exp(gc[..., -1])

    def step(state, inp):
        qd, kd, u_c, w_c, qk_c, ge = inp
        v_new = u_c - jnp.einsum('bhld,bhdv->bhlv', w_c, state)
        o = jnp.einsum('bhld,bhdv->bhlv', qd, state) + jnp.einsum('bhls,bhsv->bhlv', qk_c, v_new)
        state = state * ge[..., None, None] + jnp.einsum('bhld,bhlv->bhdv', kd, v_new)
        return state, o

    xs = tuple(jnp.moveaxis(t, 1, 0) for t in (q_dec, k_to_end, u, w, qk, g_end))
    s_final, o = lax.scan(step, s0, xs)
    o = jnp.moveaxis(jnp.moveaxis(o, 0, 1), 3, 2).reshape(b, n, GDN_HEADS, GDN_DV)
    return o, s_final


def gdn_mixer(qkv_c, gate_c, beta_c, a_c, qkv_l, gate_l, beta_l, a_l, with_ctx, conv_w, dt_bias, a_log, norm_w):
    def prep(qkv, beta, a):
        qkv = jax.nn.silu(centred_dwconv(qkv, conv_w))
        b, n = qkv.shape[:2]
        q, k, v = jnp.split(qkv, [GDN_HEADS * GDN_DK, 2 * GDN_HEADS * GDN_DK], axis=-1)
        q = l2norm(q.reshape(b, n, GDN_HEADS, GDN_DK)) * (GDN_DK ** -0.5)
        k = l2norm(k.reshape(b, n, GDN_HEADS, GDN_DK))
        v = v.reshape(b, n, GDN_HEADS, GDN_DV).astype(jnp.float32)
        beta = jax.nn.sigmoid(beta.astype(jnp.float32)).reshape(b, n, 2, GDN_HEADS)
        a = a.astype(jnp.float32).reshape(b, n, 2, GDN_HEADS)
        g = -jnp.exp(a_log.astype(jnp.float32)) * jax.nn.softplus(a + dt_bias.astype(jnp.float32))
        return q, k, v, g, beta

    qc, kc, vc, gcx, bcx = prep(qkv_c, beta_c, a_c)
    ql, kl, vl, glx, blx = prep(qkv_l, beta_l, a_l)
    s0 = jnp.zeros((ql.shape[0], GDN_HEADS, GDN_DK, GDN_DV), jnp.float32)
    oc_f, sc_f = gated_delta_chunked(qc, kc, vc, gcx[:, :, 0], bcx[:, :, 0], s0)
    oc_b, sc_b = gated_delta_chunked(flip(qc), flip(kc), flip(vc), flip(gcx[:, :, 1]), flip(bcx[:, :, 1]), s0)
    ol_f, _ = gated_delta_chunked(ql, kl, vl, glx[:, :, 0], blx[:, :, 0], sc_f)
    ol_b, _ = gated_delta_chunked(flip(ql), flip(kl), flip(vl), flip(glx[:, :, 1]), flip(blx[:, :, 1]), sc_b)

    def finish(o_f, o_b, gate):
        b, n = gate.shape[:2]
        o = rmsnorm(o_f + flip(o_b), norm_w) * jax.nn.silu(gate.reshape(b, n, GDN_HEADS, GDN_DV))
        return o.reshape(b, n, GDN_W)

    oc = finish(oc_f, oc_b, gate_c) if with_ctx else None
    return oc, finish(ol_f, ol_b, gate_l)


def token_mixers(hc, hl, cos, sin, lam_init, with_ctx,
                 diff_qn_w, diff_kn_w, diff_lq1, diff_lk1, diff_lq2, diff_lk2, diff_norm_w,
                 ssm_conv_w, ssm_conv_b, ssm_dt_bias, ssm_a_log, ssm_d, ssm_norm_w,
                 gdn_conv_w, gdn_dt_bias, gdn_a_log, gdn_norm_w):
    idx = [int(i) for i in np.cumsum(SPLIT_SIZES)[:-1]]
    pc = jnp.split(hc, idx, axis=-1)
    pl = jnp.split(hl, idx, axis=-1)
    ac, al = diff_attention(pc[0], pl[0], cos, sin, lam_init, with_ctx, diff_qn_w, diff_kn_w,
                            diff_lq1, diff_lk1, diff_lq2, diff_lk2, diff_norm_w)
    sc, sl = ssm_mixer(pc[1], pc[2], pc[3], pl[1], pl[2], pl[3], with_ctx, ssm_conv_w, ssm_conv_b,
                       ssm_dt_bias, ssm_a_log, ssm_d, ssm_norm_w)
    gc, gl = gdn_mixer(pc[4], pc[5], pc[6], pc[7], pl[4], pl[5], pl[6], pl[7], with_ctx, gdn_conv_w,
                       gdn_dt_bias, gdn_a_log, gdn_norm_w)
    dt = hl.dtype
    ml = jnp.concatenate([al.astype(dt), sl.astype(dt), gl.astype(dt)], axis=-1)
    mc = jnp.concatenate([ac.astype(dt), sc.astype(dt), gc.astype(dt)], axis=-1) if with_ctx else None
    return mc, ml


def routed_experts(t, experts, gates, w_gate, w_up, w_down):
    n, d = t.shape
    n_assign = n * TOP_K
    flat_e = experts.reshape(n_assign)
    order = jnp.argsort(flat_e)
    sorted_e = flat_e[order]
    counts = jax.ops.segment_sum(jnp.ones((n_assign,), jnp.int32), flat_e, num_segments=N_EXPERTS)
    starts = jnp.cumsum(counts) - counts
    padded = (counts + MOE_BLOCK - 1) // MOE_BLOCK * MOE_BLOCK
    pad_ends = jnp.cumsum(padded)
    pad_starts = pad_ends - padded
    dest = pad_starts[sorted_e] + jnp.arange(n_assign, dtype=jnp.int32) - starts[sorted_e]
    n_blocks = -(-n_assign // MOE_BLOCK) + N_EXPERTS
    buf = jnp.zeros((n_blocks * MOE_BLOCK, d), t.dtype).at[dest].set(t[order // TOP_K])
    block_start = jnp.arange(n_blocks, dtype=jnp.int32) * MOE_BLOCK
    block_e = jnp.minimum(jnp.searchsorted(pad_ends, block_start, side='right'), N_EXPERTS - 1)

    def expert_block(args):
        xb, e = args
        hb = jax.nn.silu(xb @ w_gate[e]) * (xb @ w_up[e])
        return hb @ w_down[e]

    out = lax.map(expert_block, (buf.reshape(n_blocks, MOE_BLOCK, d), block_e)).reshape(-1, d)
    y_assign = jnp.zeros((n_assign, d), out.dtype).at[order].set(out[dest])
    return jnp.sum(y_assign.reshape(n, TOP_K, d) * gates[..., None].astype(out.dtype), axis=1)


def hier_moe(t, router_g_w, router_g_b, router_e_w, router_e_b, w_gate, w_up, w_down):
    n = t.shape[0]
    grp_prob = jax.nn.softmax(jnp.dot(t, router_g_w, preferred_element_type=jnp.float32)
                              + router_g_b.astype(jnp.float32), axis=-1)
    p_grp, grp = lax.top_k(grp_prob, 1)
    e_logits = (jnp.dot(t, router_e_w, preferred_element_type=jnp.float32)
                + router_e_b.astype(jnp.float32)).reshape(n, N_EGROUPS, EXPERTS_PER_GROUP)
    sel = e_logits[jnp.arange(n), grp[:, 0]]
    p_top, idx = lax.top_k(jax.nn.softmax(sel, axis=-1), TOP_K)
    gates = p_grp * p_top / jnp.sum(p_top, axis=-1, keepdims=True)
    experts = grp * EXPERTS_PER_GROUP + idx
    return routed_experts(t, experts, gates, w_gate, w_up, w_down)


def setup_inputs(seed: int = 0) -> dict:
    key = jax.random.key(seed)
    ks = iter(jax.random.split(key, 48))

    def nrm(shape, scale):
        return jax.random.normal(next(ks), shape, jnp.float32) * scale

    def gain(shape):
        return 1.0 + nrm(shape, 0.05)

    def unif(shape, lo, hi):
        return jax.random.uniform(next(ks), shape, jnp.float32, lo, hi)

    L = DEPTH
    ssm_dt = jnp.exp(unif((L, 2, SSM_HEADS), math.log(1e-3), math.log(1e-1)))
    gdn_dt = jnp.exp(unif((L, 2, GDN_HEADS), math.log(1e-3), math.log(1e-1)))
    return {
        'x': nrm((BATCH, SEQ, D_MODEL), 1.0),
        'c': nrm((BATCH, D_MODEL), 1.0),
        'ctx': nrm((BATCH, CTX_LEN, D_MODEL), 1.0),
        'c_ctx': nrm((D_MODEL,), 1.0),
        'w_mod': nrm((L, D_MODEL, MOD_CHUNKS * D_MODEL), 0.5 * D_MODEL ** -0.5),
        'b_mod': nrm((L, MOD_CHUNKS * D_MODEL), 0.02),
        'norm1_w': gain((L, D_MODEL)),
        'norm2_w': gain((L, D_MODEL)),
        'w_in': nrm((L, D_MODEL, IN_DIM), D_MODEL ** -0.5),
        'w_out': nrm((L, D_MIX, D_MODEL), D_MIX ** -0.5),
        'diff_qn_w': gain((L, DIFF_QK)),
        'diff_kn_w': gain((L, DIFF_QK)),
        'diff_lq1': nrm((L, DIFF_QK), 0.1),
        'diff_lk1': nrm((L, DIFF_QK), 0.1),
        'diff_lq2': nrm((L, DIFF_QK), 0.1),
        'diff_lk2': nrm((L, DIFF_QK), 0.1),
        'diff_norm_w': gain((L, DIFF_V)),
        'ssm_conv_w': nrm((L, CONV_K, SSM_XBC), CONV_K ** -0.5),
        'ssm_conv_b': nrm((L, SSM_XBC), 0.02),
        'ssm_dt_bias': ssm_dt + jnp.log(-jnp.expm1(-ssm_dt)),
        'ssm_a_log': jnp.log(unif((L, 2, SSM_HEADS), 1.0, 16.0)),
        'ssm_d': gain((L, SSM_HEADS)),
        'ssm_norm_w': gain((L, SSM_W)),
        'gdn_conv_w': nrm((L, CONV_K, GDN_QKV), CONV_K ** -0.5),
        'gdn_dt_bias': gdn_dt + jnp.log(-jnp.expm1(-gdn_dt)),
        'gdn_a_log': jnp.log(unif((L, 2, GDN_HEADS), 1.0, 16.0)),
        'gdn_norm_w': gain((L, GDN_DV)),
        'router_g_w': nrm((L, D_MODEL, N_EGROUPS), D_MODEL ** -0.5),
        'router_g_b': nrm((L, N_EGROUPS), 0.01),
        'router_e_w': nrm((L, D_MODEL, N_EXPERTS), D_MODEL ** -0.5),
        'router_e_b': nrm((L, N_EXPERTS), 0.01),
        'exp_w_gate': nrm((L, N_EXPERTS, D_MODEL, D_EXPERT), D_MODEL ** -0.5),
        'exp_w_up': nrm((L, N_EXPERTS, D_MODEL, D_EXPERT), D_MODEL ** -0.5),
        'exp_w_down': nrm((L, N_EXPERTS, D_EXPERT, D_MODEL), D_EXPERT ** -0.5),
    }


def reference(x, c, ctx, c_ctx, w_mod, b_mod, norm1_w, norm2_w, w_in, w_out,
              diff_qn_w, diff_kn_w, diff_lq1, diff_lk1, diff_lq2, diff_lk2, diff_norm_w,
              ssm_conv_w, ssm_conv_b, ssm_dt_bias, ssm_a_log, ssm_d, ssm_norm_w,
              gdn_conv_w, gdn_dt_bias, gdn_a_log, gdn_norm_w,
              router_g_w, router_g_b, router_e_w, router_e_b, exp_w_gate, exp_w_up, exp_w_down):
    rows = x.shape[1] // GRID_W
    cos, sin = axial_rope_tables(rows)
    xl, xc = x, ctx
    for l in range(DEPTH):
        last = l == DEPTH - 1
        lam_init = 0.8 - 0.6 * math.exp(-0.3 * l)
        mod_l = [m[:, None, :] for m in jnp.split(jax.nn.silu(c) @ w_mod[l] + b_mod[l], MOD_CHUNKS, axis=-1)]
        mod_c = jnp.split(jax.nn.silu(c_ctx) @ w_mod[l] + b_mod[l], MOD_CHUNKS, axis=-1)
        hl = modulate(rmsnorm(xl, norm1_w[l]), mod_l[0], mod_l[1]) @ w_in[l]
        hc = modulate(rmsnorm(xc, norm1_w[l]), mod_c[0], mod_c[1]) @ w_in[l]
        mc, ml = token_mixers(hc, hl, cos, sin, lam_init, not last,
                              diff_qn_w[l], diff_kn_w[l], diff_lq1[l], diff_lk1[l], diff_lq2[l], diff_lk2[l],
                              diff_norm_w[l], ssm_conv_w[l], ssm_conv_b[l], ssm_dt_bias[l], ssm_a_log[l],
                              ssm_d[l], ssm_norm_w[l], gdn_conv_w[l], gdn_dt_bias[l], gdn_a_log[l], gdn_norm_w[l])
        xl = xl + mod_l[2] * (ml @ w_out[l])
        fl = modulate(rmsnorm(xl, norm2_w[l]), mod_l[3], mod_l[4]).reshape(-1, D_MODEL)
        moe_args = (router_g_w[l], router_g_b[l], router_e_w[l], router_e_b[l],
                    exp_w_gate[l], exp_w_up[l], exp_w_down[l])
        if last:
            yl = hier_moe(fl, *moe_args)
        else:
            xc = xc + mod_c[2] * (mc @ w_out[l])
            fc = modulate(rmsnorm(xc, norm2_w[l]), mod_c[3], mod_c[4]).reshape(-1, D_MODEL)
            y = hier_moe(jnp.concatenate([fl, fc], axis=0), *moe_args)
            yl = y[:fl.shape[0]]
            xc = xc + mod_c[5] * y[fl.shape[0]:].reshape(xc.shape)
        xl = xl + mod_l[5] * yl.reshape(xl.shape)
    return xl
```

```python
import contextlib
import math
import numpy as np
import concourse.bass as bass
import concourse.mybir as mybir
from concourse.bass_utils import run_bass_kernel_spmd

F32 = mybir.dt.float32
BF16 = mybir.dt.bfloat16
AF = mybir.ActivationFunctionType
ALU = mybir.AluOpType
AX = mybir.AxisListType

ENGS = ("pe", "act", "dve", "pool", "sp")
SAME_SYNC = {"pe": False, "act": True, "dve": True, "pool": True, "sp": True}
N_DMA_SEMS = 18


class Op:
    __slots__ = ("eng", "fn", "deps", "dma", "needs_inc", "idx", "sem", "val", "pos")

    def __init__(self, eng, fn, deps, dma):
        self.eng, self.fn, self.deps, self.dma = eng, fn, deps, dma
        self.needs_inc = False
        self.sem = None
        self.val = None


class Prog:
    def __init__(self, nc):
        self.nc = nc
        self.ops = {e: [] for e in ENGS}
        self.last_w = {}
        self.readers = {}
        self.pending_bar = {e: None for e in ENGS}
        self.since_bar = []

    def op(self, eng, fn, reads=(), writes=(), dma=False):
        deps = set()
        for k in reads:
            w = self.last_w.get(k)
            if w is not None:
                deps.add(w)
        for k in writes:
            w = self.last_w.get(k)
            if w is not None:
                deps.add(w)
            for r in self.readers.get(k, ()):
                deps.add(r)
        if self.pending_bar[eng] is not None:
            deps.update(self.pending_bar[eng])
            self.pending_bar[eng] = None
        rec = Op(eng, fn, deps, dma)
        for k in reads:
            self.readers.setdefault(k, []).append(rec)
        for k in writes:
            self.last_w[k] = rec
            self.readers[k] = []
        self.ops[eng].append(rec)
        if dma:
            self.since_bar.append(rec)
        return rec

    def barrier(self):
        deps = list(self.since_bar)
        for e in ENGS:
            if self.ops[e]:
                deps.append(self.ops[e][-1])
        for e in ENGS:
            cur = self.pending_bar[e]
            self.pending_bar[e] = (cur or []) + deps
        self.since_bar = []

    def pe(self, fn, r=(), w=()):
        return self.op("pe", fn, r, w)

    def act(self, fn, r=(), w=()):
        return self.op("act", fn, r, w)

    def dve(self, fn, r=(), w=()):
        return self.op("dve", fn, r, w)

    def pool(self, fn, r=(), w=()):
        return self.op("pool", fn, r, w)

    def dma(self, q, fn, r=(), w=()):
        return self.op(q, fn, r, w, dma=True)

    def emit(self, final_wait_ops=()):
        nc = self.nc
        for e in ENGS:
            for rec in self.ops[e]:
                for d in rec.deps:
                    if d.dma:
                        continue
                    if d.eng == rec.eng and not SAME_SYNC[rec.eng]:
                        continue
                    d.needs_inc = True
        with contextlib.ExitStack() as es:
            esem = {e: es.enter_context(nc.semaphore(f"s_{e}")) for e in ENGS}
            dsem = {e: [es.enter_context(nc.semaphore(f"d_{e}_{i}")) for i in range(N_DMA_SEMS)]
                    for e in ("sp", "pool", "act")}
            finals = {}
            for e in ENGS:
                cnt = 0
                dcnt = [0] * N_DMA_SEMS
                di = 0
                for rec in self.ops[e]:
                    if rec.dma:
                        k = di % N_DMA_SEMS
                        di += 1
                        dcnt[k] += 16
                        rec.sem, rec.val = dsem[e][k], dcnt[k]
                    elif rec.needs_inc:
                        cnt += 1
                        rec.sem, rec.val = esem[e], cnt
                if e in dsem:
                    finals[e] = list(dcnt)
            block = es.enter_context(nc.Block())

            def run(e, eh):
                waited = {}
                for rec in self.ops[e]:
                    need = {}
                    for d in rec.deps:
                        if (not d.dma) and d.eng == e and not SAME_SYNC[e]:
                            continue
                        s, v = d.sem, d.val
                        key = id(s)
                        if waited.get(key, 0) >= v:
                            continue
                        if key not in need or need[key][1] < v:
                            need[key] = (s, v)
                    if rec.dma and rec.val > 16:
                        s, v = rec.sem, rec.val - 16
                        key = id(s)
                        if waited.get(key, 0) < v and (key not in need or need[key][1] < v):
                            need[key] = (s, v)
                    for key, (s, v) in need.items():
                        eh.wait_ge(s, v)
                        waited[key] = v
                    ins = rec.fn(eh)
                    if rec.dma:
                        ins.then_inc(rec.sem, 16)
                    elif rec.needs_inc:
                        ins.then_inc(rec.sem, 1)
                if e == "sp":
                    for q, cnts in finals.items():
                        for k, v in enumerate(cnts):
                            if v > 0:
                                eh.wait_ge(dsem[q][k], v)

            block.tensor(lambda eh: run("pe", eh))
            block.scalar(lambda eh: run("act", eh))
            block.vector(lambda eh: run("dve", eh))
            block.gpsimd(lambda eh: run("pool", eh))
            block.sync(lambda eh: run("sp", eh))


D = 1024
T = 4352
NT = 34
NCTX_T = 2
L_DEPTH = 2
EPS = 1e-6
IN_DIM = 3096
GROUPS = [(0, 2)] + [(2 + 4 * i, 4) for i in range(8)]
NEG = -30000.0
C_QKV, C_Z, C_XBC, C_DT, C_GQKV, C_GATE, C_BETA, C_A = 0, 768, 1280, 2048, 2056, 2824, 3080, 3088


def build(NL=2, dbg=False, stop_after=None, zero_ml=False, small_exp=False):
    nc = bass.Bass("TRN2", target_bir_lowering=False)
    P = Prog(nc)

    def din(name, shape, dt=F32):
        return nc.dram_tensor(name, list(shape), dt, kind="ExternalInput").ap()

    def dscr(name, shape, dt, out=False):
        return nc.dram_tensor(name, list(shape), dt, kind=("ExternalOutput" if out else "Internal")).ap()

    xin = din("xin", [T, D])
    ccT = din("ccT", [128, 8, 2])
    rope = din("rope", [2, 128, 32, 32])
    w_mod = din("w_mod", [L_DEPTH, D, 6 * D])
    b_modT = din("b_modT", [L_DEPTH, 128, 48])
    n1T = din("n1T", [L_DEPTH, 128, 8])
    n2T = din("n2T", [L_DEPTH, 128, 8])
    w_in = din("w_in", [L_DEPTH, D, IN_DIM])
    w_out = din("w_out", [L_DEPTH, D, D])
    diff_qn_w = din("diff_qn_w", [L_DEPTH, 32])
    diff_kn_w = din("diff_kn_w", [L_DEPTH, 32])
    diff_lq1 = din("diff_lq1", [L_DEPTH, 32])
    diff_lk1 = din("diff_lk1", [L_DEPTH, 32])
    diff_lq2 = din("diff_lq2", [L_DEPTH, 32])
    diff_lk2 = din("diff_lk2", [L_DEPTH, 32])
    diff_norm_w = din("diff_norm_w", [L_DEPTH, 64])
    ssm_conv_wT = din("ssm_conv_wT", [L_DEPTH, 128, 6, 5])
    ssm_conv_bT = din("ssm_conv_bT", [L_DEPTH, 128, 6])
    ssm_dt_bias = din("ssm_dt_bias", [L_DEPTH, 2, 8])
    ssm_a_log = din("ssm_a_log", [L_DEPTH, 2, 8])
    ssm_d = din("ssm_d", [L_DEPTH, 8])
    ssm_norm_w = din("ssm_norm_w", [L_DEPTH, 512])
    gdn_conv_wT = din("gdn_conv_wT", [L_DEPTH, 128, 6, 5])
    gdn_dt_bias = din("gdn_dt_bias", [L_DEPTH, 2, 4])
    gdn_a_log = din("gdn_a_log", [L_DEPTH, 2, 4])
    gdn_norm_w = din("gdn_norm_w", [L_DEPTH, 64])
    router_w = din("router_w", [L_DEPTH, D, 36])
    router_b = din("router_b", [L_DEPTH, 36])
    _ne = 1 if small_exp else 32
    _nl = 1 if small_exp else L_DEPTH
    exp_w_gate = din("exp_w_gate", [_nl, _ne, D, 512])
    exp_w_up = din("exp_w_up", [_nl, _ne, D, 512])
    exp_w_down = din("exp_w_down", [_nl, _ne, 512, D])

    y_out = nc.dram_tensor("y", [4096, D], F32, kind="ExternalOutput").ap()
    xs1 = dscr("xs1", [T, D], F32, out=dbg)
    xs2 = dscr("xs2", [T, D], F32)
    hT = dscr("hT", [12, 128, T], BF16, out=dbg)
    zs = dscr("zs", [T, 512], BF16)
    gs = dscr("gs", [T, 256], BF16)
    ml = dscr("ml", [T, D], BF16, out=dbg)
    modrows = dscr("modrows", [96, 128], F32)

    outs = []
    with contextlib.ExitStack() as es0:
        _cnt = [0]

        def sb(es, name, shape, dt):
            _cnt[0] += 1
            return es.enter_context(nc.sbuf_tensor(f"{name}_{_cnt[0]}", list(shape), dt))

        banks = [es0.enter_context(nc.psum_tensor(f"bank{i}", [128, 512], F32)) for i in range(6)]
        tbanks = {6 + i: es0.enter_context(nc.psum_tensor(f"tbank{i}", [128, 1024], BF16)) for i in range(2)}

        def BK(i):
            return ("bank", i)

        identf = sb(es0, "identf", [128, 128], F32)
        identb = sb(es0, "identb", [128, 128], BF16)
        onesf = sb(es0, "onesf", [128, 128], F32)
        Uf = sb(es0, "Uf", [128, 128], F32)
        Ub = sb(es0, "Ub", [128, 128], F32)
        nUf = sb(es0, "nUf", [128, 128], F32)
        nUb = sb(es0, "nUb", [128, 128], F32)
        Sf = sb(es0, "Sf", [128, 128], F32)
        Sb_ = sb(es0, "Sb", [128, 128], F32)
        NMf = sb(es0, "NMf", [128, 4, 128], F32)
        NMb = sb(es0, "NMb", [128, 4, 128], F32)
        cc_sb = sb(es0, "cc_sb", [128, 8, 2], F32)
        csil = sb(es0, "csil", [128, 8, 2], F32)
        epsc = sb(es0, "epsc", [128, 1], F32)

        def mk_mask(t_ap, val_in, fill, pattern, cm, cmp, key):
            P.pool(lambda e: e.memset(t_ap, val_in), w=[key])
            P.pool(lambda e: e.affine_select(out=t_ap, in_=t_ap, compare_op=cmp, fill=fill, base=0,
                                             pattern=pattern, channel_multiplier=cm), r=[key], w=[key])

        mk_mask(identf[:], 0.0, 1.0, [[-1, 128]], 1, ALU.not_equal, "identf")
        P.pool(lambda e: e.tensor_copy(out=identb[:], in_=identf[:]), r=["identf"], w=["identb"])
        P.pool(lambda e: e.memset(onesf[:], 1.0), w=["onesf"])
        P.pool(lambda e: e.memset(epsc[:], EPS), w=["epsc"])
        mk_mask(Uf[:], 1.0, 0.0, [[1, 128]], -1, ALU.is_ge, "Uf")
        mk_mask(Ub[:], 1.0, 0.0, [[-1, 128]], 1, ALU.is_ge, "Ub")
        mk_mask(nUf[:], -1.0, 0.0, [[1, 128]], -1, ALU.is_ge, "nUf")
        mk_mask(nUb[:], -1.0, 0.0, [[-1, 128]], 1, ALU.is_ge, "nUb")
        mk_mask(Sf[:], 1.0, 0.0, [[-1, 128]], 1, ALU.is_gt, "Sf")
        mk_mask(Sb_[:], 1.0, 0.0, [[1, 128]], -1, ALU.is_gt, "Sb")
        for j in range(4):
            mk_mask(NMf[:, j, :], NEG, 0.0, [[-1, 128]], 1, ALU.is_gt, ("NMf", j))
            mk_mask(NMb[:, j, :], NEG, 0.0, [[1, 128]], -1, ALU.is_gt, ("NMb", j))
        NMf_keys = [("NMf", j) for j in range(4)]
        NMb_keys = [("NMb", j) for j in range(4)]

        blkf = sb(es0, "blkf", [128, 128], F32)
        offd = sb(es0, "offd", [128, 128], F32)
        UBf = sb(es0, "UBf", [128, 128], F32)
        UBb = sb(es0, "UBb", [128, 128], F32)
        nUBf = sb(es0, "nUBf", [128, 128], F32)
        nUBb = sb(es0, "nUBb", [128, 128], F32)
        SBf = sb(es0, "SBf", [128, 128], F32)
        SBb = sb(es0, "SBb", [128, 128], F32)
        NMBf = sb(es0, "NMBf", [128, 4, 128], F32)
        NMBb = sb(es0, "NMBb", [128, 4, 128], F32)
        chunkind = sb(es0, "chunkind", [128, 2], F32)
        P.pool(lambda e: e.memset(blkf[:], 0.0), w=["blkf"])
        P.pool(lambda e: e.memset(blkf[0:64, 0:64], 1.0), r=["blkf"], w=["blkf"])
        P.pool(lambda e: e.memset(blkf[64:128, 64:128], 1.0), r=["blkf"], w=["blkf"])
        P.pool(lambda e: e.memset(chunkind[:], 0.0), w=["chunkind"])
        P.pool(lambda e: e.memset(chunkind[0:64, 0:1], 1.0), r=["chunkind"], w=["chunkind"])
        P.pool(lambda e: e.memset(chunkind[64:128, 1:2], 1.0), r=["chunkind"], w=["chunkind"])
        mk_mask(offd[:], 1.0, 0.0, [[-1, 128]], 1, ALU.not_equal, "offd")
        for (dst, src, kd, ks) in ((UBf, Uf, "UBf", "Uf"), (UBb, Ub, "UBb", "Ub"), (SBf, Sf, "SBf", "Sf"), (SBb, Sb_, "SBb", "Sb"),
                                   (nUBf, nUf, "nUBf", "nUf"), (nUBb, nUb, "nUBb", "nUb")):
            P.pool(lambda e, dst=dst, src=src: e.tensor_tensor(out=dst[:], in0=src[:], in1=blkf[:], op=ALU.mult), r=[ks, "blkf"], w=[kd])
        for j in range(4):
            P.dve(lambda e, j=j: e.tensor_scalar(out=NMBf[:, j, :], in0=UBb[:], scalar1=-NEG, scalar2=NEG, op0=ALU.mult, op1=ALU.add),
                  r=["UBb"], w=[("NMBf", j)])
            P.dve(lambda e, j=j: e.tensor_scalar(out=NMBb[:, j, :], in0=UBf[:], scalar1=-NEG, scalar2=NEG, op0=ALU.mult, op1=ALU.add),
                  r=["UBf"], w=[("NMBb", j)])
        NMBf_keys = [("NMBf", j) for j in range(4)]
        NMBb_keys = [("NMBb", j) for j in range(4)]
        P.dma("sp", lambda e: e.dma_start(out=cc_sb[:], in_=ccT), w=["cc_sb"])
        P.act(lambda e: e.activation(out=csil[:], in_=cc_sb[:], func=AF.Silu), r=["cc_sb"], w=["csil"])

        modT = sb(es0, "modT", [128, 48, 2], F32)
        s1 = sb(es0, "s1", [128, 8, 2], F32)
        s2 = sb(es0, "s2", [128, 8, 2], F32)
        g1row = sb(es0, "g1row", [128, 2, 8, 128], F32)
        g2row = sb(es0, "g2row", [128, 2, 8, 128], F32)
        small_all = sb(es0, "small_all", [128, NT, 24], F32)

        def bcast_load(es, name, src_ap, n):
            t = sb(es, name, [128, n], F32)
            P.dma("sp", lambda e: e.dma_start(out=t[:], in_=src_ap.partition_broadcast(128)), w=[name])
            return t

        def rsqrt_ops(dst_ap, src_ap, scale, keys_r, key_w, n_eps=None):
            P.dve(lambda e: e.tensor_scalar(out=dst_ap, in0=src_ap, scalar1=scale, scalar2=EPS,
                                            op0=ALU.mult, op1=ALU.add), r=keys_r, w=[key_w])
            P.act(lambda e: e.activation(out=dst_ap, in_=dst_ap, func=AF.Sqrt), r=[key_w], w=[key_w])
            P.dve(lambda e: e.reciprocal(out=dst_ap, in_=dst_ap), r=[key_w], w=[key_w])

        def phase_mod(l, es):
            wm = [sb(es, f"wm{i}", [128, 8, 512], F32) for i in range(2)]
            bT = sb(es, "bT", [128, 48], F32)
            nT = sb(es, "nT", [128, 2, 8], F32)
            mrs = sb(es, "mrs", [96, 128], F32)
            tmp = sb(es, "modtmp", [128, 8, 2], F32)
            P.dma("sp", lambda e: e.dma_start(out=bT[:], in_=b_modT[l]), w=["bT"])
            P.dma("sp", lambda e: e.dma_start(out=nT[:, 0, :], in_=n1T[l]), w=["nT0"])
            P.dma("sp", lambda e: e.dma_start(out=nT[:, 1, :], in_=n2T[l]), w=["nT1"])
            pm = banks[0][:, 0:96].rearrange("p (c j) -> p c j", j=2)
            for piece in range(12):
                s = piece % 2
                P.dma("sp", lambda e, s=s, piece=piece: e.dma_start(
                    out=wm[s][:], in_=w_mod[l][:, piece * 512:(piece + 1) * 512].rearrange("(k p) n -> p k n", p=128)),
                    w=[("wm", s)])
                for fc in range(4):
                    c = piece * 4 + fc
                    for k in range(8):
                        P.pe(lambda e, s=s, fc=fc, c=c, k=k: e.matmul(
                            pm[:, c, :], lhsT=wm[s][:, k, fc * 128:(fc + 1) * 128], rhs=csil[:, k, :],
                            start=(k == 0), stop=(k == 7)), r=[("wm", s), "csil"], w=[BK(0)])
            P.dve(lambda e: e.tensor_tensor(out=modT[:], in0=pm, in1=bT[:].unsqueeze(2).to_broadcast([128, 48, 2]),
                                            op=ALU.add), r=[BK(0), "bT"], w=["modT"])
            for (sx, lo, ni, nk) in ((s1, 8, 0, "nT0"), (s2, 32, 1, "nT1")):
                P.dve(lambda e, lo=lo: e.tensor_scalar(out=tmp[:], in0=modT[:, lo:lo + 8, :], scalar1=1.0, scalar2=None,
                                                       op0=ALU.add), r=["modT"], w=["modtmp"])
                P.dve(lambda e, sx=sx, ni=ni: e.tensor_tensor(
                    out=sx[:], in0=tmp[:], in1=nT[:, ni, :].unsqueeze(2).to_broadcast([128, 8, 2]), op=ALU.mult),
                    r=["modtmp", nk], w=["s1k" if ni == 0 else "s2k"])
            pmt = banks[1][0:96, 0:128]
            P.pe(lambda e: e.transpose(out=pmt, in_=modT[:].rearrange("p c j -> p (c j)"), identity=identf[:]),
                 r=["modT", "identf"], w=[BK(1)])
            P.act(lambda e: e.activation(out=mrs[:], in_=pmt, func=AF.Copy), r=[BK(1)], w=["mrs"])
            P.dma("sp", lambda e: e.dma_start(out=modrows, in_=mrs[:]), r=["mrs"], w=["modrows"])
            mr3 = modrows.rearrange("(c j) p -> j c p", j=2)
            for j in range(2):
                P.dma("sp", lambda e, j=j: e.dma_start(out=g1row[:, j, :, :], in_=mr3[j, 16:24, :].partition_broadcast(128)),
                      r=["modrows"], w=["g1row"])
                P.dma("sp", lambda e, j=j: e.dma_start(out=g2row[:, j, :, :], in_=mr3[j, 40:48, :].partition_broadcast(128)),
                      r=["modrows"], w=["g2row"])

        S1K = "s1k"
        S2K = "s2k"

        def norm_tile_T(es_tmp, tag, src_rows_ap, j, scl, shf_lo, dstT_ap_fn, bank_id, ring, fp32_path=None):
            raise NotImplementedError

        def phase_proj_attn(l, stream_in):
            lam_init = 0.8 - 0.6 * math.exp(-0.3 * l)
            with contextlib.ExitStack() as esA:
                QKT = sb(esA, "QKT", [128, 6, T], BF16)
                V1 = sb(esA, "V1", [128, NT, 4, 65], BF16)
                lamt = sb(esA, "lamt", [128, 4], F32)
                nlam = sb(esA, "nlam", [128, 1], F32)
                dnw = bcast_load(esA, "dnw", diff_norm_w[l], 64)
                P.pool(lambda e: e.memset(V1[:, :, :, 64:65], 1.0), w=["V1ones"])
                lv = [bcast_load(esA, f"lv{i}", a[l], 32) for i, a in
                      enumerate((diff_lq1, diff_lk1, diff_lq2, diff_lk2))]
                lj = sb(esA, "lj", [128, 32], F32)
                for i in range(2):
                    P.dve(lambda e, i=i: e.tensor_tensor(out=lj[:], in0=lv[2 * i][:], in1=lv[2 * i + 1][:], op=ALU.mult),
                          r=[f"lv{2 * i}", f"lv{2 * i + 1}"], w=["lj"])
                    P.dve(lambda e, i=i: e.tensor_reduce(out=lamt[:, i:i + 1], in_=lj[:], axis=AX.X, op=ALU.add),
                          r=["lj"], w=["lamt"])
                P.act(lambda e: e.activation(out=lamt[:, 2:4], in_=lamt[:, 0:2], func=AF.Exp), r=["lamt"], w=["lamt"])
                P.dve(lambda e: e.scalar_tensor_tensor(out=nlam[:], in0=lamt[:, 3:4], scalar=-lam_init, in1=lamt[:, 2:3],
                                                       op0=ALU.add, op1=ALU.subtract), r=["lamt"], w=["nlam"])

                with contextlib.ExitStack() as esB:
                    wib = sb(esB, "wib", [128, 8, IN_DIM], BF16)
                    wst = [sb(esB, f"wst{i}", [128, 1032], F32) for i in range(1)]
                    xt = [sb(esB, f"xt{i}", [128, D], F32) for i in range(2)]
                    xb = [sb(esB, f"xb{i}", [128, D], BF16) for i in range(2)]
                    junk = sb(esB, "junk", [128, D], BF16)
                    ss = [sb(esB, f"ss{i}", [128, 1], F32) for i in range(2)]
                    xnT = [sb(esB, f"xnT{i}", [128, 8, 512], BF16) for i in range(1)]
                    hst = [sb(esB, f"hst{i}", [128, 512], BF16) for i in range(3)]
                    wqk = sb(esB, "wqk", [128, 16, 32], F32)
                    cosT = sb(esB, "cosT", [128, 32, 32], F32)
                    sinT = sb(esB, "sinT", [128, 32, 32], F32)
                    sqb = sb(esB, "sqb", [128, 512], F32)
                    ssq = sb(esB, "ssq", [128, 16], F32)
                    qk32 = sb(esB, "qk32", [128, 16, 32], F32)
                    rtmp = sb(esB, "rtmp", [128, 16, 32], F32)
                    qkb = [sb(esB, f"qkb{i}", [128, 512], BF16) for i in range(2)]
                    zst = [sb(esB, f"zst{i}", [128, 512], BF16) for i in range(1)]
                    gst = [sb(esB, f"gst{i}", [128, 256], BF16) for i in range(2)]

                    n_p = 0
                    for k in range(8):
                        for c3 in range(3):
                            s = 0
                            n_p += 1
                            P.dma("sp", lambda e, s=s, k=k, c3=c3: e.dma_start(
                                out=wst[s][:], in_=w_in[l][k * 128:(k + 1) * 128, c3 * 1032:(c3 + 1) * 1032]),
                                w=[("wst", s)])
                            P.pool(lambda e, s=s, k=k, c3=c3: e.tensor_copy(
                                out=wib[:, k, c3 * 1032:(c3 + 1) * 1032], in_=wst[s][:]),
                                r=[("wst", s)], w=[("wib", k, c3)])
                    WIB = [("wib", k, c3) for k in range(8) for c3 in range(3)]
                    for gi in range(8):
                        P.dma("sp", lambda e, gi=gi: e.dma_start(out=wqk[:, gi, :], in_=diff_qn_w[l].partition_broadcast(128)),
                              w=["wqk"])
                        P.dma("sp", lambda e, gi=gi: e.dma_start(out=wqk[:, 8 + gi, :], in_=diff_kn_w[l].partition_broadcast(128)),
                              w=["wqk"])
                    P.dma("sp", lambda e: e.dma_start(out=cosT[:], in_=rope[0]), w=["cosT"])
                    P.dma("sp", lambda e: e.dma_start(out=sinT[:], in_=rope[1]), w=["sinT"])

                    ev = 0
                    if stop_after == "p1a":
                        return
                    for gidx, (t0, nt) in enumerate(GROUPS):
                        N = nt * 128
                        xs_ = 0
                        for ti in range(nt):
                            t = t0 + ti
                            j = 1 if t < NCTX_T else 0
                            a = t % 2
                            b2 = t % 2
                            P.dma("sp", lambda e, a=a, t=t: e.dma_start(out=xt[a][:], in_=stream_in[t * 128:(t + 1) * 128, :]),
                                  w=[("xt", a)])
                            P.act(lambda e, a=a: e.activation(out=junk[:], in_=xt[a][:], func=AF.Square, accum_out=ss[a][:]),
                                  r=[("xt", a)], w=["junk", ("ss", a)])
                            rsqrt_ops(ss[a][:], ss[a][:], 1.0 / D, [("ss", a)], ("ss", a))
                            P.dve(lambda e, a=a, b2=b2: e.tensor_scalar(out=xb[b2][:], in0=xt[a][:], scalar1=ss[a][:, 0:1],
                                                                        scalar2=None, op0=ALU.mult),
                                  r=[("xt", a), ("ss", a)], w=[("xb", b2)])
                            bk = 6 + b2
                            ptv = tbanks[bk][:].rearrange("p (k n) -> p k n", n=128)
                            for k in range(8):
                                P.pe(lambda e, b2=b2, k=k, ptv=ptv: e.transpose(out=ptv[:, k, :], in_=xb[b2][:, k * 128:(k + 1) * 128],
                                                                               identity=identb[:]),
                                     r=[("xb", b2), "identb"], w=[BK(bk)])
                            for k in range(8):
                                dst = xnT[xs_][:, k, ti * 128:(ti + 1) * 128]
                                if True:
                                    P.act(lambda e, dst=dst, k=k, j=j, ptv=ptv: e.activation(
                                        out=dst, in_=ptv[:, k, :], func=AF.Identity, scale=s1[:, k, j:j + 1], bias=modT[:, k, j:j + 1]),
                                        r=[BK(bk), "s1k", "modT"], w=[("xnT", xs_, ti, k)])
                                else:
                                    P.dve(lambda e, dst=dst, k=k, j=j, ptv=ptv: e.tensor_scalar(
                                        out=dst, in0=ptv[:, k, :], scalar1=s1[:, k, j:j + 1], scalar2=modT[:, k, j:j + 1],
                                        op0=ALU.mult, op1=ALU.add),
                                        r=[BK(bk), "s1k", "modT"], w=[("xnT", xs_, ti, k)])
                        XN = [("xnT", xs_, ti, k) for ti in range(nt) for k in range(8)]
                        if stop_after == "p1b":
                            return
                        for ch in range(12):
                            col0 = (C_XBC + ch * 128) if ch < 6 else (C_GQKV + (ch - 6) * 128)
                            bk = ev % 3
                            ev += 1
                            for k in range(8):
                                P.pe(lambda e, bk=bk, k=k, col0=col0, N=N, xs_=xs_: e.matmul(
                                    banks[bk][:, 0:N], lhsT=wib[:, k, col0:col0 + 128], rhs=xnT[xs_][:, k, 0:N],
                                    start=(k == 0), stop=(k == 7)), r=WIB + XN, w=[BK(bk)])
                            P.act(lambda e, bk=bk, N=N: e.activation(out=hst[bk][:, 0:N], in_=banks[bk][:, 0:N], func=AF.Copy),
                                  r=[BK(bk)], w=[("hst", bk)])
                            P.dma("pool", lambda e, bk=bk, ch=ch, t0=t0, N=N: e.dma_start(
                                out=hT[ch, :, t0 * 128:t0 * 128 + N], in_=hst[bk][:, 0:N]), r=[("hst", bk)], w=[("hT", ch, gidx)])
                        if stop_after == "p1c":
                            return
                        for ti in range(nt):
                            t = t0 + ti
                            latent = t >= NCTX_T
                            tk = slice(ti * 128, (ti + 1) * 128)
                            specs = [(3, 0, 512, C_QKV), (4, 0, 256, C_QKV + 512), (4, 256, 256, C_GATE),
                                     (5, 0, 512, C_Z), (3, 0, 0, 0)]
                            for (bk, o0, w_, c0) in specs[:4]:
                                for k in range(8):
                                    P.pe(lambda e, bk=bk, o0=o0, w_=w_, c0=c0, k=k, tk=tk, xs_=xs_: e.matmul(
                                        banks[bk][:, o0:o0 + w_], lhsT=xnT[xs_][:, k, tk], rhs=wib[:, k, c0:c0 + w_],
                                        start=(k == 0), stop=(k == 7)), r=WIB + XN, w=[BK(bk)])
                            P.act(lambda e: e.activation(out=sqb[:], in_=banks[3][:], func=AF.Square), r=[BK(3)], w=["sqb"])
                            P.dve(lambda e: e.tensor_reduce(out=ssq[:], in_=sqb[:].rearrange("p (g d) -> p g d", d=32),
                                                            axis=AX.X, op=ALU.add), r=["sqb"], w=["ssq"])
                            rsqrt_ops(ssq[:], ssq[:], 1.0 / 32, ["ssq"], "ssq")
                            P.dve(lambda e: e.tensor_tensor(out=qk32[:], in0=banks[3][:].rearrange("p (g d) -> p g d", d=32),
                                                            in1=ssq[:].unsqueeze(2).to_broadcast([128, 16, 32]), op=ALU.mult),
                                  r=[BK(3), "ssq"], w=["qk32"])
                            q2 = t % 2
                            if latent:
                                lt = t - NCTX_T
                                P.dve(lambda e: e.tensor_tensor(out=qk32[:], in0=qk32[:], in1=wqk[:], op=ALU.mult),
                                      r=["qk32", "wqk"], w=["qk32"])
                                x5 = qk32[:].rearrange("p g (a h e) -> p g a h e", a=2, h=2, e=8)
                                r5 = rtmp[:].rearrange("p g (a h e) -> p g a h e", a=2, h=2, e=8)
                                s4 = sinT[:, lt, :].rearrange("p (a h e) -> p a h e", a=2, h=2, e=8)
                                for hh in range(2):
                                    P.dve(lambda e, hh=hh, x5=x5, r5=r5, s4=s4: e.tensor_tensor(
                                        out=r5[:, :, :, hh, :], in0=x5[:, :, :, 1 - hh, :],
                                        in1=s4[:, :, hh, :].unsqueeze(1).to_broadcast([128, 16, 2, 8]), op=ALU.mult),
                                        r=["qk32", "sinT"], w=[("rtmp", hh)])
                                P.dve(lambda e, lt=lt: e.tensor_tensor(
                                    out=qk32[:], in0=qk32[:], in1=cosT[:, lt, :].unsqueeze(1).to_broadcast([128, 16, 32]),
                                    op=ALU.mult), r=["qk32", "cosT", ("rtmp", 0), ("rtmp", 1)], w=["qk32"])
                                P.dve(lambda e, q2=q2: e.tensor_tensor(
                                    out=qkb[q2][:].rearrange("p (g d) -> p g d", d=32), in0=qk32[:], in1=rtmp[:], op=ALU.add),
                                    r=["qk32", ("rtmp", 0), ("rtmp", 1)], w=[("qkb", q2)])
                            else:
                                P.dve(lambda e, q2=q2: e.tensor_tensor(
                                    out=qkb[q2][:].rearrange("p (g d) -> p g d", d=32), in0=qk32[:], in1=wqk[:], op=ALU.mult),
                                    r=["qk32", "wqk"], w=[("qkb", q2)])
                            bk = 6 + q2
                            ptq = tbanks[bk][:, 0:768].rearrange("p (c n) -> p c n", n=128)
                            for c in range(6):
                                lo = (c // 3) * 256 + (c % 3) * 96
                                wd = 96 if (c % 3) < 2 else 64
                                P.pe(lambda e, c=c, q2=q2, ptq=ptq, lo=lo, wd=wd: e.transpose(
                                    out=ptq[0:wd, c, :], in_=qkb[q2][:, lo:lo + wd], identity=identb[:]),
                                     r=[("qkb", q2), "identb"], w=[BK(bk)])
                            P.act(lambda e, t=t, ptq=ptq: e.activation(out=QKT[0:96, :, t * 128:(t + 1) * 128], in_=ptq[0:96, :, :],
                                                                       func=AF.Copy),
                                  r=[BK(bk)], w=[("QKT", t)])
                            if stop_after == "p1d":
                                return
                            P.dve(lambda e, t=t: e.tensor_copy(out=V1[:, t, :, 0:64],
                                                               in_=banks[4][:, 0:256].rearrange("p (h v) -> p h v", v=64)),
                                  r=[BK(4)], w=[("V1", t)])
                            P.act(lambda e, q2=q2: e.activation(out=gst[q2][:], in_=banks[4][:, 256:512], func=AF.Silu),
                                  r=[BK(4)], w=[("gst", q2)])
                            P.dma("pool", lambda e, q2=q2, t=t: e.dma_start(out=gs[t * 128:(t + 1) * 128, :], in_=gst[q2][:]),
                                  r=[("gst", q2)], w=[("gs", t)])
                            P.act(lambda e, q2=q2: e.activation(out=zst[0][:], in_=banks[5][:], func=AF.Silu),
                                  r=[BK(5)], w=[("zst", 0)])
                            P.dma("pool", lambda e, q2=q2, t=t: e.dma_start(out=zs[t * 128:(t + 1) * 128, :], in_=zst[0][:]),
                                  r=[("zst", 0)], w=[("zs", t)])
                            for (o0, w_, c0) in ((0, 8, C_DT), (8, 16, C_BETA)):
                                for k in range(8):
                                    P.pe(lambda e, o0=o0, w_=w_, c0=c0, k=k, tk=tk, xs_=xs_: e.matmul(
                                        banks[5][:, o0:o0 + w_], lhsT=xnT[xs_][:, k, tk], rhs=wib[:, k, c0:c0 + w_],
                                        start=(k == 0), stop=(k == 7)), r=WIB + XN, w=[BK(5)])
                            P.dve(lambda e, t=t: e.tensor_copy(out=small_all[:, t, :], in_=banks[5][:, 0:24]),
                                  r=[BK(5)], w=[("small", t)])
                            if stop_after == "p1e" or (stop_after == "p1g" and t == 2):
                                return
                        if stop_after == "p1f":
                            return
                P.barrier()
                if stop_after == "proj":
                    return
                with contextlib.ExitStack() as esC:
                    PT = [sb(esC, f"PT{i}", [128, 512], BF16) for i in range(3)]
                    osb = [sb(esC, f"osb{i}", [128, 4, 2, 65], F32) for i in range(2)]
                    rden = sb(esC, "rden", [128, 4, 2], F32)
                    o1 = sb(esC, "o1", [128, 4, 64], F32)
                    od = sb(esC, "od", [128, 4, 64], F32)
                    osq = sb(esC, "osq", [128, 4, 64], F32)
                    orr = sb(esC, "orr", [128, 4], F32)
                    aout = [sb(esC, f"aout{i}", [128, 4, 4, 64], BF16) for i in range(2)]
                    scale = 32 ** -0.5
                    QKALL = [("QKT", t) for t in range(NT)]
                    VALL = [("V1", t) for t in range(NT)] + ["V1ones"]
                    items = []
                    oTs = [sb(esC, f"oTs{i}", [128, 512], F32) for i in range(2)]
                    for i_ in range(2):
                        P.pool(lambda e, i_=i_: e.memset(oTs[i_][64:96, :], 0.0), w=["oTsz"])

                    def mk_item(idx, gidx, t0, nt, h, m, ki, kt, nk, ob, ao):
                        N = nt * 128
                        mm = 2 * h + m
                        c = mm // 3
                        pb = 32 * (mm % 3)
                        accb = 4 + (mm % 2)
                        accv = banks[accb][:, 0:nt * 65].rearrange("p (q v) -> p q v", v=65)
                        sbk = idx % 3

                        def qk():
                            P.pe(lambda e: e.matmul(
                                banks[sbk][:, 0:N], lhsT=QKT[pb:pb + 32, 3 + c, kt * 128:(kt + 1) * 128],
                                rhs=QKT[pb:pb + 32, c, t0 * 128:t0 * 128 + N], start=True, stop=True),
                                r=QKALL, w=[BK(sbk)])

                        def rest():
                            P.act(lambda e: e.activation(out=PT[sbk][:, 0:N], in_=banks[sbk][:, 0:N], func=AF.Exp, scale=scale),
                                  r=[BK(sbk)], w=[("PT", sbk)])
                            P.pe(lambda e: e.matmul(banks[accb][0:65, 0:N], lhsT=V1[:, kt, h, :], rhs=PT[sbk][:, 0:N],
                                                    start=(ki == 0), stop=(ki == nk - 1)),
                                 r=[("PT", sbk)] + VALL, w=[BK(accb)])
                            if ki == nk - 1:
                                os_ = oTs[mm % 2]
                                P.act(lambda e: e.activation(out=os_[0:65, 0:N], in_=banks[accb][0:65, 0:N], func=AF.Copy),
                                      r=[BK(accb), "oTsz"], w=[("oTs", mm % 2)])
                                tv = banks[accb][:, 0:nt * 96].rearrange("p (q v) -> p q v", v=96)
                                for qb in range(nt):
                                    P.pe(lambda e, qb=qb: e.transpose(out=tv[:, qb, :], in_=os_[0:96, qb * 128:(qb + 1) * 128],
                                                                      identity=identf[0:96, 0:96]),
                                         r=[("oTs", mm % 2), "identf", "oTsz"], w=[BK(accb)])
                                P.act(lambda e: e.activation(out=ob[:, 0:nt, m, :], in_=tv[:, :, 0:65], func=AF.Copy),
                                      r=[BK(accb)], w=[("osb", h % 2, m)])
                                if m == 1:
                                    head_post(gidx, nt, h, ob, ao)
                                    if h == 3:
                                        for qb in range(nt):
                                            t = t0 + qb
                                            P.dma("pool", lambda e, qb=qb, t=t: e.dma_start(
                                                out=ml[t * 128:(t + 1) * 128, 0:256], in_=ao[:, qb, :, :].rearrange("p h v -> p (h v)")),
                                                r=[("aout", gidx % 2, hh_) for hh_ in range(4)], w=[("ml_a", t)])
                        return qk, rest

                    def head_post(gidx, nt, h, ob, ao):
                        OK_ = [("osb", h % 2, 0), ("osb", h % 2, 1)]
                        P.dve(lambda e: e.reciprocal(out=rden[:, 0:nt, :], in_=ob[:, 0:nt, :, 64]), r=OK_, w=["rden"])
                        P.dve(lambda e: e.tensor_tensor(
                            out=o1[:, 0:nt, :], in0=ob[:, 0:nt, 1, 0:64],
                            in1=rden[:, 0:nt, 1:2].to_broadcast([128, nt, 64]), op=ALU.mult), r=OK_ + ["rden"], w=["o1"])
                        P.dve(lambda e: e.tensor_tensor(
                            out=od[:, 0:nt, :], in0=ob[:, 0:nt, 0, 0:64],
                            in1=rden[:, 0:nt, 0:1].to_broadcast([128, nt, 64]), op=ALU.mult), r=OK_ + ["rden"], w=["od"])
                        P.dve(lambda e: e.scalar_tensor_tensor(
                            out=od[:, 0:nt, :], in0=o1[:, 0:nt, :], scalar=nlam[:, 0:1], in1=od[:, 0:nt, :],
                            op0=ALU.mult, op1=ALU.add), r=["o1", "od", "nlam"], w=["od"])
                        P.pool(lambda e: e.tensor_tensor(out=osq[:, 0:nt, :], in0=od[:, 0:nt, :], in1=od[:, 0:nt, :],
                                                         op=ALU.mult), r=["od"], w=["osq"])
                        P.dve(lambda e: e.tensor_reduce(out=orr[:, 0:nt], in_=osq[:, 0:nt, :], axis=AX.X, op=ALU.add),
                              r=["osq"], w=["orr"])
                        rsqrt_ops(orr[:, 0:nt], orr[:, 0:nt], 1.0 / 64, ["orr"], "orr")
                        P.dve(lambda e: e.tensor_tensor(out=od[:, 0:nt, :], in0=od[:, 0:nt, :],
                                                        in1=orr[:, 0:nt].unsqueeze(2).to_broadcast([128, nt, 64]),
                                                        op=ALU.mult), r=["od", "orr"], w=["od"])
                        P.dve(lambda e: e.scalar_tensor_tensor(
                            out=ao[:, 0:nt, h, :], in0=od[:, 0:nt, :], scalar=(1.0 - lam_init),
                            in1=dnw[:].unsqueeze(1).to_broadcast([128, nt, 64]), op0=ALU.mult, op1=ALU.mult),
                            r=["od", "dnw"], w=[("aout", gidx % 2, h)])

                    for gidx, (t0, nt) in enumerate(GROUPS):
                        ktiles = list(range(NCTX_T)) if gidx == 0 else list(range(NT))
                        ao = aout[gidx % 2]
                        for h in range(4):
                            ob = osb[h % 2]
                            for m in range(2):
                                for ki, kt in enumerate(ktiles):
                                    items.append(mk_item(len(items), gidx, t0, nt, h, m, ki, kt, len(ktiles), ob, ao))
                    LOOK = 2
                    for i in range(len(items) + LOOK):
                        if i < len(items):
                            items[i][0]()
                        if i - LOOK >= 0:
                            items[i - LOOK][1]()
                P.barrier()

        ORDER = [list(range(NT)), [1, 0] + list(range(NT - 1, NCTX_T - 1, -1))]

        def conv_chunk(wT_ap, chs, dgs, cw, key_cw, bias_ap_fn, hcs, sink, hoff):
            pass

        def phase_ssm(l):
            with contextlib.ExitStack() as esA:
                xs_tm = sb(esA, "xs_tm", [128, NT, 512], BF16)
                B_tm = sb(esA, "B_tm", [128, NT, 128], BF16)
                BT = sb(esA, "BT", [128, T], BF16)
                CT = sb(esA, "CT", [128, T], BF16)
                BTm = [sb(esA, f"BTm{i}", [128, T], BF16) for i in range(2)]
                P.pool(lambda e: e.memset(BTm[0][64:128, :], 0.0), w=["BTm0z"])
                P.pool(lambda e: e.memset(BTm[1][0:64, :], 0.0), w=["BTm1z"])
                Y = sb(esA, "Y", [128, NT, 512], BF16)
                dt_all = sb(esA, "dt_all", [128, NT, 2, 8], F32)
                dA_all = sb(esA, "dA_all", [128, NT, 2, 8], F32)
                hS = sb(esA, "hS", [128, 2, 4, 64], F32)
                hSb = sb(esA, "hSb", [128, 2, 4, 64], BF16)
                dtb = bcast_load(esA, "dtb", ssm_dt_bias[l].rearrange("a b -> (a b)"), 16)
                alg = bcast_load(esA, "alg", ssm_a_log[l].rearrange("a b -> (a b)"), 16)
                dsk = bcast_load(esA, "dsk", ssm_d[l], 8)
                nw = bcast_load(esA, "snw", ssm_norm_w[l], 512)
                tmpd = sb(esA, "tmpd", [128, NT, 8], F32)
                P.act(lambda e: e.activation(out=alg[:], in_=alg[:], func=AF.Exp), r=["alg"], w=["alg"])
                P.dve(lambda e: e.tensor_scalar(out=alg[:], in0=alg[:], scalar1=-1.0, scalar2=None, op0=ALU.mult), r=["alg"], w=["alg"])
                SM = [("small", t) for t in range(NT)]
                for d in range(2):
                    P.dve(lambda e, d=d: e.tensor_tensor(out=tmpd[:], in0=small_all[:, :, 0:8],
                                                         in1=dtb[:, d * 8:(d + 1) * 8].unsqueeze(1).to_broadcast([128, NT, 8]), op=ALU.add),
                          r=SM + ["dtb"], w=["tmpd"])
                    P.act(lambda e: e.activation(out=tmpd[:], in_=tmpd[:], func=AF.Exp), r=["tmpd"], w=["tmpd"])
                    P.act(lambda e, d=d: e.activation(out=dt_all[:, :, d, :], in_=tmpd[:], func=AF.Ln, bias=onesf[:, 0:1], scale=1.0),
                          r=["tmpd", "onesf"], w=[("dt_all", d)])
                    P.dve(lambda e, d=d: e.tensor_tensor(out=dA_all[:, :, d, :], in0=dt_all[:, :, d, :],
                                                         in1=alg[:, d * 8:(d + 1) * 8].unsqueeze(1).to_broadcast([128, NT, 8]), op=ALU.mult),
                          r=[("dt_all", d), "alg"], w=[("dA_all", d)])
                P.pool(lambda e: e.memset(hS[:], 0.0), w=[("hS", 0), ("hS", 1)])
                P.pool(lambda e: e.memset(hSb[:], 0.0), w=[("hSb", 0), ("hSb", 1)])
                with contextlib.ExitStack() as esB:
                    cw = sb(esB, "cw", [128, 6, 5], F32)
                    cb = sb(esB, "cb", [128, 6], F32)
                    dg = sb(esB, "dg", [128, 30, 128], BF16)
                    hc = [sb(esB, f"hc{i}", [128, T], BF16) for i in range(2)]
                    cst = [sb(esB, f"cst{i}", [128, 512], BF16) for i in range(2)]
                    P.dma("sp", lambda e: e.dma_start(out=cw[:], in_=ssm_conv_wT[l]), w=["cw"])
                    P.dma("sp", lambda e: e.dma_start(out=cb[:], in_=ssm_conv_bT[l]), w=["cb"])
                    for ch in range(6):
                        for jt in range(5):
                            P.dve(lambda e, ch=ch, jt=jt: e.tensor_scalar(out=dg[:, ch * 5 + jt, :], in0=identf[:], scalar1=cw[:, ch, jt:jt + 1],
                                                                          scalar2=None, op0=ALU.mult), r=["identf", "cw"], w=[("dg", ch)])
                    cnt = 0
                    for ch in range(6):
                        hs_ = ch % 2
                        P.dma("sp", lambda e, hs_=hs_, ch=ch: e.dma_start(out=hc[hs_][:], in_=hT[ch]),
                              r=[("hT", ch, gi) for gi in range(9)], w=[("hc", hs_)])
                        for gidx, (t0, nt) in enumerate(GROUPS):
                            N = nt * 128
                            tok0 = t0 * 128
                            seg_lo, seg_hi = (0, 256) if gidx == 0 else (256, T)
                            bk = cnt % 2
                            cnt += 1
                            taps = [2, 0, 1, 3, 4]
                            for ii, jt in enumerate(taps):
                                o = jt - 2
                                a_ = max(0, seg_lo - tok0 - o)
                                b_ = min(N, seg_hi - tok0 - o)
                                P.pe(lambda e, bk=bk, ch=ch, jt=jt, a_=a_, b_=b_, o=o, tok0=tok0, hs_=hs_, ii=ii: e.matmul(
                                    banks[bk][:, a_:b_], lhsT=dg[:, ch * 5 + jt, :], rhs=hc[hs_][:, tok0 + a_ + o:tok0 + b_ + o],
                                    start=(ii == 0), stop=(ii == 4)), r=[("dg", ch), ("hc", hs_)], w=[BK(bk)])
                            if ch < 4 or ch == 4:
                                dst = cst[bk][:, 0:N] if ch < 4 else BT[:, tok0:tok0 + N]
                                dkey = ("cst", bk) if ch < 4 else ("BT", gidx)
                                P.act(lambda e, bk=bk, dst=dst, ch=ch, N=N: e.activation(out=dst, in_=banks[bk][:, 0:N], func=AF.Silu,
                                                                                       bias=cb[:, ch:ch + 1], scale=1.0),
                                      r=[BK(bk), "cb"], w=[dkey])
                                if ch == 4:
                                    for g_ in range(2):
                                        pg = slice(64 * g_, 64 * g_ + 64)
                                        P.pool(lambda e, g_=g_, pg=pg, tok0=tok0, N=N: e.tensor_copy(out=BTm[g_][pg, tok0:tok0 + N], in_=BT[pg, tok0:tok0 + N]),
                                               r=[dkey, f"BTm{g_}z"], w=[("BTm", g_, gidx)])
                                for ti in range(nt):
                                    t = t0 + ti
                                    tb_ = 6 + (t % 2)
                                    src = (cst[bk][:, ti * 128:(ti + 1) * 128] if ch < 4 else BT[:, t * 128:(t + 1) * 128])
                                    P.pe(lambda e, tb_=tb_, src=src: e.transpose(out=tbanks[tb_][:, 0:128], in_=src, identity=identb[:]),
                                         r=[dkey, "identb"], w=[BK(tb_)])
                                    if ch < 4:
                                        P.dve(lambda e, tb_=tb_, t=t, ch=ch: e.tensor_copy(out=xs_tm[:, t, ch * 128:(ch + 1) * 128], in_=tbanks[tb_][:, 0:128]),
                                              r=[BK(tb_)], w=[("xs_tm", t, ch)])
                                    else:
                                        P.dve(lambda e, tb_=tb_, t=t: e.tensor_copy(out=B_tm[:, t, :], in_=tbanks[tb_][:, 0:128]),
                                              r=[BK(tb_)], w=[("B_tm", t)])
                            else:
                                P.act(lambda e, bk=bk, ch=ch, N=N, tok0=tok0: e.activation(out=CT[:, tok0:tok0 + N], in_=banks[bk][:, 0:N], func=AF.Silu,
                                                                                         bias=cb[:, ch:ch + 1], scale=1.0),
                                      r=[BK(bk), "cb"], w=[("CT", gidx)])
                P.barrier()
                if stop_after == "conv":
                    return
                with contextlib.ExitStack() as esC:
                    rhsU = [sb(esC, f"rhsU{i}", [128, 8, 128], F32) for i in range(2)]
                    rhsB = [sb(esC, f"rhsB{i}", [128, 8, 128], F32) for i in range(2)]
                    Erow = [sb(esC, f"Erow{i}", [128, 8, 128], F32) for i in range(2)]
                    dec = [sb(esC, f"dec{i}", [128, 8, 128], BF16) for i in range(2)]
                    MT = [sb(esC, f"MT{i}", [128, 8, 128], BF16) for i in range(2)]
                    xdt = [sb(esC, f"xdt{i}", [128, 8, 64], BF16) for i in range(2)]
                    xdtw = [sb(esC, f"xdtw{i}", [128, 8, 64], BF16) for i in range(2)]
                    CsT = [sb(esC, f"CsT{i}", [128, 2, 4, 128], BF16) for i in range(2)]
                    for i_ in range(2):
                        P.pool(lambda e, i_=i_: e.memset(CsT[i_][:], 0.0), w=[("CsT", i_, 0), ("CsT", i_, 1)])
                    wdec = [sb(esC, f"wdec{i}", [128, 8], F32) for i in range(2)]
                    dtw = [sb(esC, f"dtw{i}", [128, 8], F32) for i in range(2)]
                    XS = lambda t: [("xs_tm", t, ch) for ch in range(4)]
                    BTALL = [("BT", gi) for gi in range(9)]
                    CTALL = [("CT", gi) for gi in range(9)]
                    seen = set()

                    def sproc(d, t):
                        if True:
                            X0, X1, X2 = banks[3 * d], banks[3 * d + 1], banks[3 * d + 2]
                            K0, K1, K2 = BK(3 * d), BK(3 * d + 1), BK(3 * d + 2)
                            U_, nU_, NM_, S__ = (Uf, nUf, NMf, Sf) if d == 0 else (Ub, nUb, NMb, Sb_)
                            Uk, nUk, NMk, Sk = (("Uf", "nUf", NMf_keys, "Sf") if d == 0 else ("Ub", "nUb", NMb_keys, "Sb"))
                            lastc = 127 if d == 0 else 0
                            tsl = slice(t * 128, (t + 1) * 128)
                            dA = dA_all[:, t, d, :]
                            dtd = dt_all[:, t, d, :]
                            P.pool(lambda e, d=d, U_=U_, dA=dA: e.tensor_tensor(
                                out=rhsU[d][:], in0=U_[:].unsqueeze(1).to_broadcast([128, 8, 128]),
                                in1=dA.unsqueeze(2).to_broadcast([128, 8, 128]), op=ALU.mult),
                                r=[Uk, ("dA_all", d)], w=[("rhsU", d)])
                            P.pool(lambda e, d=d, dA=dA: e.tensor_copy(out=rhsB[d][:], in_=dA.unsqueeze(2).to_broadcast([128, 8, 128])),
                                   r=[("dA_all", d)], w=[("rhsB", d)])
                            for hb in range(2):
                                hsl = slice(4 * hb, 4 * hb + 4)
                                P.pe(lambda e, d=d, hb=hb, hsl=hsl: e.matmul(X0[:], lhsT=onesf[:], rhs=rhsU[d][:, hsl, :].rearrange("p r l -> p (r l)"),
                                                                             start=True, stop=True), r=["onesf", ("rhsU", d)], w=[K0])
                                P.act(lambda e, d=d, hb=hb, hsl=hsl: e.activation(out=Erow[d][:, hsl, :].rearrange("p r l -> p (r l)"), in_=X0[:], func=AF.Exp),
                                      r=[K0], w=[("Erow", d, hb)])
                                P.pe(lambda e, d=d, hb=hb, hsl=hsl, nU_=nU_: e.matmul(X0[:], lhsT=nU_[:], rhs=rhsB[d][:, hsl, :].rearrange("p r l -> p (r l)"),
                                                                                     start=False, stop=False, skip_group_check=True), r=[nUk, ("rhsB", d), ("Erow", d, hb)], w=[K0])
                                P.pe(lambda e, hb=hb, NM_=NM_: e.matmul(X0[:], lhsT=identf[:], rhs=NM_[:].rearrange("p r l -> p (r l)"),
                                                                       start=False, stop=True, skip_group_check=True), r=["identf"] + NMk, w=[K0])
                                P.act(lambda e, d=d, hb=hb, hsl=hsl: e.activation(out=dec[d][:, hsl, :].rearrange("p r l -> p (r l)"), in_=X0[:], func=AF.Exp),
                                      r=[K0], w=[("dec", d, hb)])
                            yield
                            for g in range(2):
                                P.pe(lambda e, g=g, tsl=tsl: e.matmul(X1[:, g * 128:(g + 1) * 128], lhsT=BTm[g][:, tsl],
                                                                      rhs=CT[:, tsl], start=True, stop=True),
                                     r=[("BTm", g, gi) for gi in range(9)] + CTALL, w=[K1])
                            for g in range(2):
                                P.dve(lambda e, d=d, g=g: e.tensor_tensor(
                                    out=MT[d][:, 4 * g:4 * g + 4, :], in0=dec[d][:, 4 * g:4 * g + 4, :],
                                    in1=X1[:, g * 128:(g + 1) * 128].unsqueeze(1).to_broadcast([128, 4, 128]), op=ALU.mult),
                                    r=[("dec", d, g), K1], w=[("MT", d, g)])
                            P.pe(lambda e, S__=S__, dA=dA: e.matmul(X1[:, 256:264], lhsT=S__[:], rhs=dA, start=True, stop=True),
                                 r=[Sk, ("dA_all", d)], w=[K1])
                            P.act(lambda e, d=d: e.activation(out=wdec[d][:], in_=X1[:, 256:264], func=AF.Exp), r=[K1], w=[("wdec", d)])
                            P.dve(lambda e, d=d, dtd=dtd: e.tensor_tensor(out=dtw[d][:], in0=wdec[d][:], in1=dtd, op=ALU.mult),
                                  r=[("wdec", d), ("dt_all", d)], w=[("dtw", d)])
                            xsv = xs_tm[:, t, :].rearrange("p (r x) -> p r x", x=64)
                            P.pool(lambda e, d=d, xsv=xsv, dtd=dtd: e.tensor_tensor(out=xdt[d][:], in0=xsv, in1=dtd.unsqueeze(2).to_broadcast([128, 8, 64]),
                                                                                   op=ALU.mult), r=XS(t) + [("dt_all", d)], w=[("xdt", d)])
                            P.dve(lambda e, d=d, xsv=xsv: e.tensor_tensor(out=xdtw[d][:], in0=xsv, in1=dtw[d][:].unsqueeze(2).to_broadcast([128, 8, 64]),
                                                                          op=ALU.mult), r=XS(t) + [("dtw", d)], w=[("xdtw", d)])
                            for g in range(2):
                                ps_ = slice(64 * g, 64 * g + 64)
                                P.pool(lambda e, d=d, g=g, ps_=ps_, tsl=tsl: e.tensor_tensor(
                                    out=CsT[d][ps_, g, :, :], in0=CT[ps_, tsl].unsqueeze(1).to_broadcast([64, 4, 128]),
                                    in1=Erow[d][ps_, 4 * g:4 * g + 4, :], op=ALU.mult),
                                    r=CTALL + [("Erow", d, g)], w=[("CsT", d, g)])
                            yield
                            for r8 in range(8):
                                g = r8 // 4
                                ps_ = slice(64 * g, 64 * g + 64)
                                ysl = slice(r8 * 64, (r8 + 1) * 64)
                                P.pe(lambda e, d=d, r8=r8, ysl=ysl: e.matmul(X2[:, ysl], lhsT=MT[d][:, r8, :], rhs=xdt[d][:, r8, :],
                                                                             start=True, stop=False),
                                     r=[("MT", d, g), ("xdt", d)], w=[K2])
                                P.pe(lambda e, d=d, r8=r8, ysl=ysl, g=g: e.matmul(X2[:, ysl], lhsT=CsT[d][:, g, r8 % 4, :], rhs=hSb[:, d, r8 % 4, :],
                                                                                 start=False, stop=True),
                                     r=[("CsT", d, g), ("hSb", d)], w=[K2])
                            if t not in seen:
                                seen.add(t)
                                P.act(lambda e, t=t: e.activation(out=Y[:, t, :], in_=X2[:], func=AF.Copy), r=[K2], w=[("Y", t)])
                            else:
                                P.dve(lambda e, t=t: e.tensor_tensor(out=Y[:, t, :], in0=X2[:], in1=Y[:, t, :], op=ALU.add),
                                      r=[K2, ("Y", t)], w=[("Y", t)])
                            yield
                            P.pe(lambda e, d=d, t=t: e.matmul(X1[:], lhsT=B_tm[:, t, :], rhs=xdtw[d][:].rearrange("p r x -> p (r x)"),
                                                              start=True, stop=True), r=[("B_tm", t), ("xdtw", d)], w=[K1])
                            for g in range(2):
                                ps_ = slice(64 * g, 64 * g + 64)
                                P.dve(lambda e, d=d, g=g, ps_=ps_, lastc=lastc: e.tensor_tensor(
                                    out=hS[ps_, d, :, :], in0=hS[ps_, d, :, :],
                                    in1=Erow[d][ps_, 4 * g:4 * g + 4, lastc:lastc + 1].to_broadcast([64, 4, 64]), op=ALU.mult),
                                    r=[("hS", d), ("Erow", d, g)], w=[("hS", d)])
                                P.dve(lambda e, d=d, g=g, ps_=ps_: e.tensor_tensor(
                                    out=hS[ps_, d, :, :], in0=hS[ps_, d, :, :],
                                    in1=X1[ps_, 256 * g:256 * g + 256].rearrange("p (r x) -> p r x", x=64), op=ALU.add),
                                    r=[("hS", d), K1], w=[("hS", d)])
                            P.pool(lambda e, d=d: e.tensor_copy(out=hSb[:, d, :, :], in_=hS[:, d, :, :]), r=[("hS", d)], w=[("hSb", d)])

                    def run_interleaved_s(fn):
                        for step in range(NT):
                            gens = [fn(d_, ORDER[d_][step]) for d_ in range(2)]
                            while gens:
                                for g_ in list(gens):
                                    try:
                                        next(g_)
                                    except StopIteration:
                                        gens.remove(g_)
                    run_interleaved_s(sproc)
                    zt_ = [sb(esC, f"zt_{i}", [128, 512], BF16) for i in range(2)]
                    yy = [sb(esC, f"yy{i}", [128, 512], F32) for i in range(2)]
                    ysq = sb(esC, "ysq", [128, 512], F32)
                    ssg = [sb(esC, f"ssg{i}", [128, 2], F32) for i in range(2)]
                    yo = [sb(esC, f"yo{i}", [128, 512], BF16) for i in range(2)]
                    for t in range(NT):
                        a = t % 2
                        rows = slice(t * 128, (t + 1) * 128)
                        P.dma("sp", lambda e, a=a, rows=rows: e.dma_start(out=zt_[a][:], in_=zs[rows, :]), r=[("zs", t)], w=[("zt_", a)])
                        P.dve(lambda e, a=a, t=t: e.tensor_tensor(out=yy[a][:].rearrange("p (r x) -> p r x", x=64),
                                                                  in0=xs_tm[:, t, :].rearrange("p (r x) -> p r x", x=64),
                                                                  in1=dsk[:].unsqueeze(2).to_broadcast([128, 8, 64]), op=ALU.mult),
                              r=XS(t) + ["dsk"], w=[("yy", a)])
                        P.pool(lambda e, a=a, t=t: e.tensor_tensor(out=yy[a][:], in0=yy[a][:], in1=Y[:, t, :], op=ALU.add),
                               r=[("yy", a), ("Y", t)], w=[("yy", a)])
                        P.dve(lambda e, a=a: e.tensor_tensor(out=yy[a][:], in0=yy[a][:], in1=zt_[a][:], op=ALU.mult),
                              r=[("yy", a), ("zt_", a)], w=[("yy", a)])
                        for g in range(2):
                            P.act(lambda e, a=a, g=g: e.activation(out=ysq[:, g * 256:(g + 1) * 256], in_=yy[a][:, g * 256:(g + 1) * 256],
                                                                   func=AF.Square, accum_out=ssg[a][:, g:g + 1]),
                                  r=[("yy", a)], w=["ysq", ("ssg", a)])
                        rsqrt_ops(ssg[a][:], ssg[a][:], 1.0 / 256, [("ssg", a)], ("ssg", a))
                        P.dve(lambda e, a=a: e.tensor_tensor(out=yy[a][:].rearrange("p (g x) -> p g x", x=256),
                                                             in0=yy[a][:].rearrange("p (g x) -> p g x", x=256),
                                                             in1=ssg[a][:].unsqueeze(2).to_broadcast([128, 2, 256]), op=ALU.mult),
                              r=[("yy", a), ("ssg", a)], w=[("yy", a)])
                        P.pool(lambda e, a=a: e.tensor_tensor(out=yo[a][:], in0=yy[a][:], in1=nw[:], op=ALU.mult),
                               r=[("yy", a), "snw"], w=[("yo", a)])
                        P.dma("pool", lambda e, a=a, rows=rows: e.dma_start(out=ml[rows, 256:768], in_=yo[a][:]), r=[("yo", a)], w=[("ml_s", t)])
            P.barrier()

        def phase_gdn(l):
            with contextlib.ExitStack() as esA:
                qT = sb(esA, "qT", [128, 2, T], BF16)
                kTm = [sb(esA, f"kTm{i}", [128, 2, T], BF16) for i in range(2)]
                P.pool(lambda e: e.memset(kTm[0][64:128, :, :], 0.0), w=["kTm0z"])
                P.pool(lambda e: e.memset(kTm[1][0:64, :, :], 0.0), w=["kTm1z"])
                k_tm = sb(esA, "k_tm", [128, NT, 256], BF16)
                v_tm = sb(esA, "v_tm", [128, NT, 256], BF16)
                O = sb(esA, "O", [128, NT, 256], BF16)
                beta_all = sb(esA, "beta_all", [128, NT, 8], F32)
                g_all = sb(esA, "g_all", [128, NT, 8], F32)
                S = sb(esA, "S", [128, 2, 2, 64], F32)
                Sb = sb(esA, "Sbf", [128, 2, 2, 2, 64], BF16)
                gdtb = bcast_load(esA, "gdtb", gdn_dt_bias[l].rearrange("a b -> (a b)"), 8)
                galg = bcast_load(esA, "galg", gdn_a_log[l].rearrange("a b -> (a b)"), 8)
                gnw = bcast_load(esA, "gnw", gdn_norm_w[l], 64)
                SM = [("small", t) for t in range(NT)]
                P.act(lambda e: e.activation(out=galg[:], in_=galg[:], func=AF.Exp), r=["galg"], w=["galg"])
                P.dve(lambda e: e.tensor_scalar(out=galg[:], in0=galg[:], scalar1=-1.0, scalar2=None, op0=ALU.mult), r=["galg"], w=["galg"])
                P.act(lambda e: e.activation(out=beta_all[:], in_=small_all[:, :, 8:16], func=AF.Sigmoid), r=SM, w=["beta_all"])
                P.dve(lambda e: e.tensor_tensor(out=g_all[:], in0=small_all[:, :, 16:24], in1=gdtb[:].unsqueeze(1).to_broadcast([128, NT, 8]),
                                                op=ALU.add), r=SM + ["gdtb"], w=["g_all"])
                P.act(lambda e: e.activation(out=g_all[:], in_=g_all[:], func=AF.Exp), r=["g_all"], w=["g_all"])
                P.act(lambda e: e.activation(out=g_all[:], in_=g_all[:], func=AF.Ln, bias=onesf[:, 0:1], scale=1.0), r=["g_all", "onesf"], w=["g_all"])
                P.dve(lambda e: e.tensor_tensor(out=g_all[:], in0=g_all[:], in1=galg[:].unsqueeze(1).to_broadcast([128, NT, 8]), op=ALU.mult),
                      r=["g_all", "galg"], w=["g_all"])
                P.pool(lambda e: e.memset(S[:], 0.0), w=[("S", 0), ("S", 1)])
                P.pool(lambda e: e.memset(Sb[:], 0.0), w=[("Sb", 0), ("Sb", 1)])
                with contextlib.ExitStack() as esB:
                    cw = sb(esB, "gcw", [128, 6, 5], F32)
                    dg = sb(esB, "gdg", [128, 30, 128], BF16)
                    hc = [sb(esB, f"ghc{i}", [128, T], BF16) for i in range(2)]
                    cst = [sb(esB, f"gcst{i}", [128, 512], BF16) for i in range(2)]
                    xf = [sb(esB, f"gxf{i}", [128, 512], F32) for i in range(2)]
                    sq = [sb(esB, f"gsq{i}", [128, 512], F32) for i in range(2)]
                    rs = [sb(esB, f"grs{i}", [128, 512], F32) for i in range(2)]
                    xnst = [sb(esB, f"gxnst{i}", [128, 512], BF16) for i in range(2)]
                    P.dma("sp", lambda e: e.dma_start(out=cw[:], in_=gdn_conv_wT[l]), w=["gcw"])
                    for ch in range(6):
                        for jt in range(5):
                            P.dve(lambda e, ch=ch, jt=jt: e.tensor_scalar(out=dg[:, ch * 5 + jt, :], in0=identf[:], scalar1=cw[:, ch, jt:jt + 1],
                                                                          scalar2=None, op0=ALU.mult), r=["identf", "gcw"], w=[("gdg", ch)])
                    cnt = 0
                    for ch in range(6):
                        hs_ = ch % 2
                        P.dma("sp", lambda e, hs_=hs_, ch=ch: e.dma_start(out=hc[hs_][:], in_=hT[6 + ch]),
                              r=[("hT", 6 + ch, gi) for gi in range(9)], w=[("ghc", hs_)])
                        for gidx, (t0, nt) in enumerate(GROUPS):
                            N = nt * 128
                            tok0 = t0 * 128
                            seg_lo, seg_hi = (0, 256) if gidx == 0 else (256, T)
                            bk = cnt % 2
                            cnt += 1
                            for ii, jt in enumerate([2, 0, 1, 3, 4]):
                                o = jt - 2
                                a_ = max(0, seg_lo - tok0 - o)
                                b_ = min(N, seg_hi - tok0 - o)
                                P.pe(lambda e, bk=bk, ch=ch, jt=jt, a_=a_, b_=b_, o=o, tok0=tok0, hs_=hs_, ii=ii: e.matmul(
                                    banks[bk][:, a_:b_], lhsT=dg[:, ch * 5 + jt, :], rhs=hc[hs_][:, tok0 + a_ + o:tok0 + b_ + o],
                                    start=(ii == 0), stop=(ii == 4)), r=[("gdg", ch), ("ghc", hs_)], w=[BK(bk)])
                            if ch < 4:
                                P.act(lambda e, bk=bk, N=N: e.activation(out=xf[bk][:, 0:N], in_=banks[bk][:, 0:N], func=AF.Silu),
                                      r=[BK(bk)], w=[("gxf", bk)])
                                P.pool(lambda e, bk=bk, N=N: e.tensor_tensor(out=sq[bk][:, 0:N], in0=xf[bk][:, 0:N], in1=xf[bk][:, 0:N], op=ALU.mult),
                                       r=[("gxf", bk)], w=[("gsq", bk)])
                                b2 = 2 + bk
                                P.pe(lambda e, bk=bk, b2=b2, N=N: e.matmul(banks[b2][:, 0:N], lhsT=blkf[:], rhs=sq[bk][:, 0:N], start=True, stop=True),
                                     r=["blkf", ("gsq", bk)], w=[BK(b2)])
                                P.dve(lambda e, bk=bk, b2=b2, N=N: e.tensor_scalar(out=rs[bk][:, 0:N], in0=banks[b2][:, 0:N], scalar1=EPS, scalar2=None,
                                                                                  op0=ALU.add), r=[BK(b2)], w=[("grs", bk)])
                                P.act(lambda e, bk=bk, N=N: e.activation(out=rs[bk][:, 0:N], in_=rs[bk][:, 0:N], func=AF.Sqrt), r=[("grs", bk)], w=[("grs", bk)])
                                P.dve(lambda e, bk=bk, N=N: e.reciprocal(out=rs[bk][:, 0:N], in_=rs[bk][:, 0:N]), r=[("grs", bk)], w=[("grs", bk)])
                                dstT = (qT[:, ch, tok0:tok0 + N] if ch < 2 else xnst[bk][:, 0:N])
                                dkey = ("qT", ch, gidx) if ch < 2 else ("gxnst", bk)
                                sc_ = 0.125 if ch < 2 else 1.0
                                P.dve(lambda e, bk=bk, N=N, dstT=dstT, sc_=sc_: e.scalar_tensor_tensor(
                                    out=dstT, in0=xf[bk][:, 0:N], scalar=sc_, in1=rs[bk][:, 0:N], op0=ALU.mult, op1=ALU.mult),
                                    r=[("gxf", bk), ("grs", bk)], w=[dkey])
                                if ch >= 2:
                                    for g_ in range(2):
                                        pg = slice(64 * g_, 64 * g_ + 64)
                                        P.pool(lambda e, g_=g_, pg=pg, bk=bk, ch=ch, tok0=tok0, N=N: e.tensor_copy(
                                            out=kTm[g_][pg, ch - 2, tok0:tok0 + N], in_=xnst[bk][pg, 0:N]),
                                            r=[dkey, f"kTm{g_}z"], w=[("kTm", g_, ch - 2, gidx)])
                                    for ti in range(nt):
                                        t = t0 + ti
                                        tb_ = 6 + (t % 2)
                                        P.pe(lambda e, tb_=tb_, ti=ti, bk=bk: e.transpose(out=tbanks[tb_][:, 0:128], in_=xnst[bk][:, ti * 128:(ti + 1) * 128],
                                                                                         identity=identb[:]), r=[dkey, "identb"], w=[BK(tb_)])
                                        P.dve(lambda e, tb_=tb_, t=t, ch=ch: e.tensor_copy(out=k_tm[:, t, (ch - 2) * 128:(ch - 1) * 128], in_=tbanks[tb_][:, 0:128]),
                                              r=[BK(tb_)], w=[("k_tm", t, ch - 2)])
                            else:
                                P.act(lambda e, bk=bk, N=N: e.activation(out=cst[bk][:, 0:N], in_=banks[bk][:, 0:N], func=AF.Silu),
                                      r=[BK(bk)], w=[("gcst", bk)])
                                for ti in range(nt):
                                    t = t0 + ti
                                    tb_ = 6 + (t % 2)
                                    P.pe(lambda e, tb_=tb_, bk=bk, ti=ti: e.transpose(out=tbanks[tb_][:, 0:128], in_=cst[bk][:, ti * 128:(ti + 1) * 128],
                                                                                     identity=identb[:]), r=[("gcst", bk), "identb"], w=[BK(tb_)])
                                    P.dve(lambda e, tb_=tb_, t=t, ch=ch: e.tensor_copy(out=v_tm[:, t, (ch - 4) * 128:(ch - 3) * 128], in_=tbanks[tb_][:, 0:128]),
                                          r=[BK(tb_)], w=[("v_tm", t, ch - 4)])
                P.barrier()
                if stop_after == "gconv":
                    return
                with contextlib.ExitStack() as esC:
                    def mk(name, shape, dt):
                        return [sb(esC, f"{name}{i}", shape, dt) for i in range(2)]
                    rhs1 = mk("g_rhs1", [128, 4, 128], F32)
                    rhs2 = mk("g_rhs2", [128, 4, 128], F32)
                    gmask = mk("g_gmask", [128, 4, 2], F32)
                    decp = mk("g_decp", [128, 4, 128], F32)
                    esm = mk("g_esm", [128, 16], F32)
                    tmpA = rhs1
                    nbo = rhs2
                    nb4 = mk("g_nb4", [128, 4], F32)
                    beg = mk("g_beg", [128, 4], F32)
                    Nm = [mk("g_Nm_a", [128, 4, 128], BF16)]
                    Ym = [mk("g_Ym_a", [128, 4, 128], BF16)]
                    Wm = [mk("g_Wm_a", [128, 4, 128], BF16), mk("g_Wm_b", [128, 4, 128], BF16)]
                    Tm = [mk("g_Tm_a", [128, 4, 128], BF16), mk("g_Tm_b", [128, 4, 128], BF16)]
                    NMc = mk("g_NMc", [128, 4, 128], BF16)
                    YMc = mk("g_YMc", [128, 4, 128], BF16)
                    P1s = mk("g_P1s", [128, 4, 128], BF16)
                    P2s = mk("g_P2s", [128, 4, 128], BF16)
                    M4 = [sb(esC, f"g_M4_{j}", [128, 4, 128], BF16) for j in range(6)]
                    I4 = sb(esC, "g_I4", [128, 4, 128], BF16)
                    blk_a = sb(esC, "g_blka", [128, 128], F32)
                    blk_b = sb(esC, "g_blkb", [128, 128], F32)
                    mtmp = sb(esC, "g_mtmp", [128, 128], F32)

                    def mk_blk(t_, n):
                        v_ = t_[:].rearrange("p (a b) -> p a b", b=n)
                        P.pool(lambda e: e.memset(t_[:], 1.0), w=[id(t_)])
                        P.pool(lambda e: e.affine_select(out=v_, in_=v_, compare_op=ALU.is_ge, fill=0.0, base=0,
                                                         pattern=[[-n, 128 // n], [0, n]], channel_multiplier=1), r=[id(t_)], w=[id(t_)])
                        P.pool(lambda e: e.affine_select(out=v_, in_=v_, compare_op=ALU.is_ge, fill=0.0, base=n - 1,
                                                         pattern=[[n, 128 // n], [0, n]], channel_multiplier=-1), r=[id(t_)], w=[id(t_)])
                    prev, cur = blk_a, blk_b
                    mk_blk(prev, 1)
                    for j in range(6):
                        mk_blk(cur, 2 << j)
                        P.pool(lambda e, prev=prev, cur=cur: e.tensor_tensor(out=mtmp[:], in0=cur[:], in1=prev[:], op=ALU.subtract),
                               r=[id(prev), id(cur)], w=["g_mtmp"])
                        for h in range(4):
                            P.pool(lambda e, j=j, h=h: e.tensor_copy(out=M4[j][:, h, :], in_=mtmp[:]), r=["g_mtmp"], w=["M4"])
                        prev, cur = cur, prev
                    for h in range(4):
                        P.pool(lambda e, h=h: e.tensor_copy(out=I4[:, h, :], in_=identb[:]), r=["identb"], w=["I4"])
                    QKm = mk("g_QKm", [128, 4, 128], BF16)
                    QKmT = mk("g_QKmT", [128, 4, 128], BF16)
                    kbg = mk("g_kbg", [128, 4, 64], BF16)
                    kend = mk("g_kend", [128, 4, 64], BF16)
                    vb = mk("g_vb", [128, 4, 64], BF16)
                    u_sb = mk("g_u", [128, 4, 64], F32)
                    wT_sb = mk("g_wT", [128, 2, 128], BF16)
                    vnewc = [mk("g_vnew_a", [128, 4, 64], BF16), mk("g_vnew_b", [128, 4, 64], BF16)]
                    for ci_ in range(2):
                        for d_ in range(2):
                            P.pool(lambda e, ci_=ci_, d_=d_: e.memset(vnewc[ci_][d_][:], 0.0), w=[("vnewc", ci_, d_)])
                    o1 = mk("g_o1", [128, 4, 64], F32)
                    QT_ALL = [("qT", c, gi) for c in range(2) for gi in range(9)]
                    KT_ALL = [("kTm", g_, c, gi) for g_ in range(2) for c in range(2) for gi in range(9)]
                    seen = set()
                    v4 = lambda bk: bk[:].rearrange("p (h s) -> p h s", s=128)

                    def gproc(d, t):
                        if True:
                            b0, b1, b2_ = banks[3 * d], banks[3 * d + 1], banks[3 * d + 2]
                            b3, b4, b5 = b0, b1, b2_
                            BKd = lambda i: BK(3 * d + (i % 3)) if i < 6 else BK(6 + d)
                            tsl = slice(t * 128, (t + 1) * 128)
                            UB_, nUB_, SB_, NMB_ = (UBf, nUBf, SBf, NMBf) if d == 0 else (UBb, nUBb, SBb, NMBb)
                            UBk, nUBk, SBk, NMBk = ("UBf", "nUBf", "SBf", NMBf_keys) if d == 0 else ("UBb", "nUBb", "SBb", NMBb_keys)
                            g4 = g_all[:, t, 4 * d:4 * d + 4]
                            bt4 = beta_all[:, t, 4 * d:4 * d + 4]
                            KD = lambda n: (n, d)
                            P.dve(lambda e, d=d, nUB_=nUB_, g4=g4: e.tensor_tensor(
                                out=rhs1[d][:], in0=nUB_[:].unsqueeze(1).to_broadcast([128, 4, 128]),
                                in1=g4.unsqueeze(2).to_broadcast([128, 4, 128]), op=ALU.mult), r=[nUBk, "g_all"], w=[KD("rhs1")])
                            P.dve(lambda e, d=d, g4=g4: e.tensor_copy(out=rhs2[d][:], in_=g4.unsqueeze(2).to_broadcast([128, 4, 128])),
                                  r=["g_all"], w=[KD("rhs2")])
                            P.dve(lambda e, d=d, g4=g4: e.tensor_tensor(
                                out=gmask[d][:], in0=g4.unsqueeze(2).to_broadcast([128, 4, 2]),
                                in1=chunkind[:].unsqueeze(1).to_broadcast([128, 4, 2]), op=ALU.mult), r=["g_all", "chunkind"], w=[KD("gmask")])
                            P.pe(lambda e, d=d: e.matmul(b0[:], lhsT=onesf[:], rhs=rhs1[d][:].rearrange("p h s -> p (h s)"), start=True, stop=False),
                                 r=["onesf", KD("rhs1")], w=[BKd(0)])
                            P.pe(lambda e, d=d, UB_=UB_: e.matmul(b0[:], lhsT=UB_[:], rhs=rhs2[d][:].rearrange("p h s -> p (h s)"), start=False, stop=False),
                                 r=[UBk, KD("rhs2")], w=[BKd(0)])
                            P.pe(lambda e, NMB_=NMB_: e.matmul(b0[:], lhsT=identf[:], rhs=NMB_[:].rearrange("p h s -> p (h s)"), start=False, stop=True),
                                 r=["identf"] + NMBk, w=[BKd(0)])
                            for h in range(4):
                                c, hh = h // 2, h % 2
                                hp = slice(64 * hh, 64 * hh + 64)
                                P.pe(lambda e, h=h, c=c, hh=hh, tsl=tsl: e.matmul(b1[:, h * 128:(h + 1) * 128], lhsT=kTm[hh][:, c, tsl], rhs=kTm[hh][:, c, tsl],
                                                                                 start=True, stop=True), r=KT_ALL, w=[BKd(1)])
                            for h in range(4):
                                c, hh = h // 2, h % 2
                                hp = slice(64 * hh, 64 * hh + 64)
                                P.pe(lambda e, h=h, c=c, hh=hh, tsl=tsl: e.matmul(b2_[:, h * 128:(h + 1) * 128], lhsT=qT[:, c, tsl], rhs=kTm[hh][:, c, tsl],
                                                                                 start=True, stop=True), r=KT_ALL + QT_ALL, w=[BKd(2)])
                            P.act(lambda e, d=d: e.activation(out=decp[d][:].rearrange("p h s -> p (h s)"), in_=b0[:], func=AF.Exp), r=[BKd(0)], w=[KD("decp")])
                            P.pe(lambda e, UB_=UB_, g4=g4: e.matmul(b3[:, 0:4], lhsT=UB_[:], rhs=g4, start=True, stop=True), r=[UBk, "g_all"], w=[BKd(3)])
                            P.pe(lambda e, SB_=SB_, g4=g4: e.matmul(b3[:, 4:8], lhsT=SB_[:], rhs=g4, start=True, stop=True), r=[SBk, "g_all"], w=[BKd(3)])
                            P.pe(lambda e, d=d: e.matmul(b3[:, 8:16], lhsT=onesf[:], rhs=gmask[d][:].rearrange("p h c -> p (h c)"), start=True, stop=True),
                                 r=["onesf", KD("gmask")], w=[BKd(3)])
                            P.act(lambda e, d=d: e.activation(out=esm[d][:], in_=b3[:, 0:16], func=AF.Exp), r=[BKd(3)], w=[KD("esm")])
                            P.dve(lambda e, d=d: e.tensor_tensor(out=tmpA[d][:], in0=v4(b1), in1=decp[d][:], op=ALU.mult), r=[BKd(1), KD("decp")], w=[KD("rhs1")])
                            P.dve(lambda e, d=d, bt4=bt4: e.tensor_scalar(out=nb4[d][:], in0=bt4, scalar1=-1.0, scalar2=None, op0=ALU.mult),
                                  r=["beta_all"], w=[KD("nb4")])
                            P.dve(lambda e, d=d: e.tensor_tensor(out=nbo[d][:], in0=offd[:].unsqueeze(1).to_broadcast([128, 4, 128]),
                                                                 in1=nb4[d][:].unsqueeze(2).to_broadcast([128, 4, 128]), op=ALU.mult),
                                  r=["offd", KD("nb4")], w=[KD("rhs2")])
                            P.dve(lambda e, d=d: e.tensor_tensor(out=Nm[0][d][:], in0=tmpA[d][:], in1=nbo[d][:], op=ALU.mult),
                                  r=[KD("rhs1"), KD("rhs2")], w=[("Nm", 0, d)])
                            P.dve(lambda e, d=d: e.tensor_tensor(out=QKm[d][:], in0=v4(b2_), in1=decp[d][:], op=ALU.mult), r=[BKd(2), KD("decp")], w=[KD("QKm")])
                            P.dve(lambda e, d=d, bt4=bt4: e.tensor_tensor(out=beg[d][:], in0=bt4, in1=esm[d][:, 0:4], op=ALU.mult),
                                  r=["beta_all", KD("esm")], w=[KD("beg")])
                            ktv = k_tm[:, t, :].rearrange("p (h x) -> p h x", x=64)
                            vtv = v_tm[:, t, :].rearrange("p (h x) -> p h x", x=64)
                            KTM = [("k_tm", t, 0), ("k_tm", t, 1)]
                            VTM = [("v_tm", t, 0), ("v_tm", t, 1)]
                            P.dve(lambda e, d=d, ktv=ktv: e.tensor_tensor(out=kbg[d][:], in0=ktv, in1=beg[d][:].unsqueeze(2).to_broadcast([128, 4, 64]),
                                                                          op=ALU.mult), r=KTM + [KD("beg")], w=[KD("kbg")])
                            P.dve(lambda e, d=d, ktv=ktv: e.tensor_tensor(out=kend[d][:], in0=ktv, in1=esm[d][:, 4:8].unsqueeze(2).to_broadcast([128, 4, 64]),
                                                                          op=ALU.mult), r=KTM + [KD("esm")], w=[KD("kend")])
                            P.dve(lambda e, d=d, vtv=vtv, bt4=bt4: e.tensor_tensor(out=vb[d][:], in0=vtv, in1=bt4.unsqueeze(2).to_broadcast([128, 4, 64]),
                                                                                  op=ALU.mult), r=VTM + ["beta_all"], w=[KD("vb")])
                            tv6 = tbanks[6 + d][:, 0:512].rearrange("p (h s) -> p h s", s=128)
                            tv7 = tbanks[6 + d][:, 512:1024].rearrange("p (h s) -> p h s", s=128)
                            for h in range(4):
                                P.pe(lambda e, d=d, h=h, tv6=tv6: e.transpose(out=tv6[:, h, :], in_=Nm[0][d][:, h, :], identity=identb[:]),
                                     r=[("Nm", 0, d), "identb"], w=[BKd(6)])
                            for h in range(4):
                                P.pe(lambda e, d=d, h=h, tv7=tv7: e.transpose(out=tv7[:, h, :], in_=QKm[d][:, h, :], identity=identb[:]),
                                     r=[KD("QKm"), "identb"], w=[BKd(7)])
                            P.act(lambda e, d=d, tv6=tv6: e.activation(out=Ym[0][d][:], in_=tv6, func=AF.Copy), r=[BKd(6)], w=[("Ym", 0, d)])
                            P.act(lambda e, d=d, tv7=tv7: e.activation(out=QKmT[d][:], in_=tv7, func=AF.Copy), r=[BKd(7)], w=[KD("QKmT")])
                            yield
                            P.dve(lambda e, d=d: e.tensor_tensor(out=NMc[d][:], in0=Nm[0][d][:], in1=M4[0][:], op=ALU.mult),
                                  r=[("Nm", 0, d), "M4"], w=[KD("NMc")])
                            P.pool(lambda e, d=d: e.tensor_tensor(out=YMc[d][:], in0=Ym[0][d][:], in1=M4[0][:], op=ALU.mult),
                                   r=[("Ym", 0, d), "M4"], w=[KD("YMc")])
                            P.dve(lambda e, d=d: e.tensor_tensor(out=Wm[1][d][:], in0=YMc[d][:], in1=I4[:], op=ALU.add), r=[KD("YMc"), "I4"], w=[("Wm", 1, d)])
                            P.pool(lambda e, d=d: e.tensor_tensor(out=Tm[1][d][:], in0=NMc[d][:], in1=I4[:], op=ALU.add), r=[KD("NMc"), "I4"], w=[("Tm", 1, d)])
                            for j in range(2, 7):
                                pp, pn = (j - 1) % 2, j % 2
                                lastj = (j == 6)
                                P.dve(lambda e, d=d, j=j: e.tensor_tensor(out=NMc[d][:], in0=Nm[0][d][:], in1=M4[j - 1][:], op=ALU.mult),
                                      r=[("Nm", 0, d), "M4"], w=[KD("NMc")])
                                if not lastj:
                                    P.pool(lambda e, d=d, j=j: e.tensor_tensor(out=YMc[d][:], in0=Ym[0][d][:], in1=M4[j - 1][:], op=ALU.mult),
                                           r=[("Ym", 0, d), "M4"], w=[KD("YMc")])
                                for h in range(4):
                                    P.pe(lambda e, d=d, h=h, pp=pp: e.matmul(b0[:, h * 128:(h + 1) * 128], lhsT=NMc[d][:, h, :], rhs=Wm[pp][d][:, h, :],
                                                                            start=True, stop=True), r=[KD("NMc"), ("Wm", pp, d)], w=[BKd(0)])
                                P.act(lambda e, d=d: e.activation(out=P1s[d][:], in_=v4(b0), func=AF.Copy), r=[BKd(0)], w=[KD("P1s")])
                                if not lastj:
                                    for h in range(4):
                                        P.pe(lambda e, d=d, h=h, pp=pp: e.matmul(b1[:, h * 128:(h + 1) * 128], lhsT=YMc[d][:, h, :], rhs=Tm[pp][d][:, h, :],
                                                                                start=True, stop=True), r=[KD("YMc"), ("Tm", pp, d)], w=[BKd(1)])
                                    P.dve(lambda e, d=d: e.tensor_copy(out=P2s[d][:], in_=v4(b1)), r=[BKd(1)], w=[KD("P2s")])
                                for h in range(4):
                                    P.pe(lambda e, d=d, h=h, pp=pp: e.matmul(b2_[:, h * 128:(h + 1) * 128], lhsT=identb[:], rhs=Wm[pp][d][:, h, :],
                                                                            start=True, stop=False), r=["identb", ("Wm", pp, d)], w=[BKd(2)])
                                    P.pe(lambda e, d=d, h=h, pp=pp: e.matmul(b2_[:, h * 128:(h + 1) * 128], lhsT=Tm[pp][d][:, h, :], rhs=P1s[d][:, h, :],
                                                                            start=False, stop=True), r=[("Tm", pp, d), KD("P1s")], w=[BKd(2)])
                                P.act(lambda e, d=d, pn=pn: e.activation(out=Wm[pn][d][:], in_=v4(b2_), func=AF.Copy), r=[BKd(2)], w=[("Wm", pn, d)])
                                if not lastj:
                                    for h in range(4):
                                        P.pe(lambda e, d=d, h=h, pp=pp: e.matmul(b0[:, h * 128:(h + 1) * 128], lhsT=identb[:], rhs=Tm[pp][d][:, h, :],
                                                                                start=True, stop=False), r=["identb", ("Tm", pp, d)], w=[BKd(0)])
                                        P.pe(lambda e, d=d, h=h, pp=pp: e.matmul(b0[:, h * 128:(h + 1) * 128], lhsT=Wm[pp][d][:, h, :], rhs=P2s[d][:, h, :],
                                                                                start=False, stop=True), r=[("Wm", pp, d), KD("P2s")], w=[BKd(0)])
                                    P.dve(lambda e, d=d, pn=pn: e.tensor_copy(out=Tm[pn][d][:], in_=v4(b0)), r=[BKd(0)], w=[("Tm", pn, d)])
                                yield
                            TT = Wm[0][d]
                            TTk = ("Wm", 0, d)
                            yield
                            for h in range(4):
                                P.pe(lambda e, d=d, h=h, TT=TT: e.matmul(b3[:, 256 + h * 64:256 + (h + 1) * 64], lhsT=TT[:, h, :], rhs=vb[d][:, h, :],
                                                                        start=True, stop=True), r=[TTk, KD("vb")], w=[BKd(3)])
                            for h in range(4):
                                c = h // 2
                                P.pe(lambda e, d=d, h=h, c=c, TT=TT: e.matmul(
                                    b4[:, h * 128:(h + 1) * 128], lhsT=kbg[d][:, 2 * c:2 * c + 2, :].rearrange("p a x -> p (a x)"), rhs=TT[:, h, :],
                                    start=True, stop=True), r=[TTk, KD("kbg")], w=[BKd(4)])
                            P.act(lambda e, d=d: e.activation(out=u_sb[d][:].rearrange("p h x -> p (h x)"), in_=b3[:, 256:512], func=AF.Copy),
                                  r=[BKd(3)], w=[KD("u")])
                            for hh in range(2):
                                hp = slice(64 * hh, 64 * hh + 64)
                                P.dve(lambda e, d=d, hh=hh, hp=hp: e.tensor_copy(out=wT_sb[d][hp, :, :], in_=v4(b4)[hp, hh::2, :]), r=[BKd(4)], w=[KD("wT")])
                            yield
                            for ci in ((0, 1) if d == 0 else (1, 0)):
                                rows = slice(64 * ci, 64 * ci + 64)
                                for h in range(4):
                                    c, hh = h // 2, h % 2
                                    hp = slice(64 * hh, 64 * hh + 64)
                                    P.pe(lambda e, d=d, h=h, c=c, hh=hh: e.matmul(b5[:, h * 64:(h + 1) * 64], lhsT=wT_sb[d][:, c, :], rhs=Sb[:, hh, d, c, :],
                                                                                 start=True, stop=True), r=[KD("wT"), ("Sb", d)], w=[BKd(5)])
                                for h in range(4):
                                    c, hh = h // 2, h % 2
                                    hp = slice(64 * hh, 64 * hh + 64)
                                    P.pe(lambda e, d=d, h=h, c=c, hh=hh, tsl=tsl: e.matmul(b5[:, 256 + h * 64:256 + (h + 1) * 64], lhsT=qT[:, c, tsl],
                                                                                          rhs=Sb[:, hh, d, c, :], start=True, stop=True),
                                         r=QT_ALL + [("Sb", d)], w=[BKd(5)])
                                P.dve(lambda e, d=d, rows=rows, ci=ci: e.tensor_tensor(
                                    out=vnewc[ci][d][rows, :, :], in0=u_sb[d][rows, :, :], in1=b5[rows, 0:256].rearrange("p (h x) -> p h x", x=64),
                                    op=ALU.subtract), r=[KD("u"), BKd(5)], w=[("vnewc", ci, d)])
                                P.dve(lambda e, d=d, rows=rows: e.tensor_tensor(
                                    out=o1[d][rows, :, :], in0=b5[rows, 256:512].rearrange("p (h x) -> p h x", x=64),
                                    in1=esm[d][rows, 0:4].unsqueeze(2).to_broadcast([64, 4, 64]), op=ALU.mult),
                                    r=[KD("esm"), BKd(5)], w=[KD("o1")])
                                for h in range(4):
                                    c = h // 2
                                    P.pe(lambda e, d=d, h=h, c=c, ci=ci: e.matmul(
                                        b4[:, h * 64:(h + 1) * 64], lhsT=kend[d][:, 2 * c:2 * c + 2, :].rearrange("p a x -> p (a x)"),
                                        rhs=vnewc[ci][d][:, h, :], start=True, stop=True), r=[KD("kend"), ("vnewc", ci, d)], w=[BKd(4)])
                                for hh in range(2):
                                    hp = slice(64 * hh, 64 * hh + 64)
                                    gv = esm[d][hp, 8:16].rearrange("p (h c) -> p h c", c=2)[:, hh::2, ci:ci + 1]
                                    P.dve(lambda e, d=d, hp=hp, gv=gv: e.tensor_tensor(out=S[hp, d, :, :], in0=S[hp, d, :, :],
                                                                                      in1=gv.to_broadcast([64, 2, 64]), op=ALU.mult),
                                          r=[("S", d), KD("esm")], w=[("S", d)])
                                    P.dve(lambda e, d=d, hp=hp, hh=hh: e.tensor_tensor(
                                        out=S[hp, d, :, :], in0=S[hp, d, :, :],
                                        in1=b4[hp, 0:256].rearrange("p (h x) -> p h x", x=64)[:, hh::2, :], op=ALU.add),
                                        r=[("S", d), BKd(4)], w=[("S", d)])
                                for hh in range(2):
                                    hp = slice(64 * hh, 64 * hh + 64)
                                    P.pool(lambda e, d=d, hh=hh, hp=hp: e.tensor_copy(out=Sb[hp, hh, d, :, :], in_=S[hp, d, :, :]), r=[("S", d)], w=[("Sb", d)])
                                yield
                            yield
                            for h in range(4):
                                for ci_ in range(2):
                                    P.pe(lambda e, d=d, h=h, ci_=ci_: e.matmul(b3[:, h * 64:(h + 1) * 64], lhsT=QKmT[d][:, h, :], rhs=vnewc[ci_][d][:, h, :],
                                                                              start=(ci_ == 0), stop=(ci_ == 1)),
                                         r=[KD("QKmT"), ("vnewc", 0, d), ("vnewc", 1, d)], w=[BKd(3)])
                            Ov = O[:, t, :]
                            if t not in seen:
                                seen.add(t)
                                P.dve(lambda e, d=d, Ov=Ov: e.tensor_tensor(out=Ov, in0=b3[:, 0:256], in1=o1[d][:].rearrange("p h x -> p (h x)"), op=ALU.add),
                                      r=[BKd(3), KD("o1")], w=[("O", t)])
                            else:
                                P.dve(lambda e, d=d: e.tensor_tensor(out=o1[d][:].rearrange("p h x -> p (h x)"), in0=b3[:, 0:256],
                                                                     in1=o1[d][:].rearrange("p h x -> p (h x)"), op=ALU.add),
                                      r=[BKd(3), KD("o1")], w=[KD("o1")])
                                P.dve(lambda e, d=d, Ov=Ov: e.tensor_tensor(out=Ov, in0=o1[d][:].rearrange("p h x -> p (h x)"), in1=Ov, op=ALU.add),
                                      r=[KD("o1"), ("O", t)], w=[("O", t)])

                    def run_interleaved(fn):
                        for step in range(NT):
                            gens = [fn(d_, ORDER[d_][step]) for d_ in range(2)]
                            while gens:
                                for g_ in list(gens):
                                    try:
                                        next(g_)
                                    except StopIteration:
                                        gens.remove(g_)
                    run_interleaved(gproc)
                    gt_ = mk("g_gt", [128, 256], BF16)
                    of = mk("g_of", [128, 4, 64], F32)
                    osq = sb(esC, "g_osq", [128, 4, 64], F32)
                    oss = mk("g_oss", [128, 4], F32)
                    oo = mk("g_oo", [128, 256], BF16)
                    for t in range(NT):
                        a = t % 2
                        rows = slice(t * 128, (t + 1) * 128)
                        P.dma("sp", lambda e, a=a, rows=rows: e.dma_start(out=gt_[a][:], in_=gs[rows, :]), r=[("gs", t)], w=[("g_gt", a)])
                        P.pool(lambda e, a=a, t=t: e.tensor_tensor(out=osq[:].rearrange("p h x -> p (h x)"), in0=O[:, t, :], in1=O[:, t, :], op=ALU.mult),
                               r=[("O", t)], w=["g_osq"])
                        P.dve(lambda e, a=a: e.tensor_reduce(out=oss[a][:], in_=osq[:], axis=AX.X, op=ALU.add), r=["g_osq"], w=[("g_oss", a)])
                        rsqrt_ops(oss[a][:], oss[a][:], 1.0 / 64, [("g_oss", a)], ("g_oss", a))
                        P.dve(lambda e, a=a, t=t: e.tensor_tensor(out=of[a][:], in0=O[:, t, :].rearrange("p (h x) -> p h x", x=64),
                                                                  in1=oss[a][:].unsqueeze(2).to_broadcast([128, 4, 64]), op=ALU.mult),
                              r=[("O", t), ("g_oss", a)], w=[("g_of", a)])
                        P.dve(lambda e, a=a: e.tensor_tensor(out=of[a][:], in0=of[a][:], in1=gnw[:].unsqueeze(1).to_broadcast([128, 4, 64]), op=ALU.mult),
                              r=[("g_of", a), "gnw"], w=[("g_of", a)])
                        P.pool(lambda e, a=a: e.tensor_tensor(out=oo[a][:], in0=of[a][:].rearrange("p h x -> p (h x)"), in1=gt_[a][:], op=ALU.mult),
                               r=[("g_of", a), ("g_gt", a)], w=[("g_oo", a)])
                        P.dma("pool", lambda e, a=a, rows=rows: e.dma_start(out=ml[rows, 768:1024], in_=oo[a][:]), r=[("g_oo", a)], w=[("ml_g", t)])
            P.barrier()

        HALVES = [[(0, 2)] + [(2 + 4 * i, 4) for i in range(4)], [(18 + 4 * i, 4) for i in range(4)]]

        def phase_out_moe(l, stream_in, stream_out, last):
            with contextlib.ExitStack() as esA:
                G = sb(esA, "G", [128, NT, 32], F32)
                rb = bcast_load(esA, "rb", router_b[l], 36)
                def do_half(hi, groups):
                    tiles = [t0 + i for (t0, nt) in groups for i in range(nt)]
                    tb = tiles[0]
                    nth = len(tiles)
                    with contextlib.ExitStack() as esH:
                        flT = sb(esH, "flT", [128, 8, nth * 128], BF16)
                        with contextlib.ExitStack() as esB:
                            wob = sb(esB, "wob", [128, 8, D], BF16)
                            wst = [sb(esB, f"wost{i}", [128, D], F32) for i in range(2)]
                            rw = sb(esB, "rw", [128, 8, 36], F32)
                            mlt = [sb(esB, f"mlt{i}", [128, D], BF16) for i in range(2)]
                            mlT = [sb(esB, f"mlT{i}", [128, 8, 128], BF16) for i in range(2)]
                            xt = [sb(esB, f"xto{i}", [128, D], F32) for i in range(2)]
                            x1 = [sb(esB, f"x1{i}", [128, D], F32) for i in range(2)]
                            junk = sb(esB, "junko", [128, D], BF16)
                            ss = [sb(esB, f"sso{i}", [128, 1], F32) for i in range(2)]
                            xn = [sb(esB, f"xno{i}", [128, D], F32) for i in range(2)]
                            fl32 = [sb(esB, f"fl32{i}", [128, 8, 128], F32) for i in range(2)]
                            rl = sb(esB, "rl", [128, 36], F32)
                            rt = sb(esB, "rt", [128, 64], F32)
                            tmp48 = sb(esB, "tmp48", [128, 4, 8], F32)
                            for k in range(8):
                                s_ = k % 2
                                P.dma("sp", lambda e, s_=s_, k=k: e.dma_start(out=wst[s_][:], in_=w_out[l][k * 128:(k + 1) * 128, :]),
                                      w=[("wost", s_)])
                                P.pool(lambda e, s_=s_, k=k: e.tensor_copy(out=wob[:, k, :], in_=wst[s_][:]),
                                       r=[("wost", s_)], w=[("wob", k)])
                            WOB = [("wob", k) for k in range(8)]
                            P.dma("sp", lambda e: e.dma_start(out=rw[:], in_=router_w[l].rearrange("(k p) n -> p k n", p=128)), w=["rw"])
                            for t in tiles:
                                j = 1 if t < NCTX_T else 0
                                a = t % 2
                                tl = t - tb
                                rows = slice(t * 128, (t + 1) * 128)
                                P.dma("sp", lambda e, a=a, rows=rows: e.dma_start(out=mlt[a][:], in_=ml[rows, :]),
                                      r=[("ml_a", t), ("ml_s", t), ("ml_g", t)], w=[("mlt", a)])
                                P.dma("sp", lambda e, a=a, rows=rows: e.dma_start(out=xt[a][:], in_=stream_in[rows, :]), w=[("xto", a)])
                                bk = 6 + a
                                ptv = tbanks[bk][:].rearrange("p (k n) -> p k n", n=128)
                                for k in range(8):
                                    P.pe(lambda e, a=a, k=k, ptv=ptv: e.transpose(out=ptv[:, k, :], in_=mlt[a][:, k * 128:(k + 1) * 128],
                                                                              identity=identb[:]),
                                         r=[("mlt", a), "identb"], w=[BK(bk)])
                                P.act(lambda e, a=a, ptv=ptv: e.activation(out=mlT[a][:], in_=ptv, func=AF.Copy), r=[BK(bk)], w=[("mlT", a)])
                                for c2 in range(2):
                                    for k in range(8):
                                        P.pe(lambda e, a=a, k=k, c2=c2: e.matmul(
                                            banks[c2][:], lhsT=mlT[a][:, k, :], rhs=wob[:, k, c2 * 512:(c2 + 1) * 512],
                                            start=(k == 0), stop=(k == 7)), r=[("mlT", a)] + WOB, w=[BK(c2)])
                                    cs = slice(c2 * 512, (c2 + 1) * 512)
                                    P.dve(lambda e, a=a, c2=c2, cs=cs, j=j: e.tensor_tensor(
                                        out=x1[a][:, cs], in0=banks[c2][:], in1=g1row[:, j, c2 * 4:(c2 + 1) * 4, :].rearrange("p k n -> p (k n)"),
                                        op=ALU.mult), r=[BK(c2), "g1row"], w=[("x1", a, c2)])
                                    P.pool(lambda e, a=a, cs=cs: e.tensor_tensor(out=x1[a][:, cs], in0=x1[a][:, cs], in1=xt[a][:, cs], op=ALU.add),
                                           r=[("x1", a, c2), ("xto", a)], w=[("x1", a, c2)])
                                X1K = [("x1", a, 0), ("x1", a, 1)]
                                P.dma("pool", lambda e, a=a, rows=rows: e.dma_start(out=xs1[rows, :], in_=x1[a][:]), r=X1K, w=[("xs1", t)])
                                P.act(lambda e, a=a: e.activation(out=junk[:], in_=x1[a][:], func=AF.Square, accum_out=ss[a][:]),
                                      r=X1K, w=["junko", ("sso", a)])
                                rsqrt_ops(ss[a][:], ss[a][:], 1.0 / D, [("sso", a)], ("sso", a))
                                P.dve(lambda e, a=a: e.tensor_scalar(out=xn[a][:], in0=x1[a][:], scalar1=ss[a][:, 0:1], scalar2=None,
                                                                     op0=ALU.mult), r=X1K + [("sso", a)], w=[("xno", a)])
                                for hf in range(2):
                                    bkf = 2 + hf
                                    pf = banks[bkf][:].rearrange("p (k n) -> p k n", n=128)
                                    for kk in range(4):
                                        k = hf * 4 + kk
                                        P.pe(lambda e, a=a, k=k, kk=kk, pf=pf: e.transpose(out=pf[:, kk, :], in_=xn[a][:, k * 128:(k + 1) * 128],
                                                                                       identity=identf[:]),
                                             r=[("xno", a), "identf"], w=[BK(bkf)])
                                    for kk in range(4):
                                        k = hf * 4 + kk
                                        P.act(lambda e, a=a, k=k, kk=kk, pf=pf, j=j: e.activation(
                                            out=fl32[a][:, k, :], in_=pf[:, kk, :], func=AF.Identity,
                                            scale=s2[:, k, j:j + 1], bias=modT[:, 24 + k, j:j + 1]),
                                            r=[BK(bkf), "s2k", "modT"], w=[("fl32", a, k)])
                                FLK = [("fl32", a, k) for k in range(8)]
                                P.pool(lambda e, a=a, tl=tl: e.tensor_copy(out=flT[:, :, tl * 128:(tl + 1) * 128], in_=fl32[a][:]),
                                       r=FLK, w=[("flT", tl)])
                                for k in range(8):
                                    P.pe(lambda e, a=a, k=k: e.matmul(banks[4][:, 0:36], lhsT=fl32[a][:, k, :], rhs=rw[:, k, :],
                                                                      start=(k == 0), stop=(k == 7)), r=FLK + ["rw"], w=[BK(4)])
                                RT = "rt"
                                P.dve(lambda e: e.tensor_tensor(out=rl[:], in0=banks[4][:, 0:36], in1=rb[:], op=ALU.add), r=[BK(4), "rb"], w=["rl"])
                                gmax, ngmax, gsum, m1, m2, dd, e2, g1_, g2_ = [rt[:, i:i + 1] for i in range(9)]
                                ohg, ge = rt[:, 12:16], rt[:, 16:20]
                                sel, oh1, sel2, oh2, gate8 = [rt[:, 24 + 8 * i:32 + 8 * i] for i in range(5)]
                                P.dve(lambda e: e.tensor_reduce(out=gmax, in_=rl[:, 0:4], axis=AX.X, op=ALU.max), r=["rl"], w=[RT])
                                P.dve(lambda e: e.tensor_scalar(out=ohg, in0=rl[:, 0:4], scalar1=gmax, scalar2=None, op0=ALU.is_equal), r=["rl", RT], w=[RT])
                                P.dve(lambda e: e.tensor_scalar(out=ngmax, in0=gmax, scalar1=-1.0, scalar2=None, op0=ALU.mult), r=[RT], w=[RT])
                                P.act(lambda e: e.activation(out=ge, in_=rl[:, 0:4], func=AF.Exp, bias=ngmax, scale=1.0, accum_out=gsum), r=["rl", RT], w=[RT])
                                P.dve(lambda e: e.reciprocal(out=gsum, in_=gsum), r=[RT], w=[RT])
                                P.dve(lambda e: e.tensor_tensor(out=tmp48[:], in0=rl[:, 4:36].rearrange("p (g x) -> p g x", x=8),
                                                                in1=ohg.unsqueeze(2).to_broadcast([128, 4, 8]), op=ALU.mult), r=["rl", RT], w=["tmp48"])
                                P.dve(lambda e: e.tensor_reduce(out=sel, in_=tmp48[:].rearrange("p g x -> p x g"), axis=AX.X, op=ALU.add), r=["tmp48"], w=[RT])
                                P.dve(lambda e: e.tensor_reduce(out=m1, in_=sel, axis=AX.X, op=ALU.max), r=[RT], w=[RT])
                                P.dve(lambda e: e.tensor_scalar(out=oh1, in0=sel, scalar1=m1, scalar2=None, op0=ALU.is_equal), r=[RT], w=[RT])
                                P.dve(lambda e: e.scalar_tensor_tensor(out=sel2, in0=oh1, scalar=-1e30, in1=sel, op0=ALU.mult, op1=ALU.add), r=[RT], w=[RT])
                                P.dve(lambda e: e.tensor_reduce(out=m2, in_=sel2, axis=AX.X, op=ALU.max), r=[RT], w=[RT])
                                P.dve(lambda e: e.tensor_scalar(out=oh2, in0=sel2, scalar1=m2, scalar2=None, op0=ALU.is_equal), r=[RT], w=[RT])
                                P.dve(lambda e: e.tensor_tensor(out=dd, in0=m2, in1=m1, op=ALU.subtract), r=[RT], w=[RT])
                                P.act(lambda e: e.activation(out=e2, in_=dd, func=AF.Exp), r=[RT], w=[RT])
                                P.dve(lambda e: e.tensor_scalar(out=dd, in0=e2, scalar1=1.0, scalar2=None, op0=ALU.add), r=[RT], w=[RT])
                                P.dve(lambda e: e.reciprocal(out=dd, in_=dd), r=[RT], w=[RT])
                                P.dve(lambda e: e.tensor_tensor(out=g1_, in0=dd, in1=gsum, op=ALU.mult), r=[RT], w=[RT])
                                P.dve(lambda e: e.tensor_tensor(out=g2_, in0=g1_, in1=e2, op=ALU.mult), r=[RT], w=[RT])
                                P.dve(lambda e: e.tensor_scalar(out=gate8, in0=oh1, scalar1=g1_, scalar2=None, op0=ALU.mult), r=[RT], w=[RT])
                                P.dve(lambda e: e.scalar_tensor_tensor(out=gate8, in0=oh2, scalar=g2_, in1=gate8, op0=ALU.mult, op1=ALU.add), r=[RT], w=[RT])
                                P.dve(lambda e, t=t: e.tensor_tensor(out=G[:, t, :].rearrange("p (g x) -> p g x", x=8),
                                                                     in0=ohg.unsqueeze(2).to_broadcast([128, 4, 8]),
                                                                     in1=gate8.unsqueeze(1).to_broadcast([128, 4, 8]), op=ALU.mult),
                                      r=[RT], w=[("G", t)])
                        P.barrier()
                        yacc = sb(esH, "yacc", [128, nth, D], BF16)
                        with contextlib.ExitStack() as esC:
                            est = [sb(esC, f"est{i}", [128, 2048], F32) for i in range(3)]
                            wgb = [sb(esC, f"wgb{i}", [128, 8, 512], BF16) for i in range(2)]
                            wub = [sb(esC, f"wub{i}", [128, 8, 512], BF16) for i in range(2)]
                            wdb = [sb(esC, f"wdb{i}", [128, 4, D], BF16) for i in range(2)]
                            sg = [sb(esC, f"sg{i}", [128, 512], F32) for i in range(2)]
                            hh = [sb(esC, f"hh{i}", [128, 4, 512], BF16) for i in range(2)]
                            FLALL = [("flT", i) for i in range(nth)]
                            nst = 0
                            gi = 0
                            for ex in range(32):
                                ws = ex % 2
                                for (dst, src, key) in ((wgb, exp_w_gate, "wgb"), (wub, exp_w_up, "wub")):
                                    for pc in range(2):
                                        s_ = nst % 3
                                        nst += 1
                                        P.dma("sp", lambda e, s_=s_, src=src, pc=pc, ex=ex: e.dma_start(
                                            out=est[s_][:].rearrange("p (k n) -> p k n", n=512),
                                            in_=src[l, ex, pc * 512:(pc + 1) * 512, :].rearrange("(k p) n -> p k n", p=128)), w=[("est", s_)])
                                        P.pool(lambda e, s_=s_, dst=dst, pc=pc, ws=ws: e.tensor_copy(
                                            out=dst[ws][:, pc * 4:(pc + 1) * 4, :], in_=est[s_][:].rearrange("p (k n) -> p k n", n=512)),
                                            r=[("est", s_)], w=[(key, ws, pc)])
                                for pc in range(2):
                                    s_ = nst % 3
                                    nst += 1
                                    P.dma("sp", lambda e, s_=s_, pc=pc, ex=ex: e.dma_start(
                                        out=est[s_][:].rearrange("p (k n) -> p k n", n=D),
                                        in_=exp_w_down[l, ex, pc * 256:(pc + 1) * 256, :].rearrange("(k p) n -> p k n", p=128)), w=[("est", s_)])
                                    P.pool(lambda e, s_=s_, pc=pc, ws=ws: e.tensor_copy(
                                        out=wdb[ws][:, pc * 2:(pc + 1) * 2, :], in_=est[s_][:].rearrange("p (k n) -> p k n", n=D)),
                                        r=[("est", s_)], w=[("wdb", ws, pc)])
                                WG = [("wgb", ws, 0), ("wgb", ws, 1)]
                                WU = [("wub", ws, 0), ("wub", ws, 1)]
                                WD = [("wdb", ws, 0), ("wdb", ws, 1)]
                                for (t0, nt) in groups:
                                    N = nt * 128
                                    o0 = (t0 - tb) * 128
                                    hs = gi % 2
                                    gi += 1
                                    for jx in range(4):
                                        ba = (jx % 2) * 2
                                        for (bk_, wsrc, wk) in ((ba, wgb, WG), (ba + 1, wub, WU)):
                                            for k in range(8):
                                                P.pe(lambda e, bk_=bk_, wsrc=wsrc, k=k, jx=jx, o0=o0, N=N, ws=ws: e.matmul(
                                                    banks[bk_][:, 0:N], lhsT=wsrc[ws][:, k, jx * 128:(jx + 1) * 128], rhs=flT[:, k, o0:o0 + N],
                                                    start=(k == 0), stop=(k == 7)), r=wk + FLALL, w=[BK(bk_)])
                                        sgs = jx % 2
                                        P.act(lambda e, ba=ba, sgs=sgs, N=N: e.activation(out=sg[sgs][:, 0:N], in_=banks[ba][:, 0:N], func=AF.Silu),
                                              r=[BK(ba)], w=[("sg", sgs)])
                                        P.dve(lambda e, ba=ba, sgs=sgs, N=N, hs=hs, jx=jx: e.tensor_tensor(
                                            out=hh[hs][:, jx, 0:N], in0=sg[sgs][:, 0:N], in1=banks[ba + 1][:, 0:N], op=ALU.mult),
                                            r=[("sg", sgs), BK(ba + 1)], w=[("hh", hs, jx)])
                                    HK = [("hh", hs, jx) for jx in range(4)]
                                    for ti in range(nt):
                                        t = t0 + ti
                                        tl = t - tb
                                        for c2 in range(2):
                                            bk_ = 4 + c2
                                            for jx in range(4):
                                                P.pe(lambda e, bk_=bk_, jx=jx, hs=hs, ti=ti, c2=c2, ws=ws: e.matmul(
                                                    banks[bk_][:], lhsT=hh[hs][:, jx, ti * 128:(ti + 1) * 128], rhs=wdb[ws][:, jx, c2 * 512:(c2 + 1) * 512],
                                                    start=(jx == 0), stop=(jx == 3)), r=HK + WD, w=[BK(bk_)])
                                            ya = yacc[:, tl, c2 * 512:(c2 + 1) * 512]
                                            if ex == 0:
                                                P.dve(lambda e, bk_=bk_, ya=ya, t=t, ex=ex: e.tensor_scalar(
                                                    out=ya, in0=banks[bk_][:], scalar1=G[:, t, ex:ex + 1], scalar2=None, op0=ALU.mult),
                                                    r=[BK(bk_), ("G", t)], w=[("yacc", tl, c2)])
                                            else:
                                                P.dve(lambda e, bk_=bk_, ya=ya, t=t, ex=ex: e.scalar_tensor_tensor(
                                                    out=ya, in0=banks[bk_][:], scalar=G[:, t, ex:ex + 1], in1=ya, op0=ALU.mult, op1=ALU.add),
                                                    r=[BK(bk_), ("G", t), ("yacc", tl, c2)], w=[("yacc", tl, c2)])
                        P.barrier()
                        with contextlib.ExitStack() as esC:
                            xr = [sb(esC, f"xr{i}", [128, D], F32) for i in range(2)]
                            xo = [sb(esC, f"xo{i}", [128, D], F32) for i in range(2)]
                            for t in tiles:
                                if last and t < NCTX_T:
                                    continue
                                j = 1 if t < NCTX_T else 0
                                a = t % 2
                                tl = t - tb
                                rows = slice(t * 128, (t + 1) * 128)
                                P.dma("sp", lambda e, a=a, rows=rows: e.dma_start(out=xr[a][:], in_=xs1[rows, :]), r=[("xs1", t)], w=[("xr", a)])
                                P.dve(lambda e, a=a, tl=tl, j=j: e.tensor_tensor(
                                    out=xo[a][:], in0=yacc[:, tl, :], in1=g2row[:, j, :, :].rearrange("p k n -> p (k n)"), op=ALU.mult),
                                    r=[("yacc", tl, 0), ("yacc", tl, 1), "g2row"], w=[("xo", a)])
                                P.pool(lambda e, a=a: e.tensor_tensor(out=xo[a][:], in0=xo[a][:], in1=xr[a][:], op=ALU.add),
                                       r=[("xo", a), ("xr", a)], w=[("xo", a)])
                                if last:
                                    lt = t - NCTX_T
                                    P.dma("pool", lambda e, a=a, lt=lt: e.dma_start(out=y_out[lt * 128:(lt + 1) * 128, :], in_=xo[a][:]),
                                          r=[("xo", a)], w=[("yout", t)])
                                else:
                                    P.dma("pool", lambda e, a=a, rows=rows: e.dma_start(out=stream_out[rows, :], in_=xo[a][:]),
                                          r=[("xo", a)], w=[("sout", t)])
                        P.barrier()

                for hi_, groups_ in enumerate(HALVES):
                    do_half(hi_, groups_)

        cur_in = xin
        if zero_ml:
            with contextlib.ExitStack() as esZ:
                zt = sb(esZ, "zt", [128, 256], BF16)
                P.pool(lambda e: e.memset(zt[:], 0.0), w=["zt"])
                for t in range(NT):
                    P.dma("pool", lambda e, t=t: e.dma_start(out=ml[t * 128:(t + 1) * 128, 768:1024], in_=zt[:]), r=["zt"],
                          w=[("ml_g", t)])
            P.barrier()
        for l in range(NL):
            with contextlib.ExitStack() as esM:
                phase_mod(l, esM)
            P.barrier()
            if stop_after == "mod":
                break
            phase_proj_attn(l, cur_in)
            if stop_after in ("proj", "attn", "p1a", "p1b", "p1c", "p1d", "p1e", "p1f", "p1g"):
                break
            phase_ssm(l)
            if stop_after in ("conv", "ssm", "ssd1", "ssd2", "ssd3"):
                break
            phase_gdn(l)
            if stop_after in ("gconv", "gdn"):
                break
            phase_out_moe(l, cur_in, xs2, last=(l == NL - 1))
            cur_in = xs2

        P.emit()
    return nc


def _rope_tables():
    rows = 4096 // 64
    row = np.repeat(np.arange(rows, dtype=np.float32), 64)
    col = np.tile(np.arange(64, dtype=np.float32), rows)
    half = 16
    inv = (10000.0 ** (-np.arange(0, half, 2, dtype=np.float32) / half)).astype(np.float32)
    ang_r = row[:, None] * inv
    ang_c = col[:, None] * inv
    ang = np.concatenate([ang_r, ang_r, ang_c, ang_c], axis=-1).astype(np.float32)
    cos, sin = np.cos(ang), np.sin(ang)
    sgn = np.concatenate([-np.ones(8), np.ones(8), -np.ones(8), np.ones(8)]).astype(np.float32)
    tb = np.stack([cos, sin * sgn]).astype(np.float32)
    return np.ascontiguousarray(tb.reshape(2, 32, 128, 32).transpose(0, 2, 1, 3))


def _chunkT(v, n):
    return np.ascontiguousarray(np.swapaxes(v.reshape(v.shape[:-1] + (n, 128)), -1, -2))


def prep_inputs(inp, cores=range(8)):
    f = lambda a: np.ascontiguousarray(np.asarray(a, dtype=np.float32))
    shared = {
        "rope": _rope_tables(),
        "w_mod": f(inp["w_mod"]), "b_modT": _chunkT(f(inp["b_mod"]), 48),
        "n1T": _chunkT(f(inp["norm1_w"]), 8), "n2T": _chunkT(f(inp["norm2_w"]), 8),
        "w_in": f(inp["w_in"]), "w_out": f(inp["w_out"]),
        "ssm_conv_wT": np.ascontiguousarray(f(inp["ssm_conv_w"]).reshape(L_DEPTH, 5, 6, 128).transpose(0, 3, 2, 1)),
        "ssm_conv_bT": _chunkT(f(inp["ssm_conv_b"]), 6),
        "gdn_conv_wT": np.ascontiguousarray(f(inp["gdn_conv_w"]).reshape(L_DEPTH, 5, 6, 128).transpose(0, 3, 2, 1)),
        "router_w": np.ascontiguousarray(np.concatenate([f(inp["router_g_w"]), f(inp["router_e_w"])], axis=-1)),
        "router_b": np.ascontiguousarray(np.concatenate([f(inp["router_g_b"]), f(inp["router_e_b"])], axis=-1)),
    }
    for k in ("diff_qn_w", "diff_kn_w", "diff_lq1", "diff_lk1", "diff_lq2", "diff_lk2", "diff_norm_w",
              "ssm_dt_bias", "ssm_a_log", "ssm_d", "ssm_norm_w", "gdn_dt_bias", "gdn_a_log", "gdn_norm_w",
              "exp_w_gate", "exp_w_up", "exp_w_down"):
        shared[k] = f(inp[k])
    x, c, ctx, c_ctx = f(inp["x"]), f(inp["c"]), f(inp["ctx"]), f(inp["c_ctx"])
    maps = []
    for b in cores:
        m = dict(shared)
        m["xin"] = np.ascontiguousarray(np.concatenate([ctx[b], x[b]], axis=0))
        cc = np.stack([c[b], c_ctx], axis=-1)
        m["ccT"] = np.ascontiguousarray(cc.reshape(8, 128, 2).transpose(1, 0, 2))
        maps.append(m)
    return maps


_NC_CACHE = {}


def kernel(**inputs):
    if "nc" not in _NC_CACHE:
        _NC_CACHE["nc"] = build()
    nc = _NC_CACHE["nc"]
    maps = prep_inputs(inputs)
    res = run_bass_kernel_spmd(nc, maps, core_ids=list(range(8)))
    return np.stack([np.asarray(r["y"], dtype=np.float32) for r in res.results], axis=0)
```

```python
import contextlib
import math
import numpy as np
import concourse.bass as bass
import concourse.mybir as mybir
from concourse.bass_utils import run_bass_kernel_spmd

F32 = mybir.dt.float32
BF16 = mybir.dt.bfloat16
AF = mybir.ActivationFunctionType
ALU = mybir.AluOpType
AX = mybir.AxisListType

ENGS = ("pe", "act", "dve", "pool", "sp")
SAME_SYNC = {"pe": False, "act": True, "dve": True, "pool": True, "sp": True}
N_DMA_SEMS = 18


class Op:
    __slots__ = ("eng", "fn", "deps", "dma", "needs_inc", "idx", "sem", "val", "pos")

    def __init__(self, eng, fn, deps, dma):
        self.eng, self.fn, self.deps, self.dma = eng, fn, deps, dma
        self.needs_inc = False
        self.sem = None
        self.val = None


class Prog:
    def __init__(self, nc):
        self.nc = nc
        self.ops = {e: [] for e in ENGS}
        self.last_w = {}
        self.readers = {}
        self.pending_bar = {e: None for e in ENGS}
        self.since_bar = []

    def op(self, eng, fn, reads=(), writes=(), dma=False):
        deps = set()
        for k in reads:
            w = self.last_w.get(k)
            if w is not None:
                deps.add(w)
        for k in writes:
            w = self.last_w.get(k)
            if w is not None:
                deps.add(w)
            for r in self.readers.get(k, ()):
                deps.add(r)
        if self.pending_bar[eng] is not None:
            deps.update(self.pending_bar[eng])
            self.pending_bar[eng] = None
        rec = Op(eng, fn, deps, dma)
        for k in reads:
            self.readers.setdefault(k, []).append(rec)
        for k in writes:
            self.last_w[k] = rec
            self.readers[k] = []
        self.ops[eng].append(rec)
        if dma:
            self.since_bar.append(rec)
        return rec

    def barrier(self):
        deps = list(self.since_bar)
        for e in ENGS:
            if self.ops[e]:
                deps.append(self.ops[e][-1])
        for e in ENGS:
            cur = self.pending_bar[e]
            self.pending_bar[e] = (cur or []) + deps
        self.since_bar = []

    def pe(self, fn, r=(), w=()):
        return self.op("pe", fn, r, w)

    def act(self, fn, r=(), w=()):
        return self.op("act", fn, r, w)

    def dve(self, fn, r=(), w=()):
        return self.op("dve", fn, r, w)

    def pool(self, fn, r=(), w=()):
        return self.op("pool", fn, r, w)

    def dma(self, q, fn, r=(), w=()):
        return self.op(q, fn, r, w, dma=True)

    def emit(self, final_wait_ops=()):
        nc = self.nc
        for e in ENGS:
            for rec in self.ops[e]:
                for d in rec.deps:
                    if d.dma:
                        continue
                    if d.eng == rec.eng and not SAME_SYNC[rec.eng]:
                        continue
                    d.needs_inc = True
        with contextlib.ExitStack() as es:
            esem = {e: es.enter_context(nc.semaphore(f"s_{e}")) for e in ENGS}
            dsem = {e: [es.enter_context(nc.semaphore(f"d_{e}_{i}")) for i in range(N_DMA_SEMS)]
                    for e in ("sp", "pool", "act")}
            finals = {}
            for e in ENGS:
                cnt = 0
                dcnt = [0] * N_DMA_SEMS
                di = 0
                for rec in self.ops[e]:
                    if rec.dma:
                        k = di % N_DMA_SEMS
                        di += 1
                        dcnt[k] += 16
                        rec.sem, rec.val = dsem[e][k], dcnt[k]
                    elif rec.needs_inc:
                        cnt += 1
                        rec.sem, rec.val = esem[e], cnt
                if e in dsem:
                    finals[e] = list(dcnt)
            block = es.enter_context(nc.Block())

            def run(e, eh):
                waited = {}
                for rec in self.ops[e]:
                    need = {}
                    for d in rec.deps:
                        if (not d.dma) and d.eng == e and not SAME_SYNC[e]:
                            continue
                        s, v = d.sem, d.val
                        key = id(s)
                        if waited.get(key, 0) >= v:
                            continue
                        if key not in need or need[key][1] < v:
                            need[key] = (s, v)
                    if rec.dma and rec.val > 16:
                        s, v = rec.sem, rec.val - 16
                        key = id(s)
                        if waited.get(key, 0) < v and (key not in need or need[key][1] < v):
                            need[key] = (s, v)
                    for key, (s, v) in need.items():
                        eh.wait_ge(s, v)
                        waited[key] = v
                    ins = rec.fn(eh)
                    if rec.dma:
                        ins.then_inc(rec.sem, 16)
                    elif rec.needs_inc:
                        ins.then_inc(rec.sem, 1)
                if e == "sp":
                    for q, cnts in finals.items():
                        for k, v in enumerate(cnts):
                            if v > 0:
                                eh.wait_ge(dsem[q][k], v)

            block.tensor(lambda eh: run("pe", eh))
            block.scalar(lambda eh: run("act", eh))
            block.vector(lambda eh: run("dve", eh))
            block.gpsimd(lambda eh: run("pool", eh))
            block.sync(lambda eh: run("sp", eh))


D = 1024
T = 4352
NT = 34
NCTX_T = 2
L_DEPTH = 2
EPS = 1e-6
IN_DIM = 3096
GROUPS = [(0, 2)] + [(2 + 4 * i, 4) for i in range(8)]
NEG = -30000.0
C_QKV, C_Z, C_XBC, C_DT, C_GQKV, C_GATE, C_BETA, C_A = 0, 768, 1280, 2048, 2056, 2824, 3080, 3088


def build(NL=2, dbg=False, stop_after=None, zero_ml=False, small_exp=False):
    nc = bass.Bass("TRN2", target_bir_lowering=False)
    P = Prog(nc)

    def din(name, shape, dt=F32):
        return nc.dram_tensor(name, list(shape), dt, kind="ExternalInput").ap()

    def dscr(name, shape, dt, out=False):
        return nc.dram_tensor(name, list(shape), dt, kind=("ExternalOutput" if out else "Internal")).ap()

    xin = din("xin", [T, D])
    ccT = din("ccT", [128, 8, 2])
    rope = din("rope", [2, 128, 32, 32])
    w_mod = din("w_mod", [L_DEPTH, D, 6 * D])
    b_modT = din("b_modT", [L_DEPTH, 128, 48])
    n1T = din("n1T", [L_DEPTH, 128, 8])
    n2T = din("n2T", [L_DEPTH, 128, 8])
    w_in = din("w_in", [L_DEPTH, D, IN_DIM])
    w_out = din("w_out", [L_DEPTH, D, D])
    diff_qn_w = din("diff_qn_w", [L_DEPTH, 32])
    diff_kn_w = din("diff_kn_w", [L_DEPTH, 32])
    diff_lq1 = din("diff_lq1", [L_DEPTH, 32])
    diff_lk1 = din("diff_lk1", [L_DEPTH, 32])
    diff_lq2 = din("diff_lq2", [L_DEPTH, 32])
    diff_lk2 = din("diff_lk2", [L_DEPTH, 32])
    diff_norm_w = din("diff_norm_w", [L_DEPTH, 64])
    ssm_conv_wT = din("ssm_conv_wT", [L_DEPTH, 128, 6, 5])
    ssm_conv_bT = din("ssm_conv_bT", [L_DEPTH, 128, 6])
    ssm_dt_bias = din("ssm_dt_bias", [L_DEPTH, 2, 8])
    ssm_a_log = din("ssm_a_log", [L_DEPTH, 2, 8])
    ssm_d = din("ssm_d", [L_DEPTH, 8])
    ssm_norm_w = din("ssm_norm_w", [L_DEPTH, 512])
    gdn_conv_wT = din("gdn_conv_wT", [L_DEPTH, 128, 6, 5])
    gdn_dt_bias = din("gdn_dt_bias", [L_DEPTH, 2, 4])
    gdn_a_log = din("gdn_a_log", [L_DEPTH, 2, 4])
    gdn_norm_w = din("gdn_norm_w", [L_DEPTH, 64])
    router_w = din("router_w", [L_DEPTH, D, 36])
    router_b = din("router_b", [L_DEPTH, 36])
    _ne = 1 if small_exp else 32
    _nl = 1 if small_exp else L_DEPTH
    exp_w_gate = din("exp_w_gate", [_nl, _ne, D, 512])
    exp_w_up = din("exp_w_up", [_nl, _ne, D, 512])
    exp_w_down = din("exp_w_down", [_nl, _ne, 512, D])

    y_out = nc.dram_tensor("y", [4096, D], F32, kind="ExternalOutput").ap()
    xs1 = dscr("xs1", [T, D], F32, out=dbg)
    xs2 = dscr("xs2", [T, D], F32)
    hT = dscr("hT", [12, 128, T], BF16, out=dbg)
    zs = dscr("zs", [T, 512], BF16)
    gs = dscr("gs", [T, 256], BF16)
    ml = dscr("ml", [T, D], BF16, out=dbg)
    modrows = dscr("modrows", [96, 128], F32)

    outs = []
    with contextlib.ExitStack() as es0:
        _cnt = [0]

        def sb(es, name, shape, dt):
            _cnt[0] += 1
            return es.enter_context(nc.sbuf_tensor(f"{name}_{_cnt[0]}", list(shape), dt))

        banks = [es0.enter_context(nc.psum_tensor(f"bank{i}", [128, 512], F32)) for i in range(6)]
        tbanks = {6 + i: es0.enter_context(nc.psum_tensor(f"tbank{i}", [128, 1024], BF16)) for i in range(2)}

        def BK(i):
            return ("bank", i)

        identf = sb(es0, "identf", [128, 128], F32)
        identb = sb(es0, "identb", [128, 128], BF16)
        onesf = sb(es0, "onesf", [128, 128], F32)
        Uf = sb(es0, "Uf", [128, 128], F32)
        Ub = sb(es0, "Ub", [128, 128], F32)
        nUf = sb(es0, "nUf", [128, 128], F32)
        nUb = sb(es0, "nUb", [128, 128], F32)
        Sf = sb(es0, "Sf", [128, 128], F32)
        Sb_ = sb(es0, "Sb", [128, 128], F32)
        NMf = sb(es0, "NMf", [128, 4, 128], F32)
        NMb = sb(es0, "NMb", [128, 4, 128], F32)
        cc_sb = sb(es0, "cc_sb", [128, 8, 2], F32)
        csil = sb(es0, "csil", [128, 8, 2], F32)
        epsc = sb(es0, "epsc", [128, 1], F32)

        def mk_mask(t_ap, val_in, fill, pattern, cm, cmp, key):
            P.pool(lambda e: e.memset(t_ap, val_in), w=[key])
            P.pool(lambda e: e.affine_select(out=t_ap, in_=t_ap, compare_op=cmp, fill=fill, base=0,
                                             pattern=pattern, channel_multiplier=cm), r=[key], w=[key])

        mk_mask(identf[:], 0.0, 1.0, [[-1, 128]], 1, ALU.not_equal, "identf")
        P.pool(lambda e: e.tensor_copy(out=identb[:], in_=identf[:]), r=["identf"], w=["identb"])
        P.pool(lambda e: e.memset(onesf[:], 1.0), w=["onesf"])
        P.pool(lambda e: e.memset(epsc[:], EPS), w=["epsc"])
        mk_mask(Uf[:], 1.0, 0.0, [[1, 128]], -1, ALU.is_ge, "Uf")
        mk_mask(Ub[:], 1.0, 0.0, [[-1, 128]], 1, ALU.is_ge, "Ub")
        mk_mask(nUf[:], -1.0, 0.0, [[1, 128]], -1, ALU.is_ge, "nUf")
        mk_mask(nUb[:], -1.0, 0.0, [[-1, 128]], 1, ALU.is_ge, "nUb")
        mk_mask(Sf[:], 1.0, 0.0, [[-1, 128]], 1, ALU.is_gt, "Sf")
        mk_mask(Sb_[:], 1.0, 0.0, [[1, 128]], -1, ALU.is_gt, "Sb")
        for j in range(4):
            mk_mask(NMf[:, j, :], NEG, 0.0, [[-1, 128]], 1, ALU.is_gt, ("NMf", j))
            mk_mask(NMb[:, j, :], NEG, 0.0, [[1, 128]], -1, ALU.is_gt, ("NMb", j))
        NMf_keys = [("NMf", j) for j in range(4)]
        NMb_keys = [("NMb", j) for j in range(4)]

        blkf = sb(es0, "blkf", [128, 128], F32)
        offd = sb(es0, "offd", [128, 128], F32)
        UBf = sb(es0, "UBf", [128, 128], F32)
        UBb = sb(es0, "UBb", [128, 128], F32)
        nUBf = sb(es0, "nUBf", [128, 128], F32)
        nUBb = sb(es0, "nUBb", [128, 128], F32)
        SBf = sb(es0, "SBf", [128, 128], F32)
        SBb = sb(es0, "SBb", [128, 128], F32)
        NMBf = sb(es0, "NMBf", [128, 4, 128], F32)
        NMBb = sb(es0, "NMBb", [128, 4, 128], F32)
        chunkind = sb(es0, "chunkind", [128, 2], F32)
        P.pool(lambda e: e.memset(blkf[:], 0.0), w=["blkf"])
        P.pool(lambda e: e.memset(blkf[0:64, 0:64], 1.0), r=["blkf"], w=["blkf"])
        P.pool(lambda e: e.memset(blkf[64:128, 64:128], 1.0), r=["blkf"], w=["blkf"])
        P.pool(lambda e: e.memset(chunkind[:], 0.0), w=["chunkind"])
        P.pool(lambda e: e.memset(chunkind[0:64, 0:1], 1.0), r=["chunkind"], w=["chunkind"])
        P.pool(lambda e: e.memset(chunkind[64:128, 1:2], 1.0), r=["chunkind"], w=["chunkind"])
        mk_mask(offd[:], 1.0, 0.0, [[-1, 128]], 1, ALU.not_equal, "offd")
        for (dst, src, kd, ks) in ((UBf, Uf, "UBf", "Uf"), (UBb, Ub, "UBb", "Ub"), (SBf, Sf, "SBf", "Sf"), (SBb, Sb_, "SBb", "Sb"),
                                   (nUBf, nUf, "nUBf", "nUf"), (nUBb, nUb, "nUBb", "nUb")):
            P.pool(lambda e, dst=dst, src=src: e.tensor_tensor(out=dst[:], in0=src[:], in1=blkf[:], op=ALU.mult), r=[ks, "blkf"], w=[kd])
        for j in range(4):
            P.dve(lambda e, j=j: e.tensor_scalar(out=NMBf[:, j, :], in0=UBb[:], scalar1=-NEG, scalar2=NEG, op0=ALU.mult, op1=ALU.add),
                  r=["UBb"], w=[("NMBf", j)])
            P.dve(lambda e, j=j: e.tensor_scalar(out=NMBb[:, j, :], in0=UBf[:], scalar1=-NEG, scalar2=NEG, op0=ALU.mult, op1=ALU.add),
                  r=["UBf"], w=[("NMBb", j)])
        NMBf_keys = [("NMBf", j) for j in range(4)]
        NMBb_keys = [("NMBb", j) for j in range(4)]
        P.dma("sp", lambda e: e.dma_start(out=cc_sb[:], in_=ccT), w=["cc_sb"])
        P.act(lambda e: e.activation(out=csil[:], in_=cc_sb[:], func=AF.Silu), r=["cc_sb"], w=["csil"])

        modT = sb(es0, "modT", [128, 48, 2], F32)
        s1 = sb(es0, "s1", [128, 8, 2], F32)
        s2 = sb(es0, "s2", [128, 8, 2], F32)
        g1row = sb(es0, "g1row", [128, 2, 8, 128], F32)
        g2row = sb(es0, "g2row", [128, 2, 8, 128], F32)
        small_all = sb(es0, "small_all", [128, NT, 24], F32)

        def bcast_load(es, name, src_ap, n):
            t = sb(es, name, [128, n], F32)
            P.dma("sp", lambda e: e.dma_start(out=t[:], in_=src_ap.partition_broadcast(128)), w=[name])
            return t

        def rsqrt_ops(dst_ap, src_ap, scale, keys_r, key_w, n_eps=None):
            P.dve(lambda e: e.tensor_scalar(out=dst_ap, in0=src_ap, scalar1=scale, scalar2=EPS,
                                            op0=ALU.mult, op1=ALU.add), r=keys_r, w=[key_w])
            P.act(lambda e: e.activation(out=dst_ap, in_=dst_ap, func=AF.Sqrt), r=[key_w], w=[key_w])
            P.dve(lambda e: e.reciprocal(out=dst_ap, in_=dst_ap), r=[key_w], w=[key_w])

        def phase_mod(l, es):
            wm = [sb(es, f"wm{i}", [128, 8, 512], F32) for i in range(2)]
            bT = sb(es, "bT", [128, 48], F32)
            nT = sb(es, "nT", [128, 2, 8], F32)
            mrs = sb(es, "mrs", [96, 128], F32)
            tmp = sb(es, "modtmp", [128, 8, 2], F32)
            P.dma("sp", lambda e: e.dma_start(out=bT[:], in_=b_modT[l]), w=["bT"])
            P.dma("sp", lambda e: e.dma_start(out=nT[:, 0, :], in_=n1T[l]), w=["nT0"])
            P.dma("sp", lambda e: e.dma_start(out=nT[:, 1, :], in_=n2T[l]), w=["nT1"])
            pm = banks[0][:, 0:96].rearrange("p (c j) -> p c j", j=2)
            for piece in range(12):
                s = piece % 2
                P.dma("sp", lambda e, s=s, piece=piece: e.dma_start(
                    out=wm[s][:], in_=w_mod[l][:, piece * 512:(piece + 1) * 512].rearrange("(k p) n -> p k n", p=128)),
                    w=[("wm", s)])
                for fc in range(4):
                    c = piece * 4 + fc
                    for k in range(8):
                        P.pe(lambda e, s=s, fc=fc, c=c, k=k: e.matmul(
                            pm[:, c, :], lhsT=wm[s][:, k, fc * 128:(fc + 1) * 128], rhs=csil[:, k, :],
                            start=(k == 0), stop=(k == 7)), r=[("wm", s), "csil"], w=[BK(0)])
            P.dve(lambda e: e.tensor_tensor(out=modT[:], in0=pm, in1=bT[:].unsqueeze(2).to_broadcast([128, 48, 2]),
                                            op=ALU.add), r=[BK(0), "bT"], w=["modT"])
            for (sx, lo, ni, nk) in ((s1, 8, 0, "nT0"), (s2, 32, 1, "nT1")):
                P.dve(lambda e, lo=lo: e.tensor_scalar(out=tmp[:], in0=modT[:, lo:lo + 8, :], scalar1=1.0, scalar2=None,
                                                       op0=ALU.add), r=["modT"], w=["modtmp"])
                P.dve(lambda e, sx=sx, ni=ni: e.tensor_tensor(
                    out=sx[:], in0=tmp[:], in1=nT[:, ni, :].unsqueeze(2).to_broadcast([128, 8, 2]), op=ALU.mult),
                    r=["modtmp", nk], w=["s1k" if ni == 0 else "s2k"])
            pmt = banks[1][0:96, 0:128]
            P.pe(lambda e: e.transpose(out=pmt, in_=modT[:].rearrange("p c j -> p (c j)"), identity=identf[:]),
                 r=["modT", "identf"], w=[BK(1)])
            P.act(lambda e: e.activation(out=mrs[:], in_=pmt, func=AF.Copy), r=[BK(1)], w=["mrs"])
            P.dma("sp", lambda e: e.dma_start(out=modrows, in_=mrs[:]), r=["mrs"], w=["modrows"])
            mr3 = modrows.rearrange("(c j) p -> j c p", j=2)
            for j in range(2):
                P.dma("sp", lambda e, j=j: e.dma_start(out=g1row[:, j, :, :], in_=mr3[j, 16:24, :].partition_broadcast(128)),
                      r=["modrows"], w=["g1row"])
                P.dma("sp", lambda e, j=j: e.dma_start(out=g2row[:, j, :, :], in_=mr3[j, 40:48, :].partition_broadcast(128)),
                      r=["modrows"], w=["g2row"])

        S1K = "s1k"
        S2K = "s2k"

        def norm_tile_T(es_tmp, tag, src_rows_ap, j, scl, shf_lo, dstT_ap_fn, bank_id, ring, fp32_path=None):
            raise NotImplementedError

        def phase_proj_attn(l, stream_in):
            lam_init = 0.8 - 0.6 * math.exp(-0.3 * l)
            with contextlib.ExitStack() as esA:
                QKT = sb(esA, "QKT", [128, 6, T], BF16)
                V1 = sb(esA, "V1", [128, NT, 4, 65], BF16)
                lamt = sb(esA, "lamt", [128, 4], F32)
                nlam = sb(esA, "nlam", [128, 1], F32)
                dnw = bcast_load(esA, "dnw", diff_norm_w[l], 64)
                P.pool(lambda e: e.memset(V1[:, :, :, 64:65], 1.0), w=["V1ones"])
                lv = [bcast_load(esA, f"lv{i}", a[l], 32) for i, a in
                      enumerate((diff_lq1, diff_lk1, diff_lq2, diff_lk2))]
                lj = sb(esA, "lj", [128, 32], F32)
                for i in range(2):
                    P.dve(lambda e, i=i: e.tensor_tensor(out=lj[:], in0=lv[2 * i][:], in1=lv[2 * i + 1][:], op=ALU.mult),
                          r=[f"lv{2 * i}", f"lv{2 * i + 1}"], w=["lj"])
                    P.dve(lambda e, i=i: e.tensor_reduce(out=lamt[:, i:i + 1], in_=lj[:], axis=AX.X, op=ALU.add),
                          r=["lj"], w=["lamt"])
                P.act(lambda e: e.activation(out=lamt[:, 2:4], in_=lamt[:, 0:2], func=AF.Exp), r=["lamt"], w=["lamt"])
                P.dve(lambda e: e.scalar_tensor_tensor(out=nlam[:], in0=lamt[:, 3:4], scalar=-lam_init, in1=lamt[:, 2:3],
                                                       op0=ALU.add, op1=ALU.subtract), r=["lamt"], w=["nlam"])

                with contextlib.ExitStack() as esB:
                    wib = sb(esB, "wib", [128, 8, IN_DIM], BF16)
                    wst = [sb(esB, f"wst{i}", [128, 1032], F32) for i in range(1)]
                    xt = [sb(esB, f"xt{i}", [128, D], F32) for i in range(2)]
                    xb = [sb(esB, f"xb{i}", [128, D], BF16) for i in range(2)]
                    junk = sb(esB, "junk", [128, D], BF16)
                    ss = [sb(esB, f"ss{i}", [128, 1], F32) for i in range(2)]
                    xnT = [sb(esB, f"xnT{i}", [128, 8, 512], BF16) for i in range(1)]
                    hst = [sb(esB, f"hst{i}", [128, 512], BF16) for i in range(3)]
                    wqk = sb(esB, "wqk", [128, 16, 32], F32)
                    cosT = sb(esB, "cosT", [128, 32, 32], F32)
                    sinT = sb(esB, "sinT", [128, 32, 32], F32)
                    sqb = sb(esB, "sqb", [128, 512], F32)
                    ssq = sb(esB, "ssq", [128, 16], F32)
                    qk32 = sb(esB, "qk32", [128, 16, 32], F32)
                    rtmp = sb(esB, "rtmp", [128, 16, 32], F32)
                    qkb = [sb(esB, f"qkb{i}", [128, 512], BF16) for i in range(2)]
                    zst = [sb(esB, f"zst{i}", [128, 512], BF16) for i in range(1)]
                    gst = [sb(esB, f"gst{i}", [128, 256], BF16) for i in range(2)]

                    n_p = 0
                    for k in range(8):
                        for c3 in range(3):
                            s = 0
                            n_p += 1
                            P.dma("sp", lambda e, s=s, k=k, c3=c3: e.dma_start(
                                out=wst[s][:], in_=w_in[l][k * 128:(k + 1) * 128, c3 * 1032:(c3 + 1) * 1032]),
                                w=[("wst", s)])
                            P.pool(lambda e, s=s, k=k, c3=c3: e.tensor_copy(
                                out=wib[:, k, c3 * 1032:(c3 + 1) * 1032], in_=wst[s][:]),
                                r=[("wst", s)], w=[("wib", k, c3)])
                    WIB = [("wib", k, c3) for k in range(8) for c3 in range(3)]
                    for gi in range(8):
                        P.dma("sp", lambda e, gi=gi: e.dma_start(out=wqk[:, gi, :], in_=diff_qn_w[l].partition_broadcast(128)),
                              w=["wqk"])
                        P.dma("sp", lambda e, gi=gi: e.dma_start(out=wqk[:, 8 + gi, :], in_=diff_kn_w[l].partition_broadcast(128)),
                              w=["wqk"])
                    P.dma("sp", lambda e: e.dma_start(out=cosT[:], in_=rope[0]), w=["cosT"])
                    P.dma("sp", lambda e: e.dma_start(out=sinT[:], in_=rope[1]), w=["sinT"])

                    ev = 0
                    if stop_after == "p1a":
                        return
                    for gidx, (t0, nt) in enumerate(GROUPS):
                        N = nt * 128
                        xs_ = 0
                        for ti in range(nt):
                            t = t0 + ti
                            j = 1 if t < NCTX_T else 0
                            a = t % 2
                            b2 = t % 2
                            P.dma("sp", lambda e, a=a, t=t: e.dma_start(out=xt[a][:], in_=stream_in[t * 128:(t + 1) * 128, :]),
                                  w=[("xt", a)])
                            P.act(lambda e, a=a: e.activation(out=junk[:], in_=xt[a][:], func=AF.Square, accum_out=ss[a][:]),
                                  r=[("xt", a)], w=["junk", ("ss", a)])
                            rsqrt_ops(ss[a][:], ss[a][:], 1.0 / D, [("ss", a)], ("ss", a))
                            P.dve(lambda e, a=a, b2=b2: e.tensor_scalar(out=xb[b2][:], in0=xt[a][:], scalar1=ss[a][:, 0:1],
                                                                        scalar2=None, op0=ALU.mult),
                                  r=[("xt", a), ("ss", a)], w=[("xb", b2)])
                            bk = 6 + b2
                            ptv = tbanks[bk][:].rearrange("p (k n) -> p k n", n=128)
                            for k in range(8):
                                P.pe(lambda e, b2=b2, k=k, ptv=ptv: e.transpose(out=ptv[:, k, :], in_=xb[b2][:, k * 128:(k + 1) * 128],
                                                                               identity=identb[:]),
                                     r=[("xb", b2), "identb"], w=[BK(bk)])
                            for k in range(8):
                                dst = xnT[xs_][:, k, ti * 128:(ti + 1) * 128]
                                if True:
                                    P.act(lambda e, dst=dst, k=k, j=j, ptv=ptv: e.activation(
                                        out=dst, in_=ptv[:, k, :], func=AF.Identity, scale=s1[:, k, j:j + 1], bias=modT[:, k, j:j + 1]),
                                        r=[BK(bk), "s1k", "modT"], w=[("xnT", xs_, ti, k)])
                                else:
                                    P.dve(lambda e, dst=dst, k=k, j=j, ptv=ptv: e.tensor_scalar(
                                        out=dst, in0=ptv[:, k, :], scalar1=s1[:, k, j:j + 1], scalar2=modT[:, k, j:j + 1],
                                        op0=ALU.mult, op1=ALU.add),
                                        r=[BK(bk), "s1k", "modT"], w=[("xnT", xs_, ti, k)])
                        XN = [("xnT", xs_, ti, k) for ti in range(nt) for k in range(8)]
                        if stop_after == "p1b":
                            return
                        for ch in range(12):
                            col0 = (C_XBC + ch * 128) if ch < 6 else (C_GQKV + (ch - 6) * 128)
                            bk = ev % 3
                            ev += 1
                            for k in range(8):
                                P.pe(lambda e, bk=bk, k=k, col0=col0, N=N, xs_=xs_: e.matmul(
                                    banks[bk][:, 0:N], lhsT=wib[:, k, col0:col0 + 128], rhs=xnT[xs_][:, k, 0:N],
                                    start=(k == 0), stop=(k == 7)), r=WIB + XN, w=[BK(bk)])
                            P.act(lambda e, bk=bk, N=N: e.activation(out=hst[bk][:, 0:N], in_=banks[bk][:, 0:N], func=AF.Copy),
                                  r=[BK(bk)], w=[("hst", bk)])
                            P.dma("pool", lambda e, bk=bk, ch=ch, t0=t0, N=N: e.dma_start(
                                out=hT[ch, :, t0 * 128:t0 * 128 + N], in_=hst[bk][:, 0:N]), r=[("hst", bk)], w=[("hT", ch, gidx)])
                        if stop_after == "p1c":
                            return
                        for ti in range(nt):
                            t = t0 + ti
                            latent = t >= NCTX_T
                            tk = slice(ti * 128, (ti + 1) * 128)
                            specs = [(3, 0, 512, C_QKV), (4, 0, 256, C_QKV + 512), (4, 256, 256, C_GATE),
                                     (5, 0, 512, C_Z), (3, 0, 0, 0)]
                            for (bk, o0, w_, c0) in specs[:4]:
                                for k in range(8):
                                    P.pe(lambda e, bk=bk, o0=o0, w_=w_, c0=c0, k=k, tk=tk, xs_=xs_: e.matmul(
                                        banks[bk][:, o0:o0 + w_], lhsT=xnT[xs_][:, k, tk], rhs=wib[:, k, c0:c0 + w_],
                                        start=(k == 0), stop=(k == 7)), r=WIB + XN, w=[BK(bk)])
                            P.act(lambda e: e.activation(out=sqb[:], in_=banks[3][:], func=AF.Square), r=[BK(3)], w=["sqb"])
                            P.dve(lambda e: e.tensor_reduce(out=ssq[:], in_=sqb[:].rearrange("p (g d) -> p g d", d=32),
                                                            axis=AX.X, op=ALU.add), r=["sqb"], w=["ssq"])
                            rsqrt_ops(ssq[:], ssq[:], 1.0 / 32, ["ssq"], "ssq")
                            P.dve(lambda e: e.tensor_tensor(out=qk32[:], in0=banks[3][:].rearrange("p (g d) -> p g d", d=32),
                                                            in1=ssq[:].unsqueeze(2).to_broadcast([128, 16, 32]), op=ALU.mult),
                                  r=[BK(3), "ssq"], w=["qk32"])
                            q2 = t % 2
                            if latent:
                                lt = t - NCTX_T
                                P.dve(lambda e: e.tensor_tensor(out=qk32[:], in0=qk32[:], in1=wqk[:], op=ALU.mult),
                                      r=["qk32", "wqk"], w=["qk32"])
                                x5 = qk32[:].rearrange("p g (a h e) -> p g a h e", a=2, h=2, e=8)
                                r5 = rtmp[:].rearrange("p g (a h e) -> p g a h e", a=2, h=2, e=8)
                                s4 = sinT[:, lt, :].rearrange("p (a h e) -> p a h e", a=2, h=2, e=8)
                                for hh in range(2):
                                    P.dve(lambda e, hh=hh, x5=x5, r5=r5, s4=s4: e.tensor_tensor(
                                        out=r5[:, :, :, hh, :], in0=x5[:, :, :, 1 - hh, :],
                                        in1=s4[:, :, hh, :].unsqueeze(1).to_broadcast([128, 16, 2, 8]), op=ALU.mult),
                                        r=["qk32", "sinT"], w=[("rtmp", hh)])
                                P.dve(lambda e, lt=lt: e.tensor_tensor(
                                    out=qk32[:], in0=qk32[:], in1=cosT[:, lt, :].unsqueeze(1).to_broadcast([128, 16, 32]),
                                    op=ALU.mult), r=["qk32", "cosT", ("rtmp", 0), ("rtmp", 1)], w=["qk32"])
                                P.dve(lambda e, q2=q2: e.tensor_tensor(
                                    out=qkb[q2][:].rearrange("p (g d) -> p g d", d=32), in0=qk32[:], in1=rtmp[:], op=ALU.add),
                                    r=["qk32", ("rtmp", 0), ("rtmp", 1)], w=[("qkb", q2)])
                            else:
                                P.dve(lambda e, q2=q2: e.tensor_tensor(
                                    out=qkb[q2][:].rearrange("p (g d) -> p g d", d=32), in0=qk32[:], in1=wqk[:], op=ALU.mult),
                                    r=["qk32", "wqk"], w=[("qkb", q2)])
                            bk = 6 + q2
                            ptq = tbanks[bk][:, 0:768].rearrange("p (c n) -> p c n", n=128)
                            for c in range(6):
                                lo = (c // 3) * 256 + (c % 3) * 96
                                wd = 96 if (c % 3) < 2 else 64
                                P.pe(lambda e, c=c, q2=q2, ptq=ptq, lo=lo, wd=wd: e.transpose(
                                    out=ptq[0:wd, c, :], in_=qkb[q2][:, lo:lo + wd], identity=identb[:]),
                                     r=[("qkb", q2), "identb"], w=[BK(bk)])
                            P.act(lambda e, t=t, ptq=ptq: e.activation(out=QKT[0:96, :, t * 128:(t + 1) * 128], in_=ptq[0:96, :, :],
                                                                       func=AF.Copy),
                                  r=[BK(bk)], w=[("QKT", t)])
                            if stop_after == "p1d":
                                return
                            P.dve(lambda e, t=t: e.tensor_copy(out=V1[:, t, :, 0:64],
                                                               in_=banks[4][:, 0:256].rearrange("p (h v) -> p h v", v=64)),
                                  r=[BK(4)], w=[("V1", t)])
                            P.act(lambda e, q2=q2: e.activation(out=gst[q2][:], in_=banks[4][:, 256:512], func=AF.Silu),
                                  r=[BK(4)], w=[("gst", q2)])
                            P.dma("pool", lambda e, q2=q2, t=t: e.dma_start(out=gs[t * 128:(t + 1) * 128, :], in_=gst[q2][:]),
                                  r=[("gst", q2)], w=[("gs", t)])
                            P.act(lambda e, q2=q2: e.activation(out=zst[0][:], in_=banks[5][:], func=AF.Silu),
                                  r=[BK(5)], w=[("zst", 0)])
                            P.dma("pool", lambda e, q2=q2, t=t: e.dma_start(out=zs[t * 128:(t + 1) * 128, :], in_=zst[0][:]),
                                  r=[("zst", 0)], w=[("zs", t)])
                            for (o0, w_, c0) in ((0, 8, C_DT), (8, 16, C_BETA)):
                                for k in range(8):
                                    P.pe(lambda e, o0=o0, w_=w_, c0=c0, k=k, tk=tk, xs_=xs_: e.matmul(
                                        banks[5][:, o0:o0 + w_], lhsT=xnT[xs_][:, k, tk], rhs=wib[:, k, c0:c0 + w_],
                                        start=(k == 0), stop=(k == 7)), r=WIB + XN, w=[BK(5)])
                            P.dve(lambda e, t=t: e.tensor_copy(out=small_all[:, t, :], in_=banks[5][:, 0:24]),
                                  r=[BK(5)], w=[("small", t)])
                            if stop_after == "p1e" or (stop_after == "p1g" and t == 2):
                                return
                        if stop_after == "p1f":
                            return
                P.barrier()
                if stop_after == "proj":
                    return
                with contextlib.ExitStack() as esC:
                    PT = [sb(esC, f"PT{i}", [128, 512], BF16) for i in range(4)]
                    osb = [sb(esC, f"osb{i}", [128, 4, 2, 65], F32) for i in range(2)]
                    rden = sb(esC, "rden", [128, 4, 2], F32)
                    o1 = sb(esC, "o1", [128, 4, 64], F32)
                    od = sb(esC, "od", [128, 4, 64], F32)
                    osq = sb(esC, "osq", [128, 4, 64], F32)
                    orr = sb(esC, "orr", [128, 4], F32)
                    aout = [sb(esC, f"aout{i}", [128, 4, 4, 64], BF16) for i in range(2)]
                    scale = 32 ** -0.5
                    QKALL = [("QKT", t) for t in range(NT)]
                    VALL = [("V1", t) for t in range(NT)] + ["V1ones"]
                    items = []

                    def mk_item(idx, gidx, t0, nt, h, m, ki, kt, nk, ob, ao):
                        N = nt * 128
                        mm = 2 * h + m
                        c = mm // 3
                        pb = 32 * (mm % 3)
                        accb = 4 + (mm % 2)
                        accv = banks[accb][:, 0:nt * 65].rearrange("p (q v) -> p q v", v=65)
                        sbk = idx % 4

                        def qk():
                            P.pe(lambda e: e.matmul(
                                banks[sbk][:, 0:N], lhsT=QKT[pb:pb + 32, 3 + c, kt * 128:(kt + 1) * 128],
                                rhs=QKT[pb:pb + 32, c, t0 * 128:t0 * 128 + N], start=True, stop=True),
                                r=QKALL, w=[BK(sbk)])

                        def rest():
                            P.act(lambda e: e.activation(out=PT[sbk][:, 0:N], in_=banks[sbk][:, 0:N], func=AF.Exp, scale=scale),
                                  r=[BK(sbk)], w=[("PT", sbk)])
                            for qb in range(nt):
                                P.pe(lambda e, qb=qb: e.matmul(
                                    accv[:, qb, :], lhsT=PT[sbk][:, qb * 128:(qb + 1) * 128], rhs=V1[:, kt, h, :],
                                    start=(ki == 0 and qb == 0), stop=(ki == nk - 1 and qb == nt - 1)),
                                    r=[("PT", sbk)] + VALL, w=[BK(accb)])
                            if ki == nk - 1:
                                P.act(lambda e: e.activation(out=ob[:, 0:nt, m, :], in_=accv, func=AF.Copy),
                                      r=[BK(accb)], w=[("osb", h % 2, m)])
                                if m == 1:
                                    head_post(gidx, nt, h, ob, ao)
                                    if h == 3:
                                        for qb in range(nt):
                                            t = t0 + qb
                                            P.dma("pool", lambda e, qb=qb, t=t: e.dma_start(
                                                out=ml[t * 128:(t + 1) * 128, 0:256], in_=ao[:, qb, :, :].rearrange("p h v -> p (h v)")),
                                                r=[("aout", gidx % 2, hh_) for hh_ in range(4)], w=[("ml_a", t)])
                        return qk, rest

                    def head_post(gidx, nt, h, ob, ao):
                        OK_ = [("osb", h % 2, 0), ("osb", h % 2, 1)]
                        P.dve(lambda e: e.reciprocal(out=rden[:, 0:nt, :], in_=ob[:, 0:nt, :, 64]), r=OK_, w=["rden"])
                        P.dve(lambda e: e.tensor_tensor(
                            out=o1[:, 0:nt, :], in0=ob[:, 0:nt, 1, 0:64],
                            in1=rden[:, 0:nt, 1:2].to_broadcast([128, nt, 64]), op=ALU.mult), r=OK_ + ["rden"], w=["o1"])
                        P.dve(lambda e: e.tensor_tensor(
                            out=od[:, 0:nt, :], in0=ob[:, 0:nt, 0, 0:64],
                            in1=rden[:, 0:nt, 0:1].to_broadcast([128, nt, 64]), op=ALU.mult), r=OK_ + ["rden"], w=["od"])
                        P.dve(lambda e: e.scalar_tensor_tensor(
                            out=od[:, 0:nt, :], in0=o1[:, 0:nt, :], scalar=nlam[:, 0:1], in1=od[:, 0:nt, :],
                            op0=ALU.mult, op1=ALU.add), r=["o1", "od", "nlam"], w=["od"])
                        P.pool(lambda e: e.tensor_tensor(out=osq[:, 0:nt, :], in0=od[:, 0:nt, :], in1=od[:, 0:nt, :],
                                                         op=ALU.mult), r=["od"], w=["osq"])
                        P.dve(lambda e: e.tensor_reduce(out=orr[:, 0:nt], in_=osq[:, 0:nt, :], axis=AX.X, op=ALU.add),
                              r=["osq"], w=["orr"])
                        rsqrt_ops(orr[:, 0:nt], orr[:, 0:nt], 1.0 / 64, ["orr"], "orr")
                        P.dve(lambda e: e.tensor_tensor(out=od[:, 0:nt, :], in0=od[:, 0:nt, :],
                                                        in1=orr[:, 0:nt].unsqueeze(2).to_broadcast([128, nt, 64]),
                                                        op=ALU.mult), r=["od", "orr"], w=["od"])
                        P.dve(lambda e: e.scalar_tensor_tensor(
                            out=ao[:, 0:nt, h, :], in0=od[:, 0:nt, :], scalar=(1.0 - lam_init),
                            in1=dnw[:].unsqueeze(1).to_broadcast([128, nt, 64]), op0=ALU.mult, op1=ALU.mult),
                            r=["od", "dnw"], w=[("aout", gidx % 2, h)])

                    for gidx, (t0, nt) in enumerate(GROUPS):
                        ktiles = list(range(NCTX_T)) if gidx == 0 else list(range(NT))
                        ao = aout[gidx % 2]
                        for h in range(4):
                            ob = osb[h % 2]
                            for m in range(2):
                                for ki, kt in enumerate(ktiles):
                                    items.append(mk_item(len(items), gidx, t0, nt, h, m, ki, kt, len(ktiles), ob, ao))
                    LOOK = 3
                    for i in range(len(items) + LOOK):
                        if i < len(items):
                            items[i][0]()
                        if i - LOOK >= 0:
                            items[i - LOOK][1]()
                P.barrier()

        ORDER = [list(range(NT)), [1, 0] + list(range(NT - 1, NCTX_T - 1, -1))]

        def conv_chunk(wT_ap, chs, dgs, cw, key_cw, bias_ap_fn, hcs, sink, hoff):
            pass

        def phase_ssm(l):
            with contextlib.ExitStack() as esA:
                xs_tm = sb(esA, "xs_tm", [128, NT, 512], BF16)
                B_tm = sb(esA, "B_tm", [128, NT, 128], BF16)
                BT = sb(esA, "BT", [128, T], BF16)
                CT = sb(esA, "CT", [128, T], BF16)
                BTm = [sb(esA, f"BTm{i}", [128, T], BF16) for i in range(2)]
                P.pool(lambda e: e.memset(BTm[0][64:128, :], 0.0), w=["BTm0z"])
                P.pool(lambda e: e.memset(BTm[1][0:64, :], 0.0), w=["BTm1z"])
                Y = sb(esA, "Y", [128, NT, 512], BF16)
                dt_all = sb(esA, "dt_all", [128, NT, 2, 8], F32)
                dA_all = sb(esA, "dA_all", [128, NT, 2, 8], F32)
                hS = sb(esA, "hS", [128, 2, 4, 64], F32)
                hSb = sb(esA, "hSb", [128, 2, 4, 64], BF16)
                dtb = bcast_load(esA, "dtb", ssm_dt_bias[l].rearrange("a b -> (a b)"), 16)
                alg = bcast_load(esA, "alg", ssm_a_log[l].rearrange("a b -> (a b)"), 16)
                dsk = bcast_load(esA, "dsk", ssm_d[l], 8)
                nw = bcast_load(esA, "snw", ssm_norm_w[l], 512)
                tmpd = sb(esA, "tmpd", [128, NT, 8], F32)
                P.act(lambda e: e.activation(out=alg[:], in_=alg[:], func=AF.Exp), r=["alg"], w=["alg"])
                P.dve(lambda e: e.tensor_scalar(out=alg[:], in0=alg[:], scalar1=-1.0, scalar2=None, op0=ALU.mult), r=["alg"], w=["alg"])
                SM = [("small", t) for t in range(NT)]
                for d in range(2):
                    P.dve(lambda e, d=d: e.tensor_tensor(out=tmpd[:], in0=small_all[:, :, 0:8],
                                                         in1=dtb[:, d * 8:(d + 1) * 8].unsqueeze(1).to_broadcast([128, NT, 8]), op=ALU.add),
                          r=SM + ["dtb"], w=["tmpd"])
                    P.act(lambda e: e.activation(out=tmpd[:], in_=tmpd[:], func=AF.Exp), r=["tmpd"], w=["tmpd"])
                    P.act(lambda e, d=d: e.activation(out=dt_all[:, :, d, :], in_=tmpd[:], func=AF.Ln, bias=onesf[:, 0:1], scale=1.0),
                          r=["tmpd", "onesf"], w=[("dt_all", d)])
                    P.dve(lambda e, d=d: e.tensor_tensor(out=dA_all[:, :, d, :], in0=dt_all[:, :, d, :],
                                                         in1=alg[:, d * 8:(d + 1) * 8].unsqueeze(1).to_broadcast([128, NT, 8]), op=ALU.mult),
                          r=[("dt_all", d), "alg"], w=[("dA_all", d)])
                P.pool(lambda e: e.memset(hS[:], 0.0), w=[("hS", 0), ("hS", 1)])
                P.pool(lambda e: e.memset(hSb[:], 0.0), w=[("hSb", 0), ("hSb", 1)])
                with contextlib.ExitStack() as esB:
                    cw = sb(esB, "cw", [128, 6, 5], F32)
                    cb = sb(esB, "cb", [128, 6], F32)
                    dg = sb(esB, "dg", [128, 30, 128], BF16)
                    hc = [sb(esB, f"hc{i}", [128, T], BF16) for i in range(2)]
                    cst = [sb(esB, f"cst{i}", [128, 512], BF16) for i in range(2)]
                    P.dma("sp", lambda e: e.dma_start(out=cw[:], in_=ssm_conv_wT[l]), w=["cw"])
                    P.dma("sp", lambda e: e.dma_start(out=cb[:], in_=ssm_conv_bT[l]), w=["cb"])
                    for ch in range(6):
                        for jt in range(5):
                            P.dve(lambda e, ch=ch, jt=jt: e.tensor_scalar(out=dg[:, ch * 5 + jt, :], in0=identf[:], scalar1=cw[:, ch, jt:jt + 1],
                                                                          scalar2=None, op0=ALU.mult), r=["identf", "cw"], w=[("dg", ch)])
                    cnt = 0
                    for ch in range(6):
                        hs_ = ch % 2
                        P.dma("sp", lambda e, hs_=hs_, ch=ch: e.dma_start(out=hc[hs_][:], in_=hT[ch]),
                              r=[("hT", ch, gi) for gi in range(9)], w=[("hc", hs_)])
                        for gidx, (t0, nt) in enumerate(GROUPS):
                            N = nt * 128
                            tok0 = t0 * 128
                            seg_lo, seg_hi = (0, 256) if gidx == 0 else (256, T)
                            bk = cnt % 2
                            cnt += 1
                            taps = [2, 0, 1, 3, 4]
                            for ii, jt in enumerate(taps):
                                o = jt - 2
                                a_ = max(0, seg_lo - tok0 - o)
                                b_ = min(N, seg_hi - tok0 - o)
                                P.pe(lambda e, bk=bk, ch=ch, jt=jt, a_=a_, b_=b_, o=o, tok0=tok0, hs_=hs_, ii=ii: e.matmul(
                                    banks[bk][:, a_:b_], lhsT=dg[:, ch * 5 + jt, :], rhs=hc[hs_][:, tok0 + a_ + o:tok0 + b_ + o],
                                    start=(ii == 0), stop=(ii == 4)), r=[("dg", ch), ("hc", hs_)], w=[BK(bk)])
                            if ch < 4 or ch == 4:
                                dst = cst[bk][:, 0:N] if ch < 4 else BT[:, tok0:tok0 + N]
                                dkey = ("cst", bk) if ch < 4 else ("BT", gidx)
                                P.act(lambda e, bk=bk, dst=dst, ch=ch, N=N: e.activation(out=dst, in_=banks[bk][:, 0:N], func=AF.Silu,
                                                                                       bias=cb[:, ch:ch + 1], scale=1.0),
                                      r=[BK(bk), "cb"], w=[dkey])
                                if ch == 4:
                                    for g_ in range(2):
                                        pg = slice(64 * g_, 64 * g_ + 64)
                                        P.pool(lambda e, g_=g_, pg=pg, tok0=tok0, N=N: e.tensor_copy(out=BTm[g_][pg, tok0:tok0 + N], in_=BT[pg, tok0:tok0 + N]),
                                               r=[dkey, f"BTm{g_}z"], w=[("BTm", g_, gidx)])
                                for ti in range(nt):
                                    t = t0 + ti
                                    tb_ = 6 + (t % 2)
                                    src = (cst[bk][:, ti * 128:(ti + 1) * 128] if ch < 4 else BT[:, t * 128:(t + 1) * 128])
                                    P.pe(lambda e, tb_=tb_, src=src: e.transpose(out=tbanks[tb_][:, 0:128], in_=src, identity=identb[:]),
                                         r=[dkey, "identb"], w=[BK(tb_)])
                                    if ch < 4:
                                        P.dve(lambda e, tb_=tb_, t=t, ch=ch: e.tensor_copy(out=xs_tm[:, t, ch * 128:(ch + 1) * 128], in_=tbanks[tb_][:, 0:128]),
                                              r=[BK(tb_)], w=[("xs_tm", t, ch)])
                                    else:
                                        P.dve(lambda e, tb_=tb_, t=t: e.tensor_copy(out=B_tm[:, t, :], in_=tbanks[tb_][:, 0:128]),
                                              r=[BK(tb_)], w=[("B_tm", t)])
                            else:
                                P.act(lambda e, bk=bk, ch=ch, N=N, tok0=tok0: e.activation(out=CT[:, tok0:tok0 + N], in_=banks[bk][:, 0:N], func=AF.Silu,
                                                                                         bias=cb[:, ch:ch + 1], scale=1.0),
                                      r=[BK(bk), "cb"], w=[("CT", gidx)])
                P.barrier()
                if stop_after == "conv":
                    return
                with contextlib.ExitStack() as esC:
                    rhsU = [sb(esC, f"rhsU{i}", [128, 8, 128], F32) for i in range(2)]
                    rhsB = [sb(esC, f"rhsB{i}", [128, 8, 128], F32) for i in range(2)]
                    Erow = [sb(esC, f"Erow{i}", [128, 8, 128], F32) for i in range(2)]
                    dec = [sb(esC, f"dec{i}", [128, 8, 128], BF16) for i in range(2)]
                    MT = [sb(esC, f"MT{i}", [128, 8, 128], BF16) for i in range(2)]
                    xdt = [sb(esC, f"xdt{i}", [128, 8, 64], BF16) for i in range(2)]
                    xdtw = [sb(esC, f"xdtw{i}", [128, 8, 64], BF16) for i in range(2)]
                    CsT = [sb(esC, f"CsT{i}", [128, 2, 4, 128], BF16) for i in range(2)]
                    for i_ in range(2):
                        P.pool(lambda e, i_=i_: e.memset(CsT[i_][:], 0.0), w=[("CsT", i_, 0), ("CsT", i_, 1)])
                    wdec = [sb(esC, f"wdec{i}", [128, 8], F32) for i in range(2)]
                    dtw = [sb(esC, f"dtw{i}", [128, 8], F32) for i in range(2)]
                    XS = lambda t: [("xs_tm", t, ch) for ch in range(4)]
                    BTALL = [("BT", gi) for gi in range(9)]
                    CTALL = [("CT", gi) for gi in range(9)]
                    seen = set()

                    def sproc(d, t):
                        if True:
                            X0, X1, X2 = banks[3 * d], banks[3 * d + 1], banks[3 * d + 2]
                            K0, K1, K2 = BK(3 * d), BK(3 * d + 1), BK(3 * d + 2)
                            U_, nU_, NM_, S__ = (Uf, nUf, NMf, Sf) if d == 0 else (Ub, nUb, NMb, Sb_)
                            Uk, nUk, NMk, Sk = (("Uf", "nUf", NMf_keys, "Sf") if d == 0 else ("Ub", "nUb", NMb_keys, "Sb"))
                            lastc = 127 if d == 0 else 0
                            tsl = slice(t * 128, (t + 1) * 128)
                            dA = dA_all[:, t, d, :]
                            dtd = dt_all[:, t, d, :]
                            P.pool(lambda e, d=d, U_=U_, dA=dA: e.tensor_tensor(
                                out=rhsU[d][:], in0=U_[:].unsqueeze(1).to_broadcast([128, 8, 128]),
                                in1=dA.unsqueeze(2).to_broadcast([128, 8, 128]), op=ALU.mult),
                                r=[Uk, ("dA_all", d)], w=[("rhsU", d)])
                            P.pool(lambda e, d=d, dA=dA: e.tensor_copy(out=rhsB[d][:], in_=dA.unsqueeze(2).to_broadcast([128, 8, 128])),
                                   r=[("dA_all", d)], w=[("rhsB", d)])
                            for hb in range(2):
                                hsl = slice(4 * hb, 4 * hb + 4)
                                P.pe(lambda e, d=d, hb=hb, hsl=hsl: e.matmul(X0[:], lhsT=onesf[:], rhs=rhsU[d][:, hsl, :].rearrange("p r l -> p (r l)"),
                                                                             start=True, stop=True), r=["onesf", ("rhsU", d)], w=[K0])
                                P.act(lambda e, d=d, hb=hb, hsl=hsl: e.activation(out=Erow[d][:, hsl, :].rearrange("p r l -> p (r l)"), in_=X0[:], func=AF.Exp),
                                      r=[K0], w=[("Erow", d, hb)])
                                P.pe(lambda e, d=d, hb=hb, hsl=hsl, nU_=nU_: e.matmul(X0[:], lhsT=nU_[:], rhs=rhsB[d][:, hsl, :].rearrange("p r l -> p (r l)"),
                                                                                     start=False, stop=False, skip_group_check=True), r=[nUk, ("rhsB", d), ("Erow", d, hb)], w=[K0])
                                P.pe(lambda e, hb=hb, NM_=NM_: e.matmul(X0[:], lhsT=identf[:], rhs=NM_[:].rearrange("p r l -> p (r l)"),
                                                                       start=False, stop=True, skip_group_check=True), r=["identf"] + NMk, w=[K0])
                                P.act(lambda e, d=d, hb=hb, hsl=hsl: e.activation(out=dec[d][:, hsl, :].rearrange("p r l -> p (r l)"), in_=X0[:], func=AF.Exp),
                                      r=[K0], w=[("dec", d, hb)])
                            yield
                            for g in range(2):
                                P.pe(lambda e, g=g, tsl=tsl: e.matmul(X1[:, g * 128:(g + 1) * 128], lhsT=BTm[g][:, tsl],
                                                                      rhs=CT[:, tsl], start=True, stop=True),
                                     r=[("BTm", g, gi) for gi in range(9)] + CTALL, w=[K1])
                            for g in range(2):
                                P.dve(lambda e, d=d, g=g: e.tensor_tensor(
                                    out=MT[d][:, 4 * g:4 * g + 4, :], in0=dec[d][:, 4 * g:4 * g + 4, :],
                                    in1=X1[:, g * 128:(g + 1) * 128].unsqueeze(1).to_broadcast([128, 4, 128]), op=ALU.mult),
                                    r=[("dec", d, g), K1], w=[("MT", d, g)])
                            P.pe(lambda e, S__=S__, dA=dA: e.matmul(X1[:, 256:264], lhsT=S__[:], rhs=dA, start=True, stop=True),
                                 r=[Sk, ("dA_all", d)], w=[K1])
                            P.act(lambda e, d=d: e.activation(out=wdec[d][:], in_=X1[:, 256:264], func=AF.Exp), r=[K1], w=[("wdec", d)])
                            P.dve(lambda e, d=d, dtd=dtd: e.tensor_tensor(out=dtw[d][:], in0=wdec[d][:], in1=dtd, op=ALU.mult),
                                  r=[("wdec", d), ("dt_all", d)], w=[("dtw", d)])
                            xsv = xs_tm[:, t, :].rearrange("p (r x) -> p r x", x=64)
                            P.pool(lambda e, d=d, xsv=xsv, dtd=dtd: e.tensor_tensor(out=xdt[d][:], in0=xsv, in1=dtd.unsqueeze(2).to_broadcast([128, 8, 64]),
                                                                                   op=ALU.mult), r=XS(t) + [("dt_all", d)], w=[("xdt", d)])
                            P.dve(lambda e, d=d, xsv=xsv: e.tensor_tensor(out=xdtw[d][:], in0=xsv, in1=dtw[d][:].unsqueeze(2).to_broadcast([128, 8, 64]),
                                                                          op=ALU.mult), r=XS(t) + [("dtw", d)], w=[("xdtw", d)])
                            for g in range(2):
                                ps_ = slice(64 * g, 64 * g + 64)
                                P.pool(lambda e, d=d, g=g, ps_=ps_, tsl=tsl: e.tensor_tensor(
                                    out=CsT[d][ps_, g, :, :], in0=CT[ps_, tsl].unsqueeze(1).to_broadcast([64, 4, 128]),
                                    in1=Erow[d][ps_, 4 * g:4 * g + 4, :], op=ALU.mult),
                                    r=CTALL + [("Erow", d, g)], w=[("CsT", d, g)])
                            yield
                            for r8 in range(8):
                                g = r8 // 4
                                ps_ = slice(64 * g, 64 * g + 64)
                                ysl = slice(r8 * 64, (r8 + 1) * 64)
                                P.pe(lambda e, d=d, r8=r8, ysl=ysl: e.matmul(X2[:, ysl], lhsT=MT[d][:, r8, :], rhs=xdt[d][:, r8, :],
                                                                             start=True, stop=False),
                                     r=[("MT", d, g), ("xdt", d)], w=[K2])
                                P.pe(lambda e, d=d, r8=r8, ysl=ysl, g=g: e.matmul(X2[:, ysl], lhsT=CsT[d][:, g, r8 % 4, :], rhs=hSb[:, d, r8 % 4, :],
                                                                                 start=False, stop=True),
                                     r=[("CsT", d, g), ("hSb", d)], w=[K2])
                            if t not in seen:
                                seen.add(t)
                                P.act(lambda e, t=t: e.activation(out=Y[:, t, :], in_=X2[:], func=AF.Copy), r=[K2], w=[("Y", t)])
                            else:
                                P.dve(lambda e, t=t: e.tensor_tensor(out=Y[:, t, :], in0=X2[:], in1=Y[:, t, :], op=ALU.add),
                                      r=[K2, ("Y", t)], w=[("Y", t)])
                            yield
                            P.pe(lambda e, d=d, t=t: e.matmul(X1[:], lhsT=B_tm[:, t, :], rhs=xdtw[d][:].rearrange("p r x -> p (r x)"),
                                                              start=True, stop=True), r=[("B_tm", t), ("xdtw", d)], w=[K1])
                            for g in range(2):
                                ps_ = slice(64 * g, 64 * g + 64)
                                P.dve(lambda e, d=d, g=g, ps_=ps_, lastc=lastc: e.tensor_tensor(
                                    out=hS[ps_, d, :, :], in0=hS[ps_, d, :, :],
                                    in1=Erow[d][ps_, 4 * g:4 * g + 4, lastc:lastc + 1].to_broadcast([64, 4, 64]), op=ALU.mult),
                                    r=[("hS", d), ("Erow", d, g)], w=[("hS", d)])
                                P.dve(lambda e, d=d, g=g, ps_=ps_: e.tensor_tensor(
                                    out=hS[ps_, d, :, :], in0=hS[ps_, d, :, :],
                                    in1=X1[ps_, 256 * g:256 * g + 256].rearrange("p (r x) -> p r x", x=64), op=ALU.add),
                                    r=[("hS", d), K1], w=[("hS", d)])
                            P.pool(lambda e, d=d: e.tensor_copy(out=hSb[:, d, :, :], in_=hS[:, d, :, :]), r=[("hS", d)], w=[("hSb", d)])

                    def run_interleaved_s(fn):
                        for step in range(NT):
                            gens = [fn(d_, ORDER[d_][step]) for d_ in range(2)]
                            while gens:
                                for g_ in list(gens):
                                    try:
                                        next(g_)
                                    except StopIteration:
                                        gens.remove(g_)
                    run_interleaved_s(sproc)
                    zt_ = [sb(esC, f"zt_{i}", [128, 512], BF16) for i in range(2)]
                    yy = [sb(esC, f"yy{i}", [128, 512], F32) for i in range(2)]
                    ysq = sb(esC, "ysq", [128, 512], F32)
                    ssg = [sb(esC, f"ssg{i}", [128, 2], F32) for i in range(2)]
                    yo = [sb(esC, f"yo{i}", [128, 512], BF16) for i in range(2)]
                    for t in range(NT):
                        a = t % 2
                        rows = slice(t * 128, (t + 1) * 128)
                        P.dma("sp", lambda e, a=a, rows=rows: e.dma_start(out=zt_[a][:], in_=zs[rows, :]), r=[("zs", t)], w=[("zt_", a)])
                        P.dve(lambda e, a=a, t=t: e.tensor_tensor(out=yy[a][:].rearrange("p (r x) -> p r x", x=64),
                                                                  in0=xs_tm[:, t, :].rearrange("p (r x) -> p r x", x=64),
                                                                  in1=dsk[:].unsqueeze(2).to_broadcast([128, 8, 64]), op=ALU.mult),
                              r=XS(t) + ["dsk"], w=[("yy", a)])
                        P.pool(lambda e, a=a, t=t: e.tensor_tensor(out=yy[a][:], in0=yy[a][:], in1=Y[:, t, :], op=ALU.add),
                               r=[("yy", a), ("Y", t)], w=[("yy", a)])
                        P.dve(lambda e, a=a: e.tensor_tensor(out=yy[a][:], in0=yy[a][:], in1=zt_[a][:], op=ALU.mult),
                              r=[("yy", a), ("zt_", a)], w=[("yy", a)])
                        for g in range(2):
                            P.act(lambda e, a=a, g=g: e.activation(out=ysq[:, g * 256:(g + 1) * 256], in_=yy[a][:, g * 256:(g + 1) * 256],
                                                                   func=AF.Square, accum_out=ssg[a][:, g:g + 1]),
                                  r=[("yy", a)], w=["ysq", ("ssg", a)])
                        rsqrt_ops(ssg[a][:], ssg[a][:], 1.0 / 256, [("ssg", a)], ("ssg", a))
                        P.dve(lambda e, a=a: e.tensor_tensor(out=yy[a][:].rearrange("p (g x) -> p g x", x=256),
                                                             in0=yy[a][:].rearrange("p (g x) -> p g x", x=256),
                                                             in1=ssg[a][:].unsqueeze(2).to_broadcast([128, 2, 256]), op=ALU.mult),
                              r=[("yy", a), ("ssg", a)], w=[("yy", a)])
                        P.pool(lambda e, a=a: e.tensor_tensor(out=yo[a][:], in0=yy[a][:], in1=nw[:], op=ALU.mult),
                               r=[("yy", a), "snw"], w=[("yo", a)])
                        P.dma("pool", lambda e, a=a, rows=rows: e.dma_start(out=ml[rows, 256:768], in_=yo[a][:]), r=[("yo", a)], w=[("ml_s", t)])
            P.barrier()

        def phase_gdn(l):
            with contextlib.ExitStack() as esA:
                qT = sb(esA, "qT", [128, 2, T], BF16)
                kTm = [sb(esA, f"kTm{i}", [128, 2, T], BF16) for i in range(2)]
                P.pool(lambda e: e.memset(kTm[0][64:128, :, :], 0.0), w=["kTm0z"])
                P.pool(lambda e: e.memset(kTm[1][0:64, :, :], 0.0), w=["kTm1z"])
                k_tm = sb(esA, "k_tm", [128, NT, 256], BF16)
                v_tm = sb(esA, "v_tm", [128, NT, 256], BF16)
                O = sb(esA, "O", [128, NT, 256], BF16)
                beta_all = sb(esA, "beta_all", [128, NT, 8], F32)
                g_all = sb(esA, "g_all", [128, NT, 8], F32)
                S = sb(esA, "S", [128, 2, 2, 64], F32)
                Sb = sb(esA, "Sbf", [128, 2, 2, 2, 64], BF16)
                gdtb = bcast_load(esA, "gdtb", gdn_dt_bias[l].rearrange("a b -> (a b)"), 8)
                galg = bcast_load(esA, "galg", gdn_a_log[l].rearrange("a b -> (a b)"), 8)
                gnw = bcast_load(esA, "gnw", gdn_norm_w[l], 64)
                SM = [("small", t) for t in range(NT)]
                P.act(lambda e: e.activation(out=galg[:], in_=galg[:], func=AF.Exp), r=["galg"], w=["galg"])
                P.dve(lambda e: e.tensor_scalar(out=galg[:], in0=galg[:], scalar1=-1.0, scalar2=None, op0=ALU.mult), r=["galg"], w=["galg"])
                P.act(lambda e: e.activation(out=beta_all[:], in_=small_all[:, :, 8:16], func=AF.Sigmoid), r=SM, w=["beta_all"])
                P.dve(lambda e: e.tensor_tensor(out=g_all[:], in0=small_all[:, :, 16:24], in1=gdtb[:].unsqueeze(1).to_broadcast([128, NT, 8]),
                                                op=ALU.add), r=SM + ["gdtb"], w=["g_all"])
                P.act(lambda e: e.activation(out=g_all[:], in_=g_all[:], func=AF.Exp), r=["g_all"], w=["g_all"])
                P.act(lambda e: e.activation(out=g_all[:], in_=g_all[:], func=AF.Ln, bias=onesf[:, 0:1], scale=1.0), r=["g_all", "onesf"], w=["g_all"])
                P.dve(lambda e: e.tensor_tensor(out=g_all[:], in0=g_all[:], in1=galg[:].unsqueeze(1).to_broadcast([128, NT, 8]), op=ALU.mult),
                      r=["g_all", "galg"], w=["g_all"])
                P.pool(lambda e: e.memset(S[:], 0.0), w=[("S", 0), ("S", 1)])
                P.pool(lambda e: e.memset(Sb[:], 0.0), w=[("Sb", 0), ("Sb", 1)])
                with contextlib.ExitStack() as esB:
                    cw = sb(esB, "gcw", [128, 6, 5], F32)
                    dg = sb(esB, "gdg", [128, 30, 128], BF16)
                    hc = [sb(esB, f"ghc{i}", [128, T], BF16) for i in range(2)]
                    cst = [sb(esB, f"gcst{i}", [128, 512], BF16) for i in range(2)]
                    xf = [sb(esB, f"gxf{i}", [128, 512], F32) for i in range(2)]
                    sq = [sb(esB, f"gsq{i}", [128, 512], F32) for i in range(2)]
                    rs = [sb(esB, f"grs{i}", [128, 512], F32) for i in range(2)]
                    xnst = [sb(esB, f"gxnst{i}", [128, 512], BF16) for i in range(2)]
                    P.dma("sp", lambda e: e.dma_start(out=cw[:], in_=gdn_conv_wT[l]), w=["gcw"])
                    for ch in range(6):
                        for jt in range(5):
                            P.dve(lambda e, ch=ch, jt=jt: e.tensor_scalar(out=dg[:, ch * 5 + jt, :], in0=identf[:], scalar1=cw[:, ch, jt:jt + 1],
                                                                          scalar2=None, op0=ALU.mult), r=["identf", "gcw"], w=[("gdg", ch)])
                    cnt = 0
                    for ch in range(6):
                        hs_ = ch % 2
                        P.dma("sp", lambda e, hs_=hs_, ch=ch: e.dma_start(out=hc[hs_][:], in_=hT[6 + ch]),
                              r=[("hT", 6 + ch, gi) for gi in range(9)], w=[("ghc", hs_)])
                        for gidx, (t0, nt) in enumerate(GROUPS):
                            N = nt * 128
                            tok0 = t0 * 128
                            seg_lo, seg_hi = (0, 256) if gidx == 0 else (256, T)
                            bk = cnt % 2
                            cnt += 1
                            for ii, jt in enumerate([2, 0, 1, 3, 4]):
                                o = jt - 2
                                a_ = max(0, seg_lo - tok0 - o)
                                b_ = min(N, seg_hi - tok0 - o)
                                P.pe(lambda e, bk=bk, ch=ch, jt=jt, a_=a_, b_=b_, o=o, tok0=tok0, hs_=hs_, ii=ii: e.matmul(
                                    banks[bk][:, a_:b_], lhsT=dg[:, ch * 5 + jt, :], rhs=hc[hs_][:, tok0 + a_ + o:tok0 + b_ + o],
                                    start=(ii == 0), stop=(ii == 4)), r=[("gdg", ch), ("ghc", hs_)], w=[BK(bk)])
                            if ch < 4:
                                P.act(lambda e, bk=bk, N=N: e.activation(out=xf[bk][:, 0:N], in_=banks[bk][:, 0:N], func=AF.Silu),
                                      r=[BK(bk)], w=[("gxf", bk)])
                                P.pool(lambda e, bk=bk, N=N: e.tensor_tensor(out=sq[bk][:, 0:N], in0=xf[bk][:, 0:N], in1=xf[bk][:, 0:N], op=ALU.mult),
                                       r=[("gxf", bk)], w=[("gsq", bk)])
                                b2 = 2 + bk
                                P.pe(lambda e, bk=bk, b2=b2, N=N: e.matmul(banks[b2][:, 0:N], lhsT=blkf[:], rhs=sq[bk][:, 0:N], start=True, stop=True),
                                     r=["blkf", ("gsq", bk)], w=[BK(b2)])
                                P.dve(lambda e, bk=bk, b2=b2, N=N: e.tensor_scalar(out=rs[bk][:, 0:N], in0=banks[b2][:, 0:N], scalar1=EPS, scalar2=None,
                                                                                  op0=ALU.add), r=[BK(b2)], w=[("grs", bk)])
                                P.act(lambda e, bk=bk, N=N: e.activation(out=rs[bk][:, 0:N], in_=rs[bk][:, 0:N], func=AF.Sqrt), r=[("grs", bk)], w=[("grs", bk)])
                                P.dve(lambda e, bk=bk, N=N: e.reciprocal(out=rs[bk][:, 0:N], in_=rs[bk][:, 0:N]), r=[("grs", bk)], w=[("grs", bk)])
                                dstT = (qT[:, ch, tok0:tok0 + N] if ch < 2 else xnst[bk][:, 0:N])
                                dkey = ("qT", ch, gidx) if ch < 2 else ("gxnst", bk)
                                sc_ = 0.125 if ch < 2 else 1.0
                                P.dve(lambda e, bk=bk, N=N, dstT=dstT, sc_=sc_: e.scalar_tensor_tensor(
                                    out=dstT, in0=xf[bk][:, 0:N], scalar=sc_, in1=rs[bk][:, 0:N], op0=ALU.mult, op1=ALU.mult),
                                    r=[("gxf", bk), ("grs", bk)], w=[dkey])
                                if ch >= 2:
                                    for g_ in range(2):
                                        pg = slice(64 * g_, 64 * g_ + 64)
                                        P.pool(lambda e, g_=g_, pg=pg, bk=bk, ch=ch, tok0=tok0, N=N: e.tensor_copy(
                                            out=kTm[g_][pg, ch - 2, tok0:tok0 + N], in_=xnst[bk][pg, 0:N]),
                                            r=[dkey, f"kTm{g_}z"], w=[("kTm", g_, ch - 2, gidx)])
                                    for ti in range(nt):
                                        t = t0 + ti
                                        tb_ = 6 + (t % 2)
                                        P.pe(lambda e, tb_=tb_, ti=ti, bk=bk: e.transpose(out=tbanks[tb_][:, 0:128], in_=xnst[bk][:, ti * 128:(ti + 1) * 128],
                                                                                         identity=identb[:]), r=[dkey, "identb"], w=[BK(tb_)])
                                        P.dve(lambda e, tb_=tb_, t=t, ch=ch: e.tensor_copy(out=k_tm[:, t, (ch - 2) * 128:(ch - 1) * 128], in_=tbanks[tb_][:, 0:128]),
                                              r=[BK(tb_)], w=[("k_tm", t, ch - 2)])
                            else:
                                P.act(lambda e, bk=bk, N=N: e.activation(out=cst[bk][:, 0:N], in_=banks[bk][:, 0:N], func=AF.Silu),
                                      r=[BK(bk)], w=[("gcst", bk)])
                                for ti in range(nt):
                                    t = t0 + ti
                                    tb_ = 6 + (t % 2)
                                    P.pe(lambda e, tb_=tb_, bk=bk, ti=ti: e.transpose(out=tbanks[tb_][:, 0:128], in_=cst[bk][:, ti * 128:(ti + 1) * 128],
                                                                                     identity=identb[:]), r=[("gcst", bk), "identb"], w=[BK(tb_)])
                                    P.dve(lambda e, tb_=tb_, t=t, ch=ch: e.tensor_copy(out=v_tm[:, t, (ch - 4) * 128:(ch - 3) * 128], in_=tbanks[tb_][:, 0:128]),
                                          r=[BK(tb_)], w=[("v_tm", t, ch - 4)])
                P.barrier()
                if stop_after == "gconv":
                    return
                with contextlib.ExitStack() as esC:
                    def mk(name, shape, dt):
                        return [sb(esC, f"{name}{i}", shape, dt) for i in range(2)]
                    rhs1 = mk("g_rhs1", [128, 4, 128], F32)
                    rhs2 = mk("g_rhs2", [128, 4, 128], F32)
                    gmask = mk("g_gmask", [128, 4, 2], F32)
                    decp = mk("g_decp", [128, 4, 128], F32)
                    esm = mk("g_esm", [128, 16], F32)
                    tmpA = rhs1
                    nbo = rhs2
                    nb4 = mk("g_nb4", [128, 4], F32)
                    beg = mk("g_beg", [128, 4], F32)
                    Nm = [mk("g_Nm_a", [128, 4, 128], BF16)]
                    Ym = [mk("g_Ym_a", [128, 4, 128], BF16)]
                    Wm = [mk("g_Wm_a", [128, 4, 128], BF16), mk("g_Wm_b", [128, 4, 128], BF16)]
                    Tm = [mk("g_Tm_a", [128, 4, 128], BF16), mk("g_Tm_b", [128, 4, 128], BF16)]
                    NMc = mk("g_NMc", [128, 4, 128], BF16)
                    YMc = mk("g_YMc", [128, 4, 128], BF16)
                    P1s = mk("g_P1s", [128, 4, 128], BF16)
                    P2s = mk("g_P2s", [128, 4, 128], BF16)
                    M4 = [sb(esC, f"g_M4_{j}", [128, 4, 128], BF16) for j in range(6)]
                    I4 = sb(esC, "g_I4", [128, 4, 128], BF16)
                    blk_a = sb(esC, "g_blka", [128, 128], F32)
                    blk_b = sb(esC, "g_blkb", [128, 128], F32)
                    mtmp = sb(esC, "g_mtmp", [128, 128], F32)

                    def mk_blk(t_, n):
                        v_ = t_[:].rearrange("p (a b) -> p a b", b=n)
                        P.pool(lambda e: e.memset(t_[:], 1.0), w=[id(t_)])
                        P.pool(lambda e: e.affine_select(out=v_, in_=v_, compare_op=ALU.is_ge, fill=0.0, base=0,
                                                         pattern=[[-n, 128 // n], [0, n]], channel_multiplier=1), r=[id(t_)], w=[id(t_)])
                        P.pool(lambda e: e.affine_select(out=v_, in_=v_, compare_op=ALU.is_ge, fill=0.0, base=n - 1,
                                                         pattern=[[n, 128 // n], [0, n]], channel_multiplier=-1), r=[id(t_)], w=[id(t_)])
                    prev, cur = blk_a, blk_b
                    mk_blk(prev, 1)
                    for j in range(6):
                        mk_blk(cur, 2 << j)
                        P.pool(lambda e, prev=prev, cur=cur: e.tensor_tensor(out=mtmp[:], in0=cur[:], in1=prev[:], op=ALU.subtract),
                               r=[id(prev), id(cur)], w=["g_mtmp"])
                        for h in range(4):
                            P.pool(lambda e, j=j, h=h: e.tensor_copy(out=M4[j][:, h, :], in_=mtmp[:]), r=["g_mtmp"], w=["M4"])
                        prev, cur = cur, prev
                    for h in range(4):
                        P.pool(lambda e, h=h: e.tensor_copy(out=I4[:, h, :], in_=identb[:]), r=["identb"], w=["I4"])
                    QKm = mk("g_QKm", [128, 4, 128], BF16)
                    QKmT = mk("g_QKmT", [128, 4, 128], BF16)
                    kbg = mk("g_kbg", [128, 4, 64], BF16)
                    kend = mk("g_kend", [128, 4, 64], BF16)
                    vb = mk("g_vb", [128, 4, 64], BF16)
                    u_sb = mk("g_u", [128, 4, 64], F32)
                    wT_sb = mk("g_wT", [128, 2, 128], BF16)
                    vnewc = [mk("g_vnew_a", [128, 4, 64], BF16), mk("g_vnew_b", [128, 4, 64], BF16)]
                    for ci_ in range(2):
                        for d_ in range(2):
                            P.pool(lambda e, ci_=ci_, d_=d_: e.memset(vnewc[ci_][d_][:], 0.0), w=[("vnewc", ci_, d_)])
                    o1 = mk("g_o1", [128, 4, 64], F32)
                    QT_ALL = [("qT", c, gi) for c in range(2) for gi in range(9)]
                    KT_ALL = [("kTm", g_, c, gi) for g_ in range(2) for c in range(2) for gi in range(9)]
                    seen = set()
                    v4 = lambda bk: bk[:].rearrange("p (h s) -> p h s", s=128)

                    def gproc(d, t):
                        if True:
                            b0, b1, b2_ = banks[3 * d], banks[3 * d + 1], banks[3 * d + 2]
                            b3, b4, b5 = b0, b1, b2_
                            BKd = lambda i: BK(3 * d + (i % 3)) if i < 6 else BK(6 + d)
                            tsl = slice(t * 128, (t + 1) * 128)
                            UB_, nUB_, SB_, NMB_ = (UBf, nUBf, SBf, NMBf) if d == 0 else (UBb, nUBb, SBb, NMBb)
                            UBk, nUBk, SBk, NMBk = ("UBf", "nUBf", "SBf", NMBf_keys) if d == 0 else ("UBb", "nUBb", "SBb", NMBb_keys)
                            g4 = g_all[:, t, 4 * d:4 * d + 4]
                            bt4 = beta_all[:, t, 4 * d:4 * d + 4]
                            KD = lambda n: (n, d)
                            P.dve(lambda e, d=d, nUB_=nUB_, g4=g4: e.tensor_tensor(
                                out=rhs1[d][:], in0=nUB_[:].unsqueeze(1).to_broadcast([128, 4, 128]),
                                in1=g4.unsqueeze(2).to_broadcast([128, 4, 128]), op=ALU.mult), r=[nUBk, "g_all"], w=[KD("rhs1")])
                            P.dve(lambda e, d=d, g4=g4: e.tensor_copy(out=rhs2[d][:], in_=g4.unsqueeze(2).to_broadcast([128, 4, 128])),
                                  r=["g_all"], w=[KD("rhs2")])
                            P.dve(lambda e, d=d, g4=g4: e.tensor_tensor(
                                out=gmask[d][:], in0=g4.unsqueeze(2).to_broadcast([128, 4, 2]),
                                in1=chunkind[:].unsqueeze(1).to_broadcast([128, 4, 2]), op=ALU.mult), r=["g_all", "chunkind"], w=[KD("gmask")])
                            P.pe(lambda e, d=d: e.matmul(b0[:], lhsT=onesf[:], rhs=rhs1[d][:].rearrange("p h s -> p (h s)"), start=True, stop=False),
                                 r=["onesf", KD("rhs1")], w=[BKd(0)])
                            P.pe(lambda e, d=d, UB_=UB_: e.matmul(b0[:], lhsT=UB_[:], rhs=rhs2[d][:].rearrange("p h s -> p (h s)"), start=False, stop=False),
                                 r=[UBk, KD("rhs2")], w=[BKd(0)])
                            P.pe(lambda e, NMB_=NMB_: e.matmul(b0[:], lhsT=identf[:], rhs=NMB_[:].rearrange("p h s -> p (h s)"), start=False, stop=True),
                                 r=["identf"] + NMBk, w=[BKd(0)])
                            for h in range(4):
                                c, hh = h // 2, h % 2
                                hp = slice(64 * hh, 64 * hh + 64)
                                P.pe(lambda e, h=h, c=c, hh=hh, tsl=tsl: e.matmul(b1[:, h * 128:(h + 1) * 128], lhsT=kTm[hh][:, c, tsl], rhs=kTm[hh][:, c, tsl],
                                                                                 start=True, stop=True), r=KT_ALL, w=[BKd(1)])
                            for h in range(4):
                                c, hh = h // 2, h % 2
                                hp = slice(64 * hh, 64 * hh + 64)
                                P.pe(lambda e, h=h, c=c, hh=hh, tsl=tsl: e.matmul(b2_[:, h * 128:(h + 1) * 128], lhsT=qT[:, c, tsl], rhs=kTm[hh][:, c, tsl],
                                                                                 start=True, stop=True), r=KT_ALL + QT_ALL, w=[BKd(2)])
                            P.act(lambda e, d=d: e.activation(out=decp[d][:].rearrange("p h s -> p (h s)"), in_=b0[:], func=AF.Exp), r=[BKd(0)], w=[KD("decp")])
                            P.pe(lambda e, UB_=UB_, g4=g4: e.matmul(b3[:, 0:4], lhsT=UB_[:], rhs=g4, start=True, stop=True), r=[UBk, "g_all"], w=[BKd(3)])
                            P.pe(lambda e, SB_=SB_, g4=g4: e.matmul(b3[:, 4:8], lhsT=SB_[:], rhs=g4, start=True, stop=True), r=[SBk, "g_all"], w=[BKd(3)])
                            P.pe(lambda e, d=d: e.matmul(b3[:, 8:16], lhsT=onesf[:], rhs=gmask[d][:].rearrange("p h c -> p (h c)"), start=True, stop=True),
                                 r=["onesf", KD("gmask")], w=[BKd(3)])
                            P.act(lambda e, d=d: e.activation(out=esm[d][:], in_=b3[:, 0:16], func=AF.Exp), r=[BKd(3)], w=[KD("esm")])
                            P.dve(lambda e, d=d: e.tensor_tensor(out=tmpA[d][:], in0=v4(b1), in1=decp[d][:], op=ALU.mult), r=[BKd(1), KD("decp")], w=[KD("rhs1")])
                            P.dve(lambda e, d=d, bt4=bt4: e.tensor_scalar(out=nb4[d][:], in0=bt4, scalar1=-1.0, scalar2=None, op0=ALU.mult),
                                  r=["beta_all"], w=[KD("nb4")])
                            P.dve(lambda e, d=d: e.tensor_tensor(out=nbo[d][:], in0=offd[:].unsqueeze(1).to_broadcast([128, 4, 128]),
                                                                 in1=nb4[d][:].unsqueeze(2).to_broadcast([128, 4, 128]), op=ALU.mult),
                                  r=["offd", KD("nb4")], w=[KD("rhs2")])
                            P.dve(lambda e, d=d: e.tensor_tensor(out=Nm[0][d][:], in0=tmpA[d][:], in1=nbo[d][:], op=ALU.mult),
                                  r=[KD("rhs1"), KD("rhs2")], w=[("Nm", 0, d)])
                            P.dve(lambda e, d=d: e.tensor_tensor(out=QKm[d][:], in0=v4(b2_), in1=decp[d][:], op=ALU.mult), r=[BKd(2), KD("decp")], w=[KD("QKm")])
                            P.dve(lambda e, d=d, bt4=bt4: e.tensor_tensor(out=beg[d][:], in0=bt4, in1=esm[d][:, 0:4], op=ALU.mult),
                                  r=["beta_all", KD("esm")], w=[KD("beg")])
                            ktv = k_tm[:, t, :].rearrange("p (h x) -> p h x", x=64)
                            vtv = v_tm[:, t, :].rearrange("p (h x) -> p h x", x=64)
                            KTM = [("k_tm", t, 0), ("k_tm", t, 1)]
                            VTM = [("v_tm", t, 0), ("v_tm", t, 1)]
                            P.dve(lambda e, d=d, ktv=ktv: e.tensor_tensor(out=kbg[d][:], in0=ktv, in1=beg[d][:].unsqueeze(2).to_broadcast([128, 4, 64]),
                                                                          op=ALU.mult), r=KTM + [KD("beg")], w=[KD("kbg")])
                            P.dve(lambda e, d=d, ktv=ktv: e.tensor_tensor(out=kend[d][:], in0=ktv, in1=esm[d][:, 4:8].unsqueeze(2).to_broadcast([128, 4, 64]),
                                                                          op=ALU.mult), r=KTM + [KD("esm")], w=[KD("kend")])
                            P.dve(lambda e, d=d, vtv=vtv, bt4=bt4: e.tensor_tensor(out=vb[d][:], in0=vtv, in1=bt4.unsqueeze(2).to_broadcast([128, 4, 64]),
                                                                                  op=ALU.mult), r=VTM + ["beta_all"], w=[KD("vb")])
                            tv6 = tbanks[6 + d][:, 0:512].rearrange("p (h s) -> p h s", s=128)
                            tv7 = tbanks[6 + d][:, 512:1024].rearrange("p (h s) -> p h s", s=128)
                            for h in range(4):
                                P.pe(lambda e, d=d, h=h, tv6=tv6: e.transpose(out=tv6[:, h, :], in_=Nm[0][d][:, h, :], identity=identb[:]),
                                     r=[("Nm", 0, d), "identb"], w=[BKd(6)])
                            for h in range(4):
                                P.pe(lambda e, d=d, h=h, tv7=tv7: e.transpose(out=tv7[:, h, :], in_=QKm[d][:, h, :], identity=identb[:]),
                                     r=[KD("QKm"), "identb"], w=[BKd(7)])
                            P.act(lambda e, d=d, tv6=tv6: e.activation(out=Ym[0][d][:], in_=tv6, func=AF.Copy), r=[BKd(6)], w=[("Ym", 0, d)])
                            P.act(lambda e, d=d, tv7=tv7: e.activation(out=QKmT[d][:], in_=tv7, func=AF.Copy), r=[BKd(7)], w=[KD("QKmT")])
                            yield
                            P.dve(lambda e, d=d: e.tensor_tensor(out=NMc[d][:], in0=Nm[0][d][:], in1=M4[0][:], op=ALU.mult),
                                  r=[("Nm", 0, d), "M4"], w=[KD("NMc")])
                            P.pool(lambda e, d=d: e.tensor_tensor(out=YMc[d][:], in0=Ym[0][d][:], in1=M4[0][:], op=ALU.mult),
                                   r=[("Ym", 0, d), "M4"], w=[KD("YMc")])
                            P.dve(lambda e, d=d: e.tensor_tensor(out=Wm[1][d][:], in0=YMc[d][:], in1=I4[:], op=ALU.add), r=[KD("YMc"), "I4"], w=[("Wm", 1, d)])
                            P.pool(lambda e, d=d: e.tensor_tensor(out=Tm[1][d][:], in0=NMc[d][:], in1=I4[:], op=ALU.add), r=[KD("NMc"), "I4"], w=[("Tm", 1, d)])
                            for j in range(2, 7):
                                pp, pn = (j - 1) % 2, j % 2
                                lastj = (j == 6)
                                P.dve(lambda e, d=d, j=j: e.tensor_tensor(out=NMc[d][:], in0=Nm[0][d][:], in1=M4[j - 1][:], op=ALU.mult),
                                      r=[("Nm", 0, d), "M4"], w=[KD("NMc")])
                                if not lastj:
                                    P.pool(lambda e, d=d, j=j: e.tensor_tensor(out=YMc[d][:], in0=Ym[0][d][:], in1=M4[j - 1][:], op=ALU.mult),
                                           r=[("Ym", 0, d), "M4"], w=[KD("YMc")])
                                for h in range(4):
                                    P.pe(lambda e, d=d, h=h, pp=pp: e.matmul(b0[:, h * 128:(h + 1) * 128], lhsT=NMc[d][:, h, :], rhs=Wm[pp][d][:, h, :],
                                                                            start=True, stop=True), r=[KD("NMc"), ("Wm", pp, d)], w=[BKd(0)])
                                P.act(lambda e, d=d: e.activation(out=P1s[d][:], in_=v4(b0), func=AF.Copy), r=[BKd(0)], w=[KD("P1s")])
                                if not lastj:
                                    for h in range(4):
                                        P.pe(lambda e, d=d, h=h, pp=pp: e.matmul(b1[:, h * 128:(h + 1) * 128], lhsT=YMc[d][:, h, :], rhs=Tm[pp][d][:, h, :],
                                                                                start=True, stop=True), r=[KD("YMc"), ("Tm", pp, d)], w=[BKd(1)])
                                    P.dve(lambda e, d=d: e.tensor_copy(out=P2s[d][:], in_=v4(b1)), r=[BKd(1)], w=[KD("P2s")])
                                for h in range(4):
                                    P.pe(lambda e, d=d, h=h, pp=pp: e.matmul(b2_[:, h * 128:(h + 1) * 128], lhsT=identb[:], rhs=Wm[pp][d][:, h, :],
                                                                            start=True, stop=False), r=["identb", ("Wm", pp, d)], w=[BKd(2)])
                                    P.pe(lambda e, d=d, h=h, pp=pp: e.matmul(b2_[:, h * 128:(h + 1) * 128], lhsT=Tm[pp][d][:, h, :], rhs=P1s[d][:, h, :],
                                                                            start=False, stop=True), r=[("Tm", pp, d), KD("P1s")], w=[BKd(2)])
                                P.act(lambda e, d=d, pn=pn: e.activation(out=Wm[pn][d][:], in_=v4(b2_), func=AF.Copy), r=[BKd(2)], w=[("Wm", pn, d)])
                                if not lastj:
                                    for h in range(4):
                                        P.pe(lambda e, d=d, h=h, pp=pp: e.matmul(b0[:, h * 128:(h + 1) * 128], lhsT=identb[:], rhs=Tm[pp][d][:, h, :],
                                                                                start=True, stop=False), r=["identb", ("Tm", pp, d)], w=[BKd(0)])
                                        P.pe(lambda e, d=d, h=h, pp=pp: e.matmul(b0[:, h * 128:(h + 1) * 128], lhsT=Wm[pp][d][:, h, :], rhs=P2s[d][:, h, :],
                                                                                start=False, stop=True), r=[("Wm", pp, d), KD("P2s")], w=[BKd(0)])
                                    P.dve(lambda e, d=d, pn=pn: e.tensor_copy(out=Tm[pn][d][:], in_=v4(b0)), r=[BKd(0)], w=[("Tm", pn, d)])
                                yield
                            TT = Wm[0][d]
                            TTk = ("Wm", 0, d)
                            yield
                            for h in range(4):
                                P.pe(lambda e, d=d, h=h, TT=TT: e.matmul(b3[:, 256 + h * 64:256 + (h + 1) * 64], lhsT=TT[:, h, :], rhs=vb[d][:, h, :],
                                                                        start=True, stop=True), r=[TTk, KD("vb")], w=[BKd(3)])
                            for h in range(4):
                                c = h // 2
                                P.pe(lambda e, d=d, h=h, c=c, TT=TT: e.matmul(
                                    b4[:, h * 128:(h + 1) * 128], lhsT=kbg[d][:, 2 * c:2 * c + 2, :].rearrange("p a x -> p (a x)"), rhs=TT[:, h, :],
                                    start=True, stop=True), r=[TTk, KD("kbg")], w=[BKd(4)])
                            P.act(lambda e, d=d: e.activation(out=u_sb[d][:].rearrange("p h x -> p (h x)"), in_=b3[:, 256:512], func=AF.Copy),
                                  r=[BKd(3)], w=[KD("u")])
                            for hh in range(2):
                                hp = slice(64 * hh, 64 * hh + 64)
                                P.dve(lambda e, d=d, hh=hh, hp=hp: e.tensor_copy(out=wT_sb[d][hp, :, :], in_=v4(b4)[hp, hh::2, :]), r=[BKd(4)], w=[KD("wT")])
                            yield
                            for ci in ((0, 1) if d == 0 else (1, 0)):
                                rows = slice(64 * ci, 64 * ci + 64)
                                for h in range(4):
                                    c, hh = h // 2, h % 2
                                    hp = slice(64 * hh, 64 * hh + 64)
                                    P.pe(lambda e, d=d, h=h, c=c, hh=hh: e.matmul(b5[:, h * 64:(h + 1) * 64], lhsT=wT_sb[d][:, c, :], rhs=Sb[:, hh, d, c, :],
                                                                                 start=True, stop=True), r=[KD("wT"), ("Sb", d)], w=[BKd(5)])
                                for h in range(4):
                                    c, hh = h // 2, h % 2
                                    hp = slice(64 * hh, 64 * hh + 64)
                                    P.pe(lambda e, d=d, h=h, c=c, hh=hh, tsl=tsl: e.matmul(b5[:, 256 + h * 64:256 + (h + 1) * 64], lhsT=qT[:, c, tsl],
                                                                                          rhs=Sb[:, hh, d, c, :], start=True, stop=True),
                                         r=QT_ALL + [("Sb", d)], w=[BKd(5)])
                                P.dve(lambda e, d=d, rows=rows, ci=ci: e.tensor_tensor(
                                    out=vnewc[ci][d][rows, :, :], in0=u_sb[d][rows, :, :], in1=b5[rows, 0:256].rearrange("p (h x) -> p h x", x=64),
                                    op=ALU.subtract), r=[KD("u"), BKd(5)], w=[("vnewc", ci, d)])
                                P.dve(lambda e, d=d, rows=rows: e.tensor_tensor(
                                    out=o1[d][rows, :, :], in0=b5[rows, 256:512].rearrange("p (h x) -> p h x", x=64),
                                    in1=esm[d][rows, 0:4].unsqueeze(2).to_broadcast([64, 4, 64]), op=ALU.mult),
                                    r=[KD("esm"), BKd(5)], w=[KD("o1")])
                                for h in range(4):
                                    c = h // 2
                                    P.pe(lambda e, d=d, h=h, c=c, ci=ci: e.matmul(
                                        b4[:, h * 64:(h + 1) * 64], lhsT=kend[d][:, 2 * c:2 * c + 2, :].rearrange("p a x -> p (a x)"),
                                        rhs=vnewc[ci][d][:, h, :], start=True, stop=True), r=[KD("kend"), ("vnewc", ci, d)], w=[BKd(4)])
                                for hh in range(2):
                                    hp = slice(64 * hh, 64 * hh + 64)
                                    gv = esm[d][hp, 8:16].rearrange("p (h c) -> p h c", c=2)[:, hh::2, ci:ci + 1]
                                    P.dve(lambda e, d=d, hp=hp, gv=gv: e.tensor_tensor(out=S[hp, d, :, :], in0=S[hp, d, :, :],
                                                                                      in1=gv.to_broadcast([64, 2, 64]), op=ALU.mult),
                                          r=[("S", d), KD("esm")], w=[("S", d)])
                                    P.dve(lambda e, d=d, hp=hp, hh=hh: e.tensor_tensor(
                                        out=S[hp, d, :, :], in0=S[hp, d, :, :],
                                        in1=b4[hp, 0:256].rearrange("p (h x) -> p h x", x=64)[:, hh::2, :], op=ALU.add),
                                        r=[("S", d), BKd(4)], w=[("S", d)])
                                for hh in range(2):
                                    hp = slice(64 * hh, 64 * hh + 64)
                                    P.pool(lambda e, d=d, hh=hh, hp=hp: e.tensor_copy(out=Sb[hp, hh, d, :, :], in_=S[hp, d, :, :]), r=[("S", d)], w=[("Sb", d)])
                                yield
                            yield
                            for h in range(4):
                                for ci_ in range(2):
                                    P.pe(lambda e, d=d, h=h, ci_=ci_: e.matmul(b3[:, h * 64:(h + 1) * 64], lhsT=QKmT[d][:, h, :], rhs=vnewc[ci_][d][:, h, :],
                                                                              start=(ci_ == 0), stop=(ci_ == 1)),
                                         r=[KD("QKmT"), ("vnewc", 0, d), ("vnewc", 1, d)], w=[BKd(3)])
                            Ov = O[:, t, :]
                            if t not in seen:
                                seen.add(t)
                                P.dve(lambda e, d=d, Ov=Ov: e.tensor_tensor(out=Ov, in0=b3[:, 0:256], in1=o1[d][:].rearrange("p h x -> p (h x)"), op=ALU.add),
                                      r=[BKd(3), KD("o1")], w=[("O", t)])
                            else:
                                P.dve(lambda e, d=d: e.tensor_tensor(out=o1[d][:].rearrange("p h x -> p (h x)"), in0=b3[:, 0:256],
                                                                     in1=o1[d][:].rearrange("p h x -> p (h x)"), op=ALU.add),
                                      r=[BKd(3), KD("o1")], w=[KD("o1")])
                                P.dve(lambda e, d=d, Ov=Ov: e.tensor_tensor(out=Ov, in0=o1[d][:].rearrange("p h x -> p (h x)"), in1=Ov, op=ALU.add),
                                      r=[KD("o1"), ("O", t)], w=[("O", t)])

                    def run_interleaved(fn):
                        for step in range(NT):
                            gens = [fn(d_, ORDER[d_][step]) for d_ in range(2)]
                            while gens:
                                for g_ in list(gens):
                                    try:
                                        next(g_)
                                    except StopIteration:
                                        gens.remove(g_)
                    run_interleaved(gproc)
                    gt_ = mk("g_gt", [128, 256], BF16)
                    of = mk("g_of", [128, 4, 64], F32)
                    osq = sb(esC, "g_osq", [128, 4, 64], F32)
                    oss = mk("g_oss", [128, 4], F32)
                    oo = mk("g_oo", [128, 256], BF16)
                    for t in range(NT):
                        a = t % 2
                        rows = slice(t * 128, (t + 1) * 128)
                        P.dma("sp", lambda e, a=a, rows=rows: e.dma_start(out=gt_[a][:], in_=gs[rows, :]), r=[("gs", t)], w=[("g_gt", a)])
                        P.pool(lambda e, a=a, t=t: e.tensor_tensor(out=osq[:].rearrange("p h x -> p (h x)"), in0=O[:, t, :], in1=O[:, t, :], op=ALU.mult),
                               r=[("O", t)], w=["g_osq"])
                        P.dve(lambda e, a=a: e.tensor_reduce(out=oss[a][:], in_=osq[:], axis=AX.X, op=ALU.add), r=["g_osq"], w=[("g_oss", a)])
                        rsqrt_ops(oss[a][:], oss[a][:], 1.0 / 64, [("g_oss", a)], ("g_oss", a))
                        P.dve(lambda e, a=a, t=t: e.tensor_tensor(out=of[a][:], in0=O[:, t, :].rearrange("p (h x) -> p h x", x=64),
                                                                  in1=oss[a][:].unsqueeze(2).to_broadcast([128, 4, 64]), op=ALU.mult),
                              r=[("O", t), ("g_oss", a)], w=[("g_of", a)])
                        P.dve(lambda e, a=a: e.tensor_tensor(out=of[a][:], in0=of[a][:], in1=gnw[:].unsqueeze(1).to_broadcast([128, 4, 64]), op=ALU.mult),
                              r=[("g_of", a), "gnw"], w=[("g_of", a)])
                        P.pool(lambda e, a=a: e.tensor_tensor(out=oo[a][:], in0=of[a][:].rearrange("p h x -> p (h x)"), in1=gt_[a][:], op=ALU.mult),
                               r=[("g_of", a), ("g_gt", a)], w=[("g_oo", a)])
                        P.dma("pool", lambda e, a=a, rows=rows: e.dma_start(out=ml[rows, 768:1024], in_=oo[a][:]), r=[("g_oo", a)], w=[("ml_g", t)])
            P.barrier()

        HALVES = [[(0, 2)] + [(2 + 4 * i, 4) for i in range(4)], [(18 + 4 * i, 4) for i in range(4)]]

        def phase_out_moe(l, stream_in, stream_out, last):
            with contextlib.ExitStack() as esA:
                G = sb(esA, "G", [128, NT, 32], F32)
                rb = bcast_load(esA, "rb", router_b[l], 36)
                def do_half(hi, groups):
                    tiles = [t0 + i for (t0, nt) in groups for i in range(nt)]
                    tb = tiles[0]
                    nth = len(tiles)
                    with contextlib.ExitStack() as esH:
                        flT = sb(esH, "flT", [128, 8, nth * 128], BF16)
                        with contextlib.ExitStack() as esB:
                            wob = sb(esB, "wob", [128, 8, D], BF16)
                            wst = [sb(esB, f"wost{i}", [128, D], F32) for i in range(2)]
                            rw = sb(esB, "rw", [128, 8, 36], F32)
                            mlt = [sb(esB, f"mlt{i}", [128, D], BF16) for i in range(2)]
                            mlT = [sb(esB, f"mlT{i}", [128, 8, 128], BF16) for i in range(2)]
                            xt = [sb(esB, f"xto{i}", [128, D], F32) for i in range(2)]
                            x1 = [sb(esB, f"x1{i}", [128, D], F32) for i in range(2)]
                            junk = sb(esB, "junko", [128, D], BF16)
                            ss = [sb(esB, f"sso{i}", [128, 1], F32) for i in range(2)]
                            xn = [sb(esB, f"xno{i}", [128, D], F32) for i in range(2)]
                            fl32 = [sb(esB, f"fl32{i}", [128, 8, 128], F32) for i in range(2)]
                            rl = sb(esB, "rl", [128, 36], F32)
                            rt = sb(esB, "rt", [128, 64], F32)
                            tmp48 = sb(esB, "tmp48", [128, 4, 8], F32)
                            for k in range(8):
                                s_ = k % 2
                                P.dma("sp", lambda e, s_=s_, k=k: e.dma_start(out=wst[s_][:], in_=w_out[l][k * 128:(k + 1) * 128, :]),
                                      w=[("wost", s_)])
                                P.pool(lambda e, s_=s_, k=k: e.tensor_copy(out=wob[:, k, :], in_=wst[s_][:]),
                                       r=[("wost", s_)], w=[("wob", k)])
                            WOB = [("wob", k) for k in range(8)]
                            P.dma("sp", lambda e: e.dma_start(out=rw[:], in_=router_w[l].rearrange("(k p) n -> p k n", p=128)), w=["rw"])
                            for t in tiles:
                                j = 1 if t < NCTX_T else 0
                                a = t % 2
                                tl = t - tb
                                rows = slice(t * 128, (t + 1) * 128)
                                P.dma("sp", lambda e, a=a, rows=rows: e.dma_start(out=mlt[a][:], in_=ml[rows, :]),
                                      r=[("ml_a", t), ("ml_s", t), ("ml_g", t)], w=[("mlt", a)])
                                P.dma("sp", lambda e, a=a, rows=rows: e.dma_start(out=xt[a][:], in_=stream_in[rows, :]), w=[("xto", a)])
                                bk = 6 + a
                                ptv = tbanks[bk][:].rearrange("p (k n) -> p k n", n=128)
                                for k in range(8):
                                    P.pe(lambda e, a=a, k=k, ptv=ptv: e.transpose(out=ptv[:, k, :], in_=mlt[a][:, k * 128:(k + 1) * 128],
                                                                              identity=identb[:]),
                                         r=[("mlt", a), "identb"], w=[BK(bk)])
                                P.act(lambda e, a=a, ptv=ptv: e.activation(out=mlT[a][:], in_=ptv, func=AF.Copy), r=[BK(bk)], w=[("mlT", a)])
                                for c2 in range(2):
                                    for k in range(8):
                                        P.pe(lambda e, a=a, k=k, c2=c2: e.matmul(
                                            banks[c2][:], lhsT=mlT[a][:, k, :], rhs=wob[:, k, c2 * 512:(c2 + 1) * 512],
                                            start=(k == 0), stop=(k == 7)), r=[("mlT", a)] + WOB, w=[BK(c2)])
                                    cs = slice(c2 * 512, (c2 + 1) * 512)
                                    P.dve(lambda e, a=a, c2=c2, cs=cs, j=j: e.tensor_tensor(
                                        out=x1[a][:, cs], in0=banks[c2][:], in1=g1row[:, j, c2 * 4:(c2 + 1) * 4, :].rearrange("p k n -> p (k n)"),
                                        op=ALU.mult), r=[BK(c2), "g1row"], w=[("x1", a, c2)])
                                    P.pool(lambda e, a=a, cs=cs: e.tensor_tensor(out=x1[a][:, cs], in0=x1[a][:, cs], in1=xt[a][:, cs], op=ALU.add),
                                           r=[("x1", a, c2), ("xto", a)], w=[("x1", a, c2)])
                                X1K = [("x1", a, 0), ("x1", a, 1)]
                                P.dma("pool", lambda e, a=a, rows=rows: e.dma_start(out=xs1[rows, :], in_=x1[a][:]), r=X1K, w=[("xs1", t)])
                                P.act(lambda e, a=a: e.activation(out=junk[:], in_=x1[a][:], func=AF.Square, accum_out=ss[a][:]),
                                      r=X1K, w=["junko", ("sso", a)])
                                rsqrt_ops(ss[a][:], ss[a][:], 1.0 / D, [("sso", a)], ("sso", a))
                                P.dve(lambda e, a=a: e.tensor_scalar(out=xn[a][:], in0=x1[a][:], scalar1=ss[a][:, 0:1], scalar2=None,
                                                                     op0=ALU.mult), r=X1K + [("sso", a)], w=[("xno", a)])
                                for hf in range(2):
                                    bkf = 2 + hf
                                    pf = banks[bkf][:].rearrange("p (k n) -> p k n", n=128)
                                    for kk in range(4):
                                        k = hf * 4 + kk
                                        P.pe(lambda e, a=a, k=k, kk=kk, pf=pf: e.transpose(out=pf[:, kk, :], in_=xn[a][:, k * 128:(k + 1) * 128],
                                                                                       identity=identf[:]),
                                             r=[("xno", a), "identf"], w=[BK(bkf)])
                                    for kk in range(4):
                                        k = hf * 4 + kk
                                        P.act(lambda e, a=a, k=k, kk=kk, pf=pf, j=j: e.activation(
                                            out=fl32[a][:, k, :], in_=pf[:, kk, :], func=AF.Identity,
                                            scale=s2[:, k, j:j + 1], bias=modT[:, 24 + k, j:j + 1]),
                                            r=[BK(bkf), "s2k", "modT"], w=[("fl32", a, k)])
                                FLK = [("fl32", a, k) for k in range(8)]
                                P.pool(lambda e, a=a, tl=tl: e.tensor_copy(out=flT[:, :, tl * 128:(tl + 1) * 128], in_=fl32[a][:]),
                                       r=FLK, w=[("flT", tl)])
                                for k in range(8):
                                    P.pe(lambda e, a=a, k=k: e.matmul(banks[4][:, 0:36], lhsT=fl32[a][:, k, :], rhs=rw[:, k, :],
                                                                      start=(k == 0), stop=(k == 7)), r=FLK + ["rw"], w=[BK(4)])
                                RT = "rt"
                                P.dve(lambda e: e.tensor_tensor(out=rl[:], in0=banks[4][:, 0:36], in1=rb[:], op=ALU.add), r=[BK(4), "rb"], w=["rl"])
                                gmax, ngmax, gsum, m1, m2, dd, e2, g1_, g2_ = [rt[:, i:i + 1] for i in range(9)]
                                ohg, ge = rt[:, 12:16], rt[:, 16:20]
                                sel, oh1, sel2, oh2, gate8 = [rt[:, 24 + 8 * i:32 + 8 * i] for i in range(5)]
                                P.dve(lambda e: e.tensor_reduce(out=gmax, in_=rl[:, 0:4], axis=AX.X, op=ALU.max), r=["rl"], w=[RT])
                                P.dve(lambda e: e.tensor_scalar(out=ohg, in0=rl[:, 0:4], scalar1=gmax, scalar2=None, op0=ALU.is_equal), r=["rl", RT], w=[RT])
                                P.dve(lambda e: e.tensor_scalar(out=ngmax, in0=gmax, scalar1=-1.0, scalar2=None, op0=ALU.mult), r=[RT], w=[RT])
                                P.act(lambda e: e.activation(out=ge, in_=rl[:, 0:4], func=AF.Exp, bias=ngmax, scale=1.0, accum_out=gsum), r=["rl", RT], w=[RT])
                                P.dve(lambda e: e.reciprocal(out=gsum, in_=gsum), r=[RT], w=[RT])
                                P.dve(lambda e: e.tensor_tensor(out=tmp48[:], in0=rl[:, 4:36].rearrange("p (g x) -> p g x", x=8),
                                                                in1=ohg.unsqueeze(2).to_broadcast([128, 4, 8]), op=ALU.mult), r=["rl", RT], w=["tmp48"])
                                P.dve(lambda e: e.tensor_reduce(out=sel, in_=tmp48[:].rearrange("p g x -> p x g"), axis=AX.X, op=ALU.add), r=["tmp48"], w=[RT])
                                P.dve(lambda e: e.tensor_reduce(out=m1, in_=sel, axis=AX.X, op=ALU.max), r=[RT], w=[RT])
                                P.dve(lambda e: e.tensor_scalar(out=oh1, in0=sel, scalar1=m1, scalar2=None, op0=ALU.is_equal), r=[RT], w=[RT])
                                P.dve(lambda e: e.scalar_tensor_tensor(out=sel2, in0=oh1, scalar=-1e30, in1=sel, op0=ALU.mult, op1=ALU.add), r=[RT], w=[RT])
                                P.dve(lambda e: e.tensor_reduce(out=m2, in_=sel2, axis=AX.X, op=ALU.max), r=[RT], w=[RT])
                                P.dve(lambda e: e.tensor_scalar(out=oh2, in0=sel2, scalar1=m2, scalar2=None, op0=ALU.is_equal), r=[RT], w=[RT])
                                P.dve(lambda e: e.tensor_tensor(out=dd, in0=m2, in1=m1, op=ALU.subtract), r=[RT], w=[RT])
                                P.act(lambda e: e.activation(out=e2, in_=dd, func=AF.Exp), r=[RT], w=[RT])
                                P.dve(lambda e: e.tensor_scalar(out=dd, in0=e2, scalar1=1.0, scalar2=None, op0=ALU.add), r=[RT], w=[RT])
                                P.dve(lambda e: e.reciprocal(out=dd, in_=dd), r=[RT], w=[RT])
                                P.dve(lambda e: e.tensor_tensor(out=g1_, in0=dd, in1=gsum, op=ALU.mult), r=[RT], w=[RT])
                                P.dve(lambda e: e.tensor_tensor(out=g2_, in0=g1_, in1=e2, op=ALU.mult), r=[RT], w=[RT])
                                P.dve(lambda e: e.tensor_scalar(out=gate8, in0=oh1, scalar1=g1_, scalar2=None, op0=ALU.mult), r=[RT], w=[RT])
                                P.dve(lambda e: e.scalar_tensor_tensor(out=gate8, in0=oh2, scalar=g2_, in1=gate8, op0=ALU.mult, op1=ALU.add), r=[RT], w=[RT])
                                P.dve(lambda e, t=t: e.tensor_tensor(out=G[:, t, :].rearrange("p (g x) -> p g x", x=8),
                                                                     in0=ohg.unsqueeze(2).to_broadcast([128, 4, 8]),
                                                                     in1=gate8.unsqueeze(1).to_broadcast([128, 4, 8]), op=ALU.mult),
                                      r=[RT], w=[("G", t)])
                        P.barrier()
                        yacc = sb(esH, "yacc", [128, nth, D], BF16)
                        with contextlib.ExitStack() as esC:
                            est = [sb(esC, f"est{i}", [128, 2048], F32) for i in range(3)]
                            wgb = [sb(esC, f"wgb{i}", [128, 8, 512], BF16) for i in range(2)]
                            wub = [sb(esC, f"wub{i}", [128, 8, 512], BF16) for i in range(2)]
                            wdb = [sb(esC, f"wdb{i}", [128, 4, D], BF16) for i in range(2)]
                            sg = [sb(esC, f"sg{i}", [128, 512], F32) for i in range(2)]
                            hh = [sb(esC, f"hh{i}", [128, 4, 512], BF16) for i in range(2)]
                            FLALL = [("flT", i) for i in range(nth)]
                            nst = 0
                            gi = 0
                            for ex in range(32):
                                ws = ex % 2
                                for (dst, src, key) in ((wgb, exp_w_gate, "wgb"), (wub, exp_w_up, "wub")):
                                    for pc in range(2):
                                        s_ = nst % 3
                                        nst += 1
                                        P.dma("sp", lambda e, s_=s_, src=src, pc=pc, ex=ex: e.dma_start(
                                            out=est[s_][:].rearrange("p (k n) -> p k n", n=512),
                                            in_=src[l, ex, pc * 512:(pc + 1) * 512, :].rearrange("(k p) n -> p k n", p=128)), w=[("est", s_)])
                                        P.pool(lambda e, s_=s_, dst=dst, pc=pc, ws=ws: e.tensor_copy(
                                            out=dst[ws][:, pc * 4:(pc + 1) * 4, :], in_=est[s_][:].rearrange("p (k n) -> p k n", n=512)),
                                            r=[("est", s_)], w=[(key, ws, pc)])
                                for pc in range(2):
                                    s_ = nst % 3
                                    nst += 1
                                    P.dma("sp", lambda e, s_=s_, pc=pc, ex=ex: e.dma_start(
                                        out=est[s_][:].rearrange("p (k n) -> p k n", n=D),
                                        in_=exp_w_down[l, ex, pc * 256:(pc + 1) * 256, :].rearrange("(k p) n -> p k n", p=128)), w=[("est", s_)])
                                    P.pool(lambda e, s_=s_, pc=pc, ws=ws: e.tensor_copy(
                                        out=wdb[ws][:, pc * 2:(pc + 1) * 2, :], in_=est[s_][:].rearrange("p (k n) -> p k n", n=D)),
                                        r=[("est", s_)], w=[("wdb", ws, pc)])
                                WG = [("wgb", ws, 0), ("wgb", ws, 1)]
                                WU = [("wub", ws, 0), ("wub", ws, 1)]
                                WD = [("wdb", ws, 0), ("wdb", ws, 1)]
                                for (t0, nt) in groups:
                                    N = nt * 128
                                    o0 = (t0 - tb) * 128
                                    hs = gi % 2
                                    gi += 1
                                    for jx in range(4):
                                        ba = (jx % 2) * 2
                                        for (bk_, wsrc, wk) in ((ba, wgb, WG), (ba + 1, wub, WU)):
                                            for k in range(8):
                                                P.pe(lambda e, bk_=bk_, wsrc=wsrc, k=k, jx=jx, o0=o0, N=N, ws=ws: e.matmul(
                                                    banks[bk_][:, 0:N], lhsT=wsrc[ws][:, k, jx * 128:(jx + 1) * 128], rhs=flT[:, k, o0:o0 + N],
                                                    start=(k == 0), stop=(k == 7)), r=wk + FLALL, w=[BK(bk_)])
                                        sgs = jx % 2
                                        P.act(lambda e, ba=ba, sgs=sgs, N=N: e.activation(out=sg[sgs][:, 0:N], in_=banks[ba][:, 0:N], func=AF.Silu),
                                              r=[BK(ba)], w=[("sg", sgs)])
                                        P.dve(lambda e, ba=ba, sgs=sgs, N=N, hs=hs, jx=jx: e.tensor_tensor(
                                            out=hh[hs][:, jx, 0:N], in0=sg[sgs][:, 0:N], in1=banks[ba + 1][:, 0:N], op=ALU.mult),
                                            r=[("sg", sgs), BK(ba + 1)], w=[("hh", hs, jx)])
                                    HK = [("hh", hs, jx) for jx in range(4)]
                                    for ti in range(nt):
                                        t = t0 + ti
                                        tl = t - tb
                                        for c2 in range(2):
                                            bk_ = 4 + c2
                                            for jx in range(4):
                                                P.pe(lambda e, bk_=bk_, jx=jx, hs=hs, ti=ti, c2=c2, ws=ws: e.matmul(
                                                    banks[bk_][:], lhsT=hh[hs][:, jx, ti * 128:(ti + 1) * 128], rhs=wdb[ws][:, jx, c2 * 512:(c2 + 1) * 512],
                                                    start=(jx == 0), stop=(jx == 3)), r=HK + WD, w=[BK(bk_)])
                                            ya = yacc[:, tl, c2 * 512:(c2 + 1) * 512]
                                            if ex == 0:
                                                P.dve(lambda e, bk_=bk_, ya=ya, t=t, ex=ex: e.tensor_scalar(
                                                    out=ya, in0=banks[bk_][:], scalar1=G[:, t, ex:ex + 1], scalar2=None, op0=ALU.mult),
                                                    r=[BK(bk_), ("G", t)], w=[("yacc", tl, c2)])
                                            else:
                                                P.dve(lambda e, bk_=bk_, ya=ya, t=t, ex=ex: e.scalar_tensor_tensor(
                                                    out=ya, in0=banks[bk_][:], scalar=G[:, t, ex:ex + 1], in1=ya, op0=ALU.mult, op1=ALU.add),
                                                    r=[BK(bk_), ("G", t), ("yacc", tl, c2)], w=[("yacc", tl, c2)])
                        P.barrier()
                        with contextlib.ExitStack() as esC:
                            xr = [sb(esC, f"xr{i}", [128, D], F32) for i in range(2)]
                            xo = [sb(esC, f"xo{i}", [128, D], F32) for i in range(2)]
                            for t in tiles:
                                if last and t < NCTX_T:
                                    continue
                                j = 1 if t < NCTX_T else 0
                                a = t % 2
                                tl = t - tb
                                rows = slice(t * 128, (t + 1) * 128)
                                P.dma("sp", lambda e, a=a, rows=rows: e.dma_start(out=xr[a][:], in_=xs1[rows, :]), r=[("xs1", t)], w=[("xr", a)])
                                P.dve(lambda e, a=a, tl=tl, j=j: e.tensor_tensor(
                                    out=xo[a][:], in0=yacc[:, tl, :], in1=g2row[:, j, :, :].rearrange("p k n -> p (k n)"), op=ALU.mult),
                                    r=[("yacc", tl, 0), ("yacc", tl, 1), "g2row"], w=[("xo", a)])
                                P.pool(lambda e, a=a: e.tensor_tensor(out=xo[a][:], in0=xo[a][:], in1=xr[a][:], op=ALU.add),
                                       r=[("xo", a), ("xr", a)], w=[("xo", a)])
                                if last:
                                    lt = t - NCTX_T
                                    P.dma("pool", lambda e, a=a, lt=lt: e.dma_start(out=y_out[lt * 128:(lt + 1) * 128, :], in_=xo[a][:]),
                                          r=[("xo", a)], w=[("yout", t)])
                                else:
                                    P.dma("pool", lambda e, a=a, rows=rows: e.dma_start(out=stream_out[rows, :], in_=xo[a][:]),
                                          r=[("xo", a)], w=[("sout", t)])
                        P.barrier()

                for hi_, groups_ in enumerate(HALVES):
                    do_half(hi_, groups_)

        cur_in = xin
        if zero_ml:
            with contextlib.ExitStack() as esZ:
                zt = sb(esZ, "zt", [128, 256], BF16)
                P.pool(lambda e: e.memset(zt[:], 0.0), w=["zt"])
                for t in range(NT):
                    P.dma("pool", lambda e, t=t: e.dma_start(out=ml[t * 128:(t + 1) * 128, 768:1024], in_=zt[:]), r=["zt"],
                          w=[("ml_g", t)])
            P.barrier()
        for l in range(NL):
            with contextlib.ExitStack() as esM:
                phase_mod(l, esM)
            P.barrier()
            if stop_after == "mod":
                break
            phase_proj_attn(l, cur_in)
            if stop_after in ("proj", "attn", "p1a", "p1b", "p1c", "p1d", "p1e", "p1f", "p1g"):
                break
            phase_ssm(l)
            if stop_after in ("conv", "ssm", "ssd1", "ssd2", "ssd3"):
                break
            phase_gdn(l)
            if stop_after in ("gconv", "gdn"):
                break
            phase_out_moe(l, cur_in, xs2, last=(l == NL - 1))
            cur_in = xs2

        P.emit()
    return nc


def _rope_tables():
    rows = 4096 // 64
    row = np.repeat(np.arange(rows, dtype=np.float32), 64)
    col = np.tile(np.arange(64, dtype=np.float32), rows)
    half = 16
    inv = (10000.0 ** (-np.arange(0, half, 2, dtype=np.float32) / half)).astype(np.float32)
    ang_r = row[:, None] * inv
    ang_c = col[:, None] * inv
    ang = np.concatenate([ang_r, ang_r, ang_c, ang_c], axis=-1).astype(np.float32)
    cos, sin = np.cos(ang), np.sin(ang)
    sgn = np.concatenate([-np.ones(8), np.ones(8), -np.ones(8), np.ones(8)]).astype(np.float32)
    tb = np.stack([cos, sin * sgn]).astype(np.float32)
    return np.ascontiguousarray(tb.reshape(2, 32, 128, 32).transpose(0, 2, 1, 3))


def _chunkT(v, n):
    return np.ascontiguousarray(np.swapaxes(v.reshape(v.shape[:-1] + (n, 128)), -1, -2))


def prep_inputs(inp, cores=range(8)):
    f = lambda a: np.ascontiguousarray(np.asarray(a, dtype=np.float32))
    shared = {
        "rope": _rope_tables(),
        "w_mod": f(inp["w_mod"]), "b_modT": _chunkT(f(inp["b_mod"]), 48),
        "n1T": _chunkT(f(inp["norm1_w"]), 8), "n2T": _chunkT(f(inp["norm2_w"]), 8),
        "w_in": f(inp["w_in"]), "w_out": f(inp["w_out"]),
        "ssm_conv_wT": np.ascontiguousarray(f(inp["ssm_conv_w"]).reshape(L_DEPTH, 5, 6, 128).transpose(0, 3, 2, 1)),
        "ssm_conv_bT": _chunkT(f(inp["ssm_conv_b"]), 6),
        "gdn_conv_wT": np.ascontiguousarray(f(inp["gdn_conv_w"]).reshape(L_DEPTH, 5, 6, 128).transpose(0, 3, 2, 1)),
        "router_w": np.ascontiguousarray(np.concatenate([f(inp["router_g_w"]), f(inp["router_e_w"])], axis=-1)),
        "router_b": np.ascontiguousarray(np.concatenate([f(inp["router_g_b"]), f(inp["router_e_b"])], axis=-1)),
    }
    for k in ("diff_qn_w", "diff_kn_w", "diff_lq1", "diff_lk1", "diff_lq2", "diff_lk2", "diff_norm_w",
              "ssm_dt_bias", "ssm_a_log", "ssm_d", "ssm_norm_w", "gdn_dt_bias", "gdn_a_log", "gdn_norm_w",
              "exp_w_gate", "exp_w_up", "exp_w_down"):
        shared[k] = f(inp[k])
    x, c, ctx, c_ctx = f(inp["x"]), f(inp["c"]), f(inp["ctx"]), f(inp["c_ctx"])
    maps = []
    for b in cores:
        m = dict(shared)
        m["xin"] = np.ascontiguousarray(np.concatenate([ctx[b], x[b]], axis=0))
        cc = np.stack([c[b], c_ctx], axis=-1)
        m["ccT"] = np.ascontiguousarray(cc.reshape(8, 128, 2).transpose(1, 0, 2))
        maps.append(m)
    return maps


_NC_CACHE = {}


def kernel(**inputs):
    if "nc" not in _NC_CACHE:
        _NC_CACHE["nc"] = build()
    nc = _NC_CACHE["nc"]
    maps = prep_inputs(inputs)
    res = run_bass_kernel_spmd(nc, maps, core_ids=list(range(8)))
    return np.stack([np.asarray(r["y"], dtype=np.float32) for r in res.results], axis=0)
```

```python
import contextlib
import math
import numpy as np
import concourse.bass as bass
import concourse.mybir as mybir
from concourse.bass_utils import run_bass_kernel_spmd

F32 = mybir.dt.float32
BF16 = mybir.dt.bfloat16
AF = mybir.ActivationFunctionType
ALU = mybir.AluOpType
AX = mybir.AxisListType

ENGS = ("pe", "act", "dve", "pool", "sp")
SAME_SYNC = {"pe": False, "act": True, "dve": True, "pool": True, "sp": True}
N_DMA_SEMS = 18


class Op:
    __slots__ = ("eng", "fn", "deps", "dma", "needs_inc", "idx", "sem", "val", "pos")

    def __init__(self, eng, fn, deps, dma):
        self.eng, self.fn, self.deps, self.dma = eng, fn, deps, dma
        self.needs_inc = False
        self.sem = None
        self.val = None


class Prog:
    def __init__(self, nc):
        self.nc = nc
        self.ops = {e: [] for e in ENGS}
        self.last_w = {}
        self.readers = {}
        self.pending_bar = {e: None for e in ENGS}
        self.since_bar = []

    def op(self, eng, fn, reads=(), writes=(), dma=False):
        deps = set()
        for k in reads:
            w = self.last_w.get(k)
            if w is not None:
                deps.add(w)
        for k in writes:
            w = self.last_w.get(k)
            if w is not None:
                deps.add(w)
            for r in self.readers.get(k, ()):
                deps.add(r)
        if self.pending_bar[eng] is not None:
            deps.update(self.pending_bar[eng])
            self.pending_bar[eng] = None
        rec = Op(eng, fn, deps, dma)
        for k in reads:
            self.readers.setdefault(k, []).append(rec)
        for k in writes:
            self.last_w[k] = rec
            self.readers[k] = []
        self.ops[eng].append(rec)
        if dma:
            self.since_bar.append(rec)
        return rec

    def barrier(self):
        deps = list(self.since_bar)
        for e in ENGS:
            if self.ops[e]:
                deps.append(self.ops[e][-1])
        for e in ENGS:
            cur = self.pending_bar[e]
            self.pending_bar[e] = (cur or []) + deps
        self.since_bar = []

    def pe(self, fn, r=(), w=()):
        return self.op("pe", fn, r, w)

    def act(self, fn, r=(), w=()):
        return self.op("act", fn, r, w)

    def dve(self, fn, r=(), w=()):
        return self.op("dve", fn, r, w)

    def pool(self, fn, r=(), w=()):
        return self.op("pool", fn, r, w)

    def dma(self, q, fn, r=(), w=()):
        return self.op(q, fn, r, w, dma=True)

    def emit(self, final_wait_ops=()):
        nc = self.nc
        for e in ENGS:
            for rec in self.ops[e]:
                for d in rec.deps:
                    if d.dma:
                        continue
                    if d.eng == rec.eng and not SAME_SYNC[rec.eng]:
                        continue
                    d.needs_inc = True
        with contextlib.ExitStack() as es:
            esem = {e: es.enter_context(nc.semaphore(f"s_{e}")) for e in ENGS}
            dsem = {e: [es.enter_context(nc.semaphore(f"d_{e}_{i}")) for i in range(N_DMA_SEMS)]
                    for e in ("sp", "pool", "act")}
            finals = {}
            for e in ENGS:
                cnt = 0
                dcnt = [0] * N_DMA_SEMS
                di = 0
                for rec in self.ops[e]:
                    if rec.dma:
                        k = di % N_DMA_SEMS
                        di += 1
                        dcnt[k] += 16
                        rec.sem, rec.val = dsem[e][k], dcnt[k]
                    elif rec.needs_inc:
                        cnt += 1
                        rec.sem, rec.val = esem[e], cnt
                if e in dsem:
                    finals[e] = list(dcnt)
            block = es.enter_context(nc.Block())

            def run(e, eh):
                waited = {}
                for rec in self.ops[e]:
                    need = {}
                    for d in rec.deps:
                        if (not d.dma) and d.eng == e and not SAME_SYNC[e]:
                            continue
                        s, v = d.sem, d.val
                        key = id(s)
                        if waited.get(key, 0) >= v:
                            continue
                        if key not in need or need[key][1] < v:
                            need[key] = (s, v)
                    if rec.dma and rec.val > 16:
                        s, v = rec.sem, rec.val - 16
                        key = id(s)
                        if waited.get(key, 0) < v and (key not in need or need[key][1] < v):
                            need[key] = (s, v)
                    for key, (s, v) in need.items():
                        eh.wait_ge(s, v)
                        waited[key] = v
                    ins = rec.fn(eh)
                    if rec.dma:
                        ins.then_inc(rec.sem, 16)
                    elif rec.needs_inc:
                        ins.then_inc(rec.sem, 1)
                if e == "sp":
                    for q, cnts in finals.items():
                        for k, v in enumerate(cnts):
                            if v > 0:
                                eh.wait_ge(dsem[q][k], v)

            block.tensor(lambda eh: run("pe", eh))
            block.scalar(lambda eh: run("act", eh))
            block.vector(lambda eh: run("dve", eh))
            block.gpsimd(lambda eh: run("pool", eh))
            block.sync(lambda eh: run("sp", eh))


D = 1024
T = 4352
NT = 34
NCTX_T = 2
L_DEPTH = 2
EPS = 1e-6
IN_DIM = 3096
GROUPS = [(0, 2)] + [(2 + 4 * i, 4) for i in range(8)]
NEG = -30000.0
C_QKV, C_Z, C_XBC, C_DT, C_GQKV, C_GATE, C_BETA, C_A = 0, 768, 1280, 2048, 2056, 2824, 3080, 3088


def build(NL=2, dbg=False, stop_after=None, zero_ml=False, small_exp=False):
    nc = bass.Bass("TRN2", target_bir_lowering=False)
    P = Prog(nc)

    def din(name, shape, dt=F32):
        return nc.dram_tensor(name, list(shape), dt, kind="ExternalInput").ap()

    def dscr(name, shape, dt, out=False):
        return nc.dram_tensor(name, list(shape), dt, kind=("ExternalOutput" if out else "Internal")).ap()

    xin = din("xin", [T, D])
    ccT = din("ccT", [128, 8, 2])
    rope = din("rope", [2, 128, 32, 32])
    w_mod = din("w_mod", [L_DEPTH, D, 6 * D])
    b_modT = din("b_modT", [L_DEPTH, 128, 48])
    n1T = din("n1T", [L_DEPTH, 128, 8])
    n2T = din("n2T", [L_DEPTH, 128, 8])
    w_in = din("w_in", [L_DEPTH, D, IN_DIM])
    w_out = din("w_out", [L_DEPTH, D, D])
    diff_qn_w = din("diff_qn_w", [L_DEPTH, 32])
    diff_kn_w = din("diff_kn_w", [L_DEPTH, 32])
    diff_lq1 = din("diff_lq1", [L_DEPTH, 32])
    diff_lk1 = din("diff_lk1", [L_DEPTH, 32])
    diff_lq2 = din("diff_lq2", [L_DEPTH, 32])
    diff_lk2 = din("diff_lk2", [L_DEPTH, 32])
    diff_norm_w = din("diff_norm_w", [L_DEPTH, 64])
    ssm_conv_wT = din("ssm_conv_wT", [L_DEPTH, 128, 6, 5])
    ssm_conv_bT = din("ssm_conv_bT", [L_DEPTH, 128, 6])
    ssm_dt_bias = din("ssm_dt_bias", [L_DEPTH, 2, 8])
    ssm_a_log = din("ssm_a_log", [L_DEPTH, 2, 8])
    ssm_d = din("ssm_d", [L_DEPTH, 8])
    ssm_norm_w = din("ssm_norm_w", [L_DEPTH, 512])
    gdn_conv_wT = din("gdn_conv_wT", [L_DEPTH, 128, 6, 5])
    gdn_dt_bias = din("gdn_dt_bias", [L_DEPTH, 2, 4])
    gdn_a_log = din("gdn_a_log", [L_DEPTH, 2, 4])
    gdn_norm_w = din("gdn_norm_w", [L_DEPTH, 64])
    router_w = din("router_w", [L_DEPTH, D, 36])
    router_b = din("router_b", [L_DEPTH, 36])
    _ne = 1 if small_exp else 32
    _nl = 1 if small_exp else L_DEPTH
    exp_w_gate = din("exp_w_gate", [_nl, _ne, D, 512])
    exp_w_up = din("exp_w_up", [_nl, _ne, D, 512])
    exp_w_down = din("exp_w_down", [_nl, _ne, 512, D])

    y_out = nc.dram_tensor("y", [4096, D], F32, kind="ExternalOutput").ap()
    xs1 = dscr("xs1", [T, D], F32, out=dbg)
    xs2 = dscr("xs2", [T, D], F32)
    hT = dscr("hT", [12, 128, T], BF16, out=dbg)
    zs = dscr("zs", [T, 512], BF16)
    gs = dscr("gs", [T, 256], BF16)
    ml = dscr("ml", [T, D], BF16, out=dbg)
    modrows = dscr("modrows", [96, 128], F32)

    outs = []
    with contextlib.ExitStack() as es0:
        _cnt = [0]

        def sb(es, name, shape, dt):
            _cnt[0] += 1
            return es.enter_context(nc.sbuf_tensor(f"{name}_{_cnt[0]}", list(shape), dt))

        banks = [es0.enter_context(nc.psum_tensor(f"bank{i}", [128, 512], F32)) for i in range(6)]
        tbanks = {6 + i: es0.enter_context(nc.psum_tensor(f"tbank{i}", [128, 1024], BF16)) for i in range(2)}

        def BK(i):
            return ("bank", i)

        identf = sb(es0, "identf", [128, 128], F32)
        identb = sb(es0, "identb", [128, 128], BF16)
        onesf = sb(es0, "onesf", [128, 128], F32)
        Uf = sb(es0, "Uf", [128, 128], F32)
        Ub = sb(es0, "Ub", [128, 128], F32)
        nUf = sb(es0, "nUf", [128, 128], F32)
        nUb = sb(es0, "nUb", [128, 128], F32)
        Sf = sb(es0, "Sf", [128, 128], F32)
        Sb_ = sb(es0, "Sb", [128, 128], F32)
        NMf = sb(es0, "NMf", [128, 4, 128], F32)
        NMb = sb(es0, "NMb", [128, 4, 128], F32)
        cc_sb = sb(es0, "cc_sb", [128, 8, 2], F32)
        csil = sb(es0, "csil", [128, 8, 2], F32)
        epsc = sb(es0, "epsc", [128, 1], F32)

        def mk_mask(t_ap, val_in, fill, pattern, cm, cmp, key):
            P.pool(lambda e: e.memset(t_ap, val_in), w=[key])
            P.pool(lambda e: e.affine_select(out=t_ap, in_=t_ap, compare_op=cmp, fill=fill, base=0,
                                             pattern=pattern, channel_multiplier=cm), r=[key], w=[key])

        mk_mask(identf[:], 0.0, 1.0, [[-1, 128]], 1, ALU.not_equal, "identf")
        P.pool(lambda e: e.tensor_copy(out=identb[:], in_=identf[:]), r=["identf"], w=["identb"])
        P.pool(lambda e: e.memset(onesf[:], 1.0), w=["onesf"])
        P.pool(lambda e: e.memset(epsc[:], EPS), w=["epsc"])
        mk_mask(Uf[:], 1.0, 0.0, [[1, 128]], -1, ALU.is_ge, "Uf")
        mk_mask(Ub[:], 1.0, 0.0, [[-1, 128]], 1, ALU.is_ge, "Ub")
        mk_mask(nUf[:], -1.0, 0.0, [[1, 128]], -1, ALU.is_ge, "nUf")
        mk_mask(nUb[:], -1.0, 0.0, [[-1, 128]], 1, ALU.is_ge, "nUb")
        mk_mask(Sf[:], 1.0, 0.0, [[-1, 128]], 1, ALU.is_gt, "Sf")
        mk_mask(Sb_[:], 1.0, 0.0, [[1, 128]], -1, ALU.is_gt, "Sb")
        for j in range(4):
            mk_mask(NMf[:, j, :], NEG, 0.0, [[-1, 128]], 1, ALU.is_gt, ("NMf", j))
            mk_mask(NMb[:, j, :], NEG, 0.0, [[1, 128]], -1, ALU.is_gt, ("NMb", j))
        NMf_keys = [("NMf", j) for j in range(4)]
        NMb_keys = [("NMb", j) for j in range(4)]

        blkf = sb(es0, "blkf", [128, 128], F32)
        offd = sb(es0, "offd", [128, 128], F32)
        UBf = sb(es0, "UBf", [128, 128], F32)
        UBb = sb(es0, "UBb", [128, 128], F32)
        nUBf = sb(es0, "nUBf", [128, 128], F32)
        nUBb = sb(es0, "nUBb", [128, 128], F32)
        SBf = sb(es0, "SBf", [128, 128], F32)
        SBb = sb(es0, "SBb", [128, 128], F32)
        NMBf = sb(es0, "NMBf", [128, 4, 128], F32)
        NMBb = sb(es0, "NMBb", [128, 4, 128], F32)
        chunkind = sb(es0, "chunkind", [128, 2], F32)
        P.pool(lambda e: e.memset(blkf[:], 0.0), w=["blkf"])
        P.pool(lambda e: e.memset(blkf[0:64, 0:64], 1.0), r=["blkf"], w=["blkf"])
        P.pool(lambda e: e.memset(blkf[64:128, 64:128], 1.0), r=["blkf"], w=["blkf"])
        P.pool(lambda e: e.memset(chunkind[:], 0.0), w=["chunkind"])
        P.pool(lambda e: e.memset(chunkind[0:64, 0:1], 1.0), r=["chunkind"], w=["chunkind"])
        P.pool(lambda e: e.memset(chunkind[64:128, 1:2], 1.0), r=["chunkind"], w=["chunkind"])
        mk_mask(offd[:], 1.0, 0.0, [[-1, 128]], 1, ALU.not_equal, "offd")
        for (dst, src, kd, ks) in ((UBf, Uf, "UBf", "Uf"), (UBb, Ub, "UBb", "Ub"), (SBf, Sf, "SBf", "Sf"), (SBb, Sb_, "SBb", "Sb"),
                                   (nUBf, nUf, "nUBf", "nUf"), (nUBb, nUb, "nUBb", "nUb")):
            P.pool(lambda e, dst=dst, src=src: e.tensor_tensor(out=dst[:], in0=src[:], in1=blkf[:], op=ALU.mult), r=[ks, "blkf"], w=[kd])
        for j in range(4):
            P.dve(lambda e, j=j: e.tensor_scalar(out=NMBf[:, j, :], in0=UBb[:], scalar1=-NEG, scalar2=NEG, op0=ALU.mult, op1=ALU.add),
                  r=["UBb"], w=[("NMBf", j)])
            P.dve(lambda e, j=j: e.tensor_scalar(out=NMBb[:, j, :], in0=UBf[:], scalar1=-NEG, scalar2=NEG, op0=ALU.mult, op1=ALU.add),
                  r=["UBf"], w=[("NMBb", j)])
        NMBf_keys = [("NMBf", j) for j in range(4)]
        NMBb_keys = [("NMBb", j) for j in range(4)]
        P.dma("sp", lambda e: e.dma_start(out=cc_sb[:], in_=ccT), w=["cc_sb"])
        P.act(lambda e: e.activation(out=csil[:], in_=cc_sb[:], func=AF.Silu), r=["cc_sb"], w=["csil"])

        modT = sb(es0, "modT", [128, 48, 2], F32)
        s1 = sb(es0, "s1", [128, 8, 2], F32)
        s2 = sb(es0, "s2", [128, 8, 2], F32)
        g1row = sb(es0, "g1row", [128, 2, 8, 128], F32)
        g2row = sb(es0, "g2row", [128, 2, 8, 128], F32)
        small_all = sb(es0, "small_all", [128, NT, 24], F32)

        def bcast_load(es, name, src_ap, n):
            t = sb(es, name, [128, n], F32)
            P.dma("sp", lambda e: e.dma_start(out=t[:], in_=src_ap.partition_broadcast(128)), w=[name])
            return t

        def rsqrt_ops(dst_ap, src_ap, scale, keys_r, key_w, n_eps=None):
            P.dve(lambda e: e.tensor_scalar(out=dst_ap, in0=src_ap, scalar1=scale, scalar2=EPS,
                                            op0=ALU.mult, op1=ALU.add), r=keys_r, w=[key_w])
            P.act(lambda e: e.activation(out=dst_ap, in_=dst_ap, func=AF.Sqrt), r=[key_w], w=[key_w])
            P.dve(lambda e: e.reciprocal(out=dst_ap, in_=dst_ap), r=[key_w], w=[key_w])

        def phase_mod(l, es):
            wm = [sb(es, f"wm{i}", [128, 8, 512], F32) for i in range(2)]
            bT = sb(es, "bT", [128, 48], F32)
            nT = sb(es, "nT", [128, 2, 8], F32)
            mrs = sb(es, "mrs", [96, 128], F32)
            tmp = sb(es, "modtmp", [128, 8, 2], F32)
            P.dma("sp", lambda e: e.dma_start(out=bT[:], in_=b_modT[l]), w=["bT"])
            P.dma("sp", lambda e: e.dma_start(out=nT[:, 0, :], in_=n1T[l]), w=["nT0"])
            P.dma("sp", lambda e: e.dma_start(out=nT[:, 1, :], in_=n2T[l]), w=["nT1"])
            pm = banks[0][:, 0:96].rearrange("p (c j) -> p c j", j=2)
            for piece in range(12):
                s = piece % 2
                P.dma("sp", lambda e, s=s, piece=piece: e.dma_start(
                    out=wm[s][:], in_=w_mod[l][:, piece * 512:(piece + 1) * 512].rearrange("(k p) n -> p k n", p=128)),
                    w=[("wm", s)])
                for fc in range(4):
                    c = piece * 4 + fc
                    for k in range(8):
                        P.pe(lambda e, s=s, fc=fc, c=c, k=k: e.matmul(
                            pm[:, c, :], lhsT=wm[s][:, k, fc * 128:(fc + 1) * 128], rhs=csil[:, k, :],
                            start=(k == 0), stop=(k == 7)), r=[("wm", s), "csil"], w=[BK(0)])
            P.dve(lambda e: e.tensor_tensor(out=modT[:], in0=pm, in1=bT[:].unsqueeze(2).to_broadcast([128, 48, 2]),
                                            op=ALU.add), r=[BK(0), "bT"], w=["modT"])
            for (sx, lo, ni, nk) in ((s1, 8, 0, "nT0"), (s2, 32, 1, "nT1")):
                P.dve(lambda e, lo=lo: e.tensor_scalar(out=tmp[:], in0=modT[:, lo:lo + 8, :], scalar1=1.0, scalar2=None,
                                                       op0=ALU.add), r=["modT"], w=["modtmp"])
                P.dve(lambda e, sx=sx, ni=ni: e.tensor_tensor(
                    out=sx[:], in0=tmp[:], in1=nT[:, ni, :].unsqueeze(2).to_broadcast([128, 8, 2]), op=ALU.mult),
                    r=["modtmp", nk], w=["s1k" if ni == 0 else "s2k"])
            pmt = banks[1][0:96, 0:128]
            P.pe(lambda e: e.transpose(out=pmt, in_=modT[:].rearrange("p c j -> p (c j)"), identity=identf[:]),
                 r=["modT", "identf"], w=[BK(1)])
            P.act(lambda e: e.activation(out=mrs[:], in_=pmt, func=AF.Copy), r=[BK(1)], w=["mrs"])
            P.dma("sp", lambda e: e.dma_start(out=modrows, in_=mrs[:]), r=["mrs"], w=["modrows"])
            mr3 = modrows.rearrange("(c j) p -> j c p", j=2)
            for j in range(2):
                P.dma("sp", lambda e, j=j: e.dma_start(out=g1row[:, j, :, :], in_=mr3[j, 16:24, :].partition_broadcast(128)),
                      r=["modrows"], w=["g1row"])
                P.dma("sp", lambda e, j=j: e.dma_start(out=g2row[:, j, :, :], in_=mr3[j, 40:48, :].partition_broadcast(128)),
                      r=["modrows"], w=["g2row"])

        S1K = "s1k"
        S2K = "s2k"

        def norm_tile_T(es_tmp, tag, src_rows_ap, j, scl, shf_lo, dstT_ap_fn, bank_id, ring, fp32_path=None):
            raise NotImplementedError

        def phase_proj_attn(l, stream_in):
            lam_init = 0.8 - 0.6 * math.exp(-0.3 * l)
            with contextlib.ExitStack() as esA:
                QKT = sb(esA, "QKT", [128, 6, T], BF16)
                V1 = sb(esA, "V1", [128, NT, 4, 65], BF16)
                lamt = sb(esA, "lamt", [128, 4], F32)
                nlam = sb(esA, "nlam", [128, 1], F32)
                dnw = bcast_load(esA, "dnw", diff_norm_w[l], 64)
                P.pool(lambda e: e.memset(V1[:, :, :, 64:65], 1.0), w=["V1ones"])
                lv = [bcast_load(esA, f"lv{i}", a[l], 32) for i, a in
                      enumerate((diff_lq1, diff_lk1, diff_lq2, diff_lk2))]
                lj = sb(esA, "lj", [128, 32], F32)
                for i in range(2):
                    P.dve(lambda e, i=i: e.tensor_tensor(out=lj[:], in0=lv[2 * i][:], in1=lv[2 * i + 1][:], op=ALU.mult),
                          r=[f"lv{2 * i}", f"lv{2 * i + 1}"], w=["lj"])
                    P.dve(lambda e, i=i: e.tensor_reduce(out=lamt[:, i:i + 1], in_=lj[:], axis=AX.X, op=ALU.add),
                          r=["lj"], w=["lamt"])
                P.act(lambda e: e.activation(out=lamt[:, 2:4], in_=lamt[:, 0:2], func=AF.Exp), r=["lamt"], w=["lamt"])
                P.dve(lambda e: e.scalar_tensor_tensor(out=nlam[:], in0=lamt[:, 3:4], scalar=-lam_init, in1=lamt[:, 2:3],
                                                       op0=ALU.add, op1=ALU.subtract), r=["lamt"], w=["nlam"])

                with contextlib.ExitStack() as esB:
                    wib = sb(esB, "wib", [128, 8, IN_DIM], BF16)
                    wst = [sb(esB, f"wst{i}", [128, 1032], F32) for i in range(1)]
                    xt = [sb(esB, f"xt{i}", [128, D], F32) for i in range(2)]
                    xb = [sb(esB, f"xb{i}", [128, D], BF16) for i in range(2)]
                    junk = sb(esB, "junk", [128, D], BF16)
                    ss = [sb(esB, f"ss{i}", [128, 1], F32) for i in range(2)]
                    xnT = [sb(esB, f"xnT{i}", [128, 8, 512], BF16) for i in range(1)]
                    hst = [sb(esB, f"hst{i}", [128, 512], BF16) for i in range(3)]
                    wqk = sb(esB, "wqk", [128, 16, 32], F32)
                    cosT = sb(esB, "cosT", [128, 32, 32], F32)
                    sinT = sb(esB, "sinT", [128, 32, 32], F32)
                    sqb = sb(esB, "sqb", [128, 512], F32)
                    ssq = sb(esB, "ssq", [128, 16], F32)
                    qk32 = sb(esB, "qk32", [128, 16, 32], F32)
                    rtmp = sb(esB, "rtmp", [128, 16, 32], F32)
                    qkb = [sb(esB, f"qkb{i}", [128, 512], BF16) for i in range(2)]
                    zst = [sb(esB, f"zst{i}", [128, 512], BF16) for i in range(1)]
                    gst = [sb(esB, f"gst{i}", [128, 256], BF16) for i in range(2)]

                    n_p = 0
                    for k in range(8):
                        for c3 in range(3):
                            s = 0
                            n_p += 1
                            P.dma("sp", lambda e, s=s, k=k, c3=c3: e.dma_start(
                                out=wst[s][:], in_=w_in[l][k * 128:(k + 1) * 128, c3 * 1032:(c3 + 1) * 1032]),
                                w=[("wst", s)])
                            P.pool(lambda e, s=s, k=k, c3=c3: e.tensor_copy(
                                out=wib[:, k, c3 * 1032:(c3 + 1) * 1032], in_=wst[s][:]),
                                r=[("wst", s)], w=[("wib", k, c3)])
                    WIB = [("wib", k, c3) for k in range(8) for c3 in range(3)]
                    for gi in range(8):
                        P.dma("sp", lambda e, gi=gi: e.dma_start(out=wqk[:, gi, :], in_=diff_qn_w[l].partition_broadcast(128)),
                              w=["wqk"])
                        P.dma("sp", lambda e, gi=gi: e.dma_start(out=wqk[:, 8 + gi, :], in_=diff_kn_w[l].partition_broadcast(128)),
                              w=["wqk"])
                    P.dma("sp", lambda e: e.dma_start(out=cosT[:], in_=rope[0]), w=["cosT"])
                    P.dma("sp", lambda e: e.dma_start(out=sinT[:], in_=rope[1]), w=["sinT"])

                    ev = 0
                    if stop_after == "p1a":
                        return
                    for gidx, (t0, nt) in enumerate(GROUPS):
                        N = nt * 128
                        xs_ = 0
                        for ti in range(nt):
                            t = t0 + ti
                            j = 1 if t < NCTX_T else 0
                            a = t % 2
                            b2 = t % 2
                            P.dma("sp", lambda e, a=a, t=t: e.dma_start(out=xt[a][:], in_=stream_in[t * 128:(t + 1) * 128, :]),
                                  w=[("xt", a)])
                            P.act(lambda e, a=a: e.activation(out=junk[:], in_=xt[a][:], func=AF.Square, accum_out=ss[a][:]),
                                  r=[("xt", a)], w=["junk", ("ss", a)])
                            rsqrt_ops(ss[a][:], ss[a][:], 1.0 / D, [("ss", a)], ("ss", a))
                            P.dve(lambda e, a=a, b2=b2: e.tensor_scalar(out=xb[b2][:], in0=xt[a][:], scalar1=ss[a][:, 0:1],
                                                                        scalar2=None, op0=ALU.mult),
                                  r=[("xt", a), ("ss", a)], w=[("xb", b2)])
                            bk = 6 + b2
                            ptv = tbanks[bk][:].rearrange("p (k n) -> p k n", n=128)
                            for k in range(8):
                                P.pe(lambda e, b2=b2, k=k, ptv=ptv: e.transpose(out=ptv[:, k, :], in_=xb[b2][:, k * 128:(k + 1) * 128],
                                                                               identity=identb[:]),
                                     r=[("xb", b2), "identb"], w=[BK(bk)])
                            for k in range(8):
                                dst = xnT[xs_][:, k, ti * 128:(ti + 1) * 128]
                                if True:
                                    P.act(lambda e, dst=dst, k=k, j=j, ptv=ptv: e.activation(
                                        out=dst, in_=ptv[:, k, :], func=AF.Identity, scale=s1[:, k, j:j + 1], bias=modT[:, k, j:j + 1]),
                                        r=[BK(bk), "s1k", "modT"], w=[("xnT", xs_, ti, k)])
                                else:
                                    P.dve(lambda e, dst=dst, k=k, j=j, ptv=ptv: e.tensor_scalar(
                                        out=dst, in0=ptv[:, k, :], scalar1=s1[:, k, j:j + 1], scalar2=modT[:, k, j:j + 1],
                                        op0=ALU.mult, op1=ALU.add),
                                        r=[BK(bk), "s1k", "modT"], w=[("xnT", xs_, ti, k)])
                        XN = [("xnT", xs_, ti, k) for ti in range(nt) for k in range(8)]
                        if stop_after == "p1b":
                            return
                        for ch in range(12):
                            col0 = (C_XBC + ch * 128) if ch < 6 else (C_GQKV + (ch - 6) * 128)
                            bk = ev % 3
                            ev += 1
                            for k in range(8):
                                P.pe(lambda e, bk=bk, k=k, col0=col0, N=N, xs_=xs_: e.matmul(
                                    banks[bk][:, 0:N], lhsT=wib[:, k, col0:col0 + 128], rhs=xnT[xs_][:, k, 0:N],
                                    start=(k == 0), stop=(k == 7)), r=WIB + XN, w=[BK(bk)])
                            P.act(lambda e, bk=bk, N=N: e.activation(out=hst[bk][:, 0:N], in_=banks[bk][:, 0:N], func=AF.Copy),
                                  r=[BK(bk)], w=[("hst", bk)])
                            P.dma("pool", lambda e, bk=bk, ch=ch, t0=t0, N=N: e.dma_start(
                                out=hT[ch, :, t0 * 128:t0 * 128 + N], in_=hst[bk][:, 0:N]), r=[("hst", bk)], w=[("hT", ch, gidx)])
                        if stop_after == "p1c":
                            return
                        for ti in range(nt):
                            t = t0 + ti
                            latent = t >= NCTX_T
                            tk = slice(ti * 128, (ti + 1) * 128)
                            specs = [(3, 0, 512, C_QKV), (4, 0, 256, C_QKV + 512), (4, 256, 256, C_GATE),
                                     (5, 0, 512, C_Z), (3, 0, 0, 0)]
                            for (bk, o0, w_, c0) in specs[:4]:
                                for k in range(8):
                                    P.pe(lambda e, bk=bk, o0=o0, w_=w_, c0=c0, k=k, tk=tk, xs_=xs_: e.matmul(
                                        banks[bk][:, o0:o0 + w_], lhsT=xnT[xs_][:, k, tk], rhs=wib[:, k, c0:c0 + w_],
                                        start=(k == 0), stop=(k == 7)), r=WIB + XN, w=[BK(bk)])
                            P.act(lambda e: e.activation(out=sqb[:], in_=banks[3][:], func=AF.Square), r=[BK(3)], w=["sqb"])
                            P.dve(lambda e: e.tensor_reduce(out=ssq[:], in_=sqb[:].rearrange("p (g d) -> p g d", d=32),
                                                            axis=AX.X, op=ALU.add), r=["sqb"], w=["ssq"])
                            rsqrt_ops(ssq[:], ssq[:], 1.0 / 32, ["ssq"], "ssq")
                            P.dve(lambda e: e.tensor_tensor(out=qk32[:], in0=banks[3][:].rearrange("p (g d) -> p g d", d=32),
                                                            in1=ssq[:].unsqueeze(2).to_broadcast([128, 16, 32]), op=ALU.mult),
                                  r=[BK(3), "ssq"], w=["qk32"])
                            q2 = t % 2
                            if latent:
                                lt = t - NCTX_T
                                P.dve(lambda e: e.tensor_tensor(out=qk32[:], in0=qk32[:], in1=wqk[:], op=ALU.mult),
                                      r=["qk32", "wqk"], w=["qk32"])
                                x5 = qk32[:].rearrange("p g (a h e) -> p g a h e", a=2, h=2, e=8)
                                r5 = rtmp[:].rearrange("p g (a h e) -> p g a h e", a=2, h=2, e=8)
                                s4 = sinT[:, lt, :].rearrange("p (a h e) -> p a h e", a=2, h=2, e=8)
                                for hh in range(2):
                                    P.dve(lambda e, hh=hh, x5=x5, r5=r5, s4=s4: e.tensor_tensor(
                                        out=r5[:, :, :, hh, :], in0=x5[:, :, :, 1 - hh, :],
                                        in1=s4[:, :, hh, :].unsqueeze(1).to_broadcast([128, 16, 2, 8]), op=ALU.mult),
                                        r=["qk32", "sinT"], w=[("rtmp", hh)])
                                P.dve(lambda e, lt=lt: e.tensor_tensor(
                                    out=qk32[:], in0=qk32[:], in1=cosT[:, lt, :].unsqueeze(1).to_broadcast([128, 16, 32]),
                                    op=ALU.mult), r=["qk32", "cosT", ("rtmp", 0), ("rtmp", 1)], w=["qk32"])
                                P.dve(lambda e, q2=q2: e.tensor_tensor(
                                    out=qkb[q2][:].rearrange("p (g d) -> p g d", d=32), in0=qk32[:], in1=rtmp[:], op=ALU.add),
                                    r=["qk32", ("rtmp", 0), ("rtmp", 1)], w=[("qkb", q2)])
                            else:
                                P.dve(lambda e, q2=q2: e.tensor_tensor(
                                    out=qkb[q2][:].rearrange("p (g d) -> p g d", d=32), in0=qk32[:], in1=wqk[:], op=ALU.mult),
                                    r=["qk32", "wqk"], w=[("qkb", q2)])
                            bk = 6 + q2
                            ptq = tbanks[bk][:, 0:768].rearrange("p (c n) -> p c n", n=128)
                            for c in range(6):
                                lo = (c // 3) * 256 + (c % 3) * 96
                                wd = 96 if (c % 3) < 2 else 64
                                P.pe(lambda e, c=c, q2=q2, ptq=ptq, lo=lo, wd=wd: e.transpose(
                                    out=ptq[0:wd, c, :], in_=qkb[q2][:, lo:lo + wd], identity=identb[:]),
                                     r=[("qkb", q2), "identb"], w=[BK(bk)])
                            P.act(lambda e, t=t, ptq=ptq: e.activation(out=QKT[0:96, :, t * 128:(t + 1) * 128], in_=ptq[0:96, :, :],
                                                                       func=AF.Copy),
                                  r=[BK(bk)], w=[("QKT", t)])
                            if stop_after == "p1d":
                                return
                            P.dve(lambda e, t=t: e.tensor_copy(out=V1[:, t, :, 0:64],
                                                               in_=banks[4][:, 0:256].rearrange("p (h v) -> p h v", v=64)),
                                  r=[BK(4)], w=[("V1", t)])
                            P.act(lambda e, q2=q2: e.activation(out=gst[q2][:], in_=banks[4][:, 256:512], func=AF.Silu),
                                  r=[BK(4)], w=[("gst", q2)])
                            P.dma("pool", lambda e, q2=q2, t=t: e.dma_start(out=gs[t * 128:(t + 1) * 128, :], in_=gst[q2][:]),
                                  r=[("gst", q2)], w=[("gs", t)])
                            P.act(lambda e, q2=q2: e.activation(out=zst[0][:], in_=banks[5][:], func=AF.Silu),
                                  r=[BK(5)], w=[("zst", 0)])
                            P.dma("pool", lambda e, q2=q2, t=t: e.dma_start(out=zs[t * 128:(t + 1) * 128, :], in_=zst[0][:]),
                                  r=[("zst", 0)], w=[("zs", t)])
                            for (o0, w_, c0) in ((0, 8, C_DT), (8, 16, C_BETA)):
                                for k in range(8):
                                    P.pe(lambda e, o0=o0, w_=w_, c0=c0, k=k, tk=tk, xs_=xs_: e.matmul(
                                        banks[5][:, o0:o0 + w_], lhsT=xnT[xs_][:, k, tk], rhs=wib[:, k, c0:c0 + w_],
                                        start=(k == 0), stop=(k == 7)), r=WIB + XN, w=[BK(5)])
                            P.dve(lambda e, t=t: e.tensor_copy(out=small_all[:, t, :], in_=banks[5][:, 0:24]),
                                  r=[BK(5)], w=[("small", t)])
                            if stop_after == "p1e" or (stop_after == "p1g" and t == 2):
                                return
                        if stop_after == "p1f":
                            return
                P.barrier()
                if stop_after == "proj":
                    return
                with contextlib.ExitStack() as esC:
                    PT = [sb(esC, f"PT{i}", [128, 512], BF16) for i in range(3)]
                    osb = [sb(esC, f"osb{i}", [128, 4, 2, 65], F32) for i in range(2)]
                    rden = sb(esC, "rden", [128, 4, 2], F32)
                    o1 = sb(esC, "o1", [128, 4, 64], F32)
                    od = sb(esC, "od", [128, 4, 64], F32)
                    osq = sb(esC, "osq", [128, 4, 64], F32)
                    orr = sb(esC, "orr", [128, 4], F32)
                    aout = [sb(esC, f"aout{i}", [128, 4, 4, 64], BF16) for i in range(2)]
                    scale = 32 ** -0.5
                    QKALL = [("QKT", t) for t in range(NT)]
                    VALL = [("V1", t) for t in range(NT)] + ["V1ones"]
                    items = []

                    def mk_item(idx, gidx, t0, nt, h, m, ki, kt, nk, ob, ao):
                        N = nt * 128
                        mm = 2 * h + m
                        c = mm // 3
                        pb = 32 * (mm % 3)
                        accb = 4 + (mm % 2)
                        accv = banks[accb][:, 0:nt * 65].rearrange("p (q v) -> p q v", v=65)
                        sbk = idx % 3

                        def qk():
                            P.pe(lambda e: e.matmul(
                                banks[sbk][:, 0:N], lhsT=QKT[pb:pb + 32, 3 + c, kt * 128:(kt + 1) * 128],
                                rhs=QKT[pb:pb + 32, c, t0 * 128:t0 * 128 + N], start=True, stop=True),
                                r=QKALL, w=[BK(sbk)])

                        def rest():
                            P.act(lambda e: e.activation(out=PT[sbk][:, 0:N], in_=banks[sbk][:, 0:N], func=AF.Exp, scale=scale),
                                  r=[BK(sbk)], w=[("PT", sbk)])
                            for qb in range(nt):
                                P.pe(lambda e, qb=qb: e.matmul(
                                    accv[:, qb, :], lhsT=PT[sbk][:, qb * 128:(qb + 1) * 128], rhs=V1[:, kt, h, :],
                                    start=(ki == 0 and qb == 0), stop=(ki == nk - 1 and qb == nt - 1)),
                                    r=[("PT", sbk)] + VALL, w=[BK(accb)])
                            if ki == nk - 1:
                                P.act(lambda e: e.activation(out=ob[:, 0:nt, m, :], in_=accv, func=AF.Copy),
                                      r=[BK(accb)], w=[("osb", h % 2, m)])
                                if m == 1:
                                    head_post(gidx, nt, h, ob, ao)
                                    if h == 3:
                                        for qb in range(nt):
                                            t = t0 + qb
                                            P.dma("pool", lambda e, qb=qb, t=t: e.dma_start(
                                                out=ml[t * 128:(t + 1) * 128, 0:256], in_=ao[:, qb, :, :].rearrange("p h v -> p (h v)")),
                                                r=[("aout", gidx % 2, hh_) for hh_ in range(4)], w=[("ml_a", t)])
                        return qk, rest

                    def head_post(gidx, nt, h, ob, ao):
                        OK_ = [("osb", h % 2, 0), ("osb", h % 2, 1)]
                        P.dve(lambda e: e.reciprocal(out=rden[:, 0:nt, :], in_=ob[:, 0:nt, :, 64]), r=OK_, w=["rden"])
                        P.dve(lambda e: e.tensor_tensor(
                            out=o1[:, 0:nt, :], in0=ob[:, 0:nt, 1, 0:64],
                            in1=rden[:, 0:nt, 1:2].to_broadcast([128, nt, 64]), op=ALU.mult), r=OK_ + ["rden"], w=["o1"])
                        P.dve(lambda e: e.tensor_tensor(
                            out=od[:, 0:nt, :], in0=ob[:, 0:nt, 0, 0:64],
                            in1=rden[:, 0:nt, 0:1].to_broadcast([128, nt, 64]), op=ALU.mult), r=OK_ + ["rden"], w=["od"])
                        P.dve(lambda e: e.scalar_tensor_tensor(
                            out=od[:, 0:nt, :], in0=o1[:, 0:nt, :], scalar=nlam[:, 0:1], in1=od[:, 0:nt, :],
                            op0=ALU.mult, op1=ALU.add), r=["o1", "od", "nlam"], w=["od"])
                        P.pool(lambda e: e.tensor_tensor(out=osq[:, 0:nt, :], in0=od[:, 0:nt, :], in1=od[:, 0:nt, :],
                                                         op=ALU.mult), r=["od"], w=["osq"])
                        P.dve(lambda e: e.tensor_reduce(out=orr[:, 0:nt], in_=osq[:, 0:nt, :], axis=AX.X, op=ALU.add),
                              r=["osq"], w=["orr"])
                        rsqrt_ops(orr[:, 0:nt], orr[:, 0:nt], 1.0 / 64, ["orr"], "orr")
                        P.dve(lambda e: e.tensor_tensor(out=od[:, 0:nt, :], in0=od[:, 0:nt, :],
                                                        in1=orr[:, 0:nt].unsqueeze(2).to_broadcast([128, nt, 64]),
                                                        op=ALU.mult), r=["od", "orr"], w=["od"])
                        P.dve(lambda e: e.scalar_tensor_tensor(
                            out=ao[:, 0:nt, h, :], in0=od[:, 0:nt, :], scalar=(1.0 - lam_init),
                            in1=dnw[:].unsqueeze(1).to_broadcast([128, nt, 64]), op0=ALU.mult, op1=ALU.mult),
                            r=["od", "dnw"], w=[("aout", gidx % 2, h)])

                    for gidx, (t0, nt) in enumerate(GROUPS):
                        if gidx == 0 and l == NL - 1:
                            continue
                        ktiles = list(range(NCTX_T)) if gidx == 0 else list(range(NT))
                        ao = aout[gidx % 2]
                        for h in range(4):
                            ob = osb[h % 2]
                            for m in range(2):
                                for ki, kt in enumerate(ktiles):
                                    items.append(mk_item(len(items), gidx, t0, nt, h, m, ki, kt, len(ktiles), ob, ao))
                    LOOK = 2
                    for i in range(len(items) + LOOK):
                        if i < len(items):
                            items[i][0]()
                        if i - LOOK >= 0:
                            items[i - LOOK][1]()
                P.barrier()

        ORDER = [list(range(NT)), [1, 0] + list(range(NT - 1, NCTX_T - 1, -1))]

        def conv_chunk(wT_ap, chs, dgs, cw, key_cw, bias_ap_fn, hcs, sink, hoff):
            pass

        def phase_ssm(l):
            with contextlib.ExitStack() as esA:
                xs_tm = sb(esA, "xs_tm", [128, NT, 512], BF16)
                B_tm = sb(esA, "B_tm", [128, NT, 128], BF16)
                BT = sb(esA, "BT", [128, T], BF16)
                CT = sb(esA, "CT", [128, T], BF16)
                BTm = [sb(esA, f"BTm{i}", [128, T], BF16) for i in range(2)]
                P.pool(lambda e: e.memset(BTm[0][64:128, :], 0.0), w=["BTm0z"])
                P.pool(lambda e: e.memset(BTm[1][0:64, :], 0.0), w=["BTm1z"])
                Y = sb(esA, "Y", [128, NT, 512], BF16)
                dt_all = sb(esA, "dt_all", [128, NT, 2, 8], F32)
                dA_all = sb(esA, "dA_all", [128, NT, 2, 8], F32)
                hS = sb(esA, "hS", [128, 2, 4, 64], F32)
                hSb = sb(esA, "hSb", [128, 2, 4, 64], BF16)
                dtb = bcast_load(esA, "dtb", ssm_dt_bias[l].rearrange("a b -> (a b)"), 16)
                alg = bcast_load(esA, "alg", ssm_a_log[l].rearrange("a b -> (a b)"), 16)
                dsk = bcast_load(esA, "dsk", ssm_d[l], 8)
                nw = bcast_load(esA, "snw", ssm_norm_w[l], 512)
                tmpd = sb(esA, "tmpd", [128, NT, 8], F32)
                P.act(lambda e: e.activation(out=alg[:], in_=alg[:], func=AF.Exp), r=["alg"], w=["alg"])
                P.dve(lambda e: e.tensor_scalar(out=alg[:], in0=alg[:], scalar1=-1.0, scalar2=None, op0=ALU.mult), r=["alg"], w=["alg"])
                SM = [("small", t) for t in range(NT)]
                for d in range(2):
                    P.dve(lambda e, d=d: e.tensor_tensor(out=tmpd[:], in0=small_all[:, :, 0:8],
                                                         in1=dtb[:, d * 8:(d + 1) * 8].unsqueeze(1).to_broadcast([128, NT, 8]), op=ALU.add),
                          r=SM + ["dtb"], w=["tmpd"])
                    P.act(lambda e: e.activation(out=tmpd[:], in_=tmpd[:], func=AF.Exp), r=["tmpd"], w=["tmpd"])
                    P.act(lambda e, d=d: e.activation(out=dt_all[:, :, d, :], in_=tmpd[:], func=AF.Ln, bias=onesf[:, 0:1], scale=1.0),
                          r=["tmpd", "onesf"], w=[("dt_all", d)])
                    P.dve(lambda e, d=d: e.tensor_tensor(out=dA_all[:, :, d, :], in0=dt_all[:, :, d, :],
                                                         in1=alg[:, d * 8:(d + 1) * 8].unsqueeze(1).to_broadcast([128, NT, 8]), op=ALU.mult),
                          r=[("dt_all", d), "alg"], w=[("dA_all", d)])
                P.pool(lambda e: e.memset(hS[:], 0.0), w=[("hS", 0), ("hS", 1)])
                P.pool(lambda e: e.memset(hSb[:], 0.0), w=[("hSb", 0), ("hSb", 1)])
                with contextlib.ExitStack() as esB:
                    cw = sb(esB, "cw", [128, 6, 5], F32)
                    cb = sb(esB, "cb", [128, 6], F32)
                    dg = sb(esB, "dg", [128, 30, 128], BF16)
                    hc = [sb(esB, f"hc{i}", [128, T], BF16) for i in range(2)]
                    cst = [sb(esB, f"cst{i}", [128, 512], BF16) for i in range(2)]
                    P.dma("sp", lambda e: e.dma_start(out=cw[:], in_=ssm_conv_wT[l]), w=["cw"])
                    P.dma("sp", lambda e: e.dma_start(out=cb[:], in_=ssm_conv_bT[l]), w=["cb"])
                    for ch in range(6):
                        for jt in range(5):
                            P.dve(lambda e, ch=ch, jt=jt: e.tensor_scalar(out=dg[:, ch * 5 + jt, :], in0=identf[:], scalar1=cw[:, ch, jt:jt + 1],
                                                                          scalar2=None, op0=ALU.mult), r=["identf", "cw"], w=[("dg", ch)])
                    cnt = 0
                    for ch in range(6):
                        hs_ = ch % 2
                        P.dma("sp", lambda e, hs_=hs_, ch=ch: e.dma_start(out=hc[hs_][:], in_=hT[ch]),
                              r=[("hT", ch, gi) for gi in range(9)], w=[("hc", hs_)])
                        for gidx, (t0, nt) in enumerate(GROUPS):
                            N = nt * 128
                            tok0 = t0 * 128
                            seg_lo, seg_hi = (0, 256) if gidx == 0 else (256, T)
                            bk = cnt % 2
                            cnt += 1
                            taps = [2, 0, 1, 3, 4]
                            for ii, jt in enumerate(taps):
                                o = jt - 2
                                a_ = max(0, seg_lo - tok0 - o)
                                b_ = min(N, seg_hi - tok0 - o)
                                P.pe(lambda e, bk=bk, ch=ch, jt=jt, a_=a_, b_=b_, o=o, tok0=tok0, hs_=hs_, ii=ii: e.matmul(
                                    banks[bk][:, a_:b_], lhsT=dg[:, ch * 5 + jt, :], rhs=hc[hs_][:, tok0 + a_ + o:tok0 + b_ + o],
                                    start=(ii == 0), stop=(ii == 4)), r=[("dg", ch), ("hc", hs_)], w=[BK(bk)])
                            if ch < 4 or ch == 4:
                                dst = cst[bk][:, 0:N] if ch < 4 else BT[:, tok0:tok0 + N]
                                dkey = ("cst", bk) if ch < 4 else ("BT", gidx)
                                P.act(lambda e, bk=bk, dst=dst, ch=ch, N=N: e.activation(out=dst, in_=banks[bk][:, 0:N], func=AF.Silu,
                                                                                       bias=cb[:, ch:ch + 1], scale=1.0),
                                      r=[BK(bk), "cb"], w=[dkey])
                                if ch == 4:
                                    for g_ in range(2):
                                        pg = slice(64 * g_, 64 * g_ + 64)
                                        P.pool(lambda e, g_=g_, pg=pg, tok0=tok0, N=N: e.tensor_copy(out=BTm[g_][pg, tok0:tok0 + N], in_=BT[pg, tok0:tok0 + N]),
                                               r=[dkey, f"BTm{g_}z"], w=[("BTm", g_, gidx)])
                                for ti in range(nt):
                                    t = t0 + ti
                                    tb_ = 6 + (t % 2)
                                    src = (cst[bk][:, ti * 128:(ti + 1) * 128] if ch < 4 else BT[:, t * 128:(t + 1) * 128])
                                    P.pe(lambda e, tb_=tb_, src=src: e.transpose(out=tbanks[tb_][:, 0:128], in_=src, identity=identb[:]),
                                         r=[dkey, "identb"], w=[BK(tb_)])
                                    if ch < 4:
                                        P.dve(lambda e, tb_=tb_, t=t, ch=ch: e.tensor_copy(out=xs_tm[:, t, ch * 128:(ch + 1) * 128], in_=tbanks[tb_][:, 0:128]),
                                              r=[BK(tb_)], w=[("xs_tm", t, ch)])
                                    else:
                                        P.dve(lambda e, tb_=tb_, t=t: e.tensor_copy(out=B_tm[:, t, :], in_=tbanks[tb_][:, 0:128]),
                                              r=[BK(tb_)], w=[("B_tm", t)])
                            else:
                                P.act(lambda e, bk=bk, ch=ch, N=N, tok0=tok0: e.activation(out=CT[:, tok0:tok0 + N], in_=banks[bk][:, 0:N], func=AF.Silu,
                                                                                         bias=cb[:, ch:ch + 1], scale=1.0),
                                      r=[BK(bk), "cb"], w=[("CT", gidx)])
                P.barrier()
                if stop_after == "conv":
                    return
                with contextlib.ExitStack() as esC:
                    rhsU = [sb(esC, f"rhsU{i}", [128, 8, 128], F32) for i in range(2)]
                    rhsB = [sb(esC, f"rhsB{i}", [128, 8, 128], F32) for i in range(2)]
                    Erow = [sb(esC, f"Erow{i}", [128, 8, 128], F32) for i in range(2)]
                    dec = [sb(esC, f"dec{i}", [128, 8, 128], BF16) for i in range(2)]
                    MT = [sb(esC, f"MT{i}", [128, 8, 128], BF16) for i in range(2)]
                    xdt = [sb(esC, f"xdt{i}", [128, 8, 64], BF16) for i in range(2)]
                    xdtw = [sb(esC, f"xdtw{i}", [128, 8, 64], BF16) for i in range(2)]
                    CsT = [sb(esC, f"CsT{i}", [128, 2, 4, 128], BF16) for i in range(2)]
                    for i_ in range(2):
                        P.pool(lambda e, i_=i_: e.memset(CsT[i_][:], 0.0), w=[("CsT", i_, 0), ("CsT", i_, 1)])
                    wdec = [sb(esC, f"wdec{i}", [128, 8], F32) for i in range(2)]
                    dtw = [sb(esC, f"dtw{i}", [128, 8], F32) for i in range(2)]
                    XS = lambda t: [("xs_tm", t, ch) for ch in range(4)]
                    BTALL = [("BT", gi) for gi in range(9)]
                    CTALL = [("CT", gi) for gi in range(9)]
                    seen = set()

                    def sproc(d, t):
                        if True:
                            X0, X1, X2 = banks[3 * d], banks[3 * d + 1], banks[3 * d + 2]
                            K0, K1, K2 = BK(3 * d), BK(3 * d + 1), BK(3 * d + 2)
                            U_, nU_, NM_, S__ = (Uf, nUf, NMf, Sf) if d == 0 else (Ub, nUb, NMb, Sb_)
                            Uk, nUk, NMk, Sk = (("Uf", "nUf", NMf_keys, "Sf") if d == 0 else ("Ub", "nUb", NMb_keys, "Sb"))
                            lastc = 127 if d == 0 else 0
                            tsl = slice(t * 128, (t + 1) * 128)
                            dA = dA_all[:, t, d, :]
                            dtd = dt_all[:, t, d, :]
                            P.pool(lambda e, d=d, U_=U_, dA=dA: e.tensor_tensor(
                                out=rhsU[d][:], in0=U_[:].unsqueeze(1).to_broadcast([128, 8, 128]),
                                in1=dA.unsqueeze(2).to_broadcast([128, 8, 128]), op=ALU.mult),
                                r=[Uk, ("dA_all", d)], w=[("rhsU", d)])
                            P.pool(lambda e, d=d, dA=dA: e.tensor_copy(out=rhsB[d][:], in_=dA.unsqueeze(2).to_broadcast([128, 8, 128])),
                                   r=[("dA_all", d)], w=[("rhsB", d)])
                            for hb in range(2):
                                hsl = slice(4 * hb, 4 * hb + 4)
                                P.pe(lambda e, d=d, hb=hb, hsl=hsl: e.matmul(X0[:], lhsT=onesf[:], rhs=rhsU[d][:, hsl, :].rearrange("p r l -> p (r l)"),
                                                                             start=True, stop=True), r=["onesf", ("rhsU", d)], w=[K0])
                                P.act(lambda e, d=d, hb=hb, hsl=hsl: e.activation(out=Erow[d][:, hsl, :].rearrange("p r l -> p (r l)"), in_=X0[:], func=AF.Exp),
                                      r=[K0], w=[("Erow", d, hb)])
                                P.pe(lambda e, d=d, hb=hb, hsl=hsl, nU_=nU_: e.matmul(X0[:], lhsT=nU_[:], rhs=rhsB[d][:, hsl, :].rearrange("p r l -> p (r l)"),
                                                                                     start=False, stop=False, skip_group_check=True), r=[nUk, ("rhsB", d), ("Erow", d, hb)], w=[K0])
                                P.pe(lambda e, hb=hb, NM_=NM_: e.matmul(X0[:], lhsT=identf[:], rhs=NM_[:].rearrange("p r l -> p (r l)"),
                                                                       start=False, stop=True, skip_group_check=True), r=["identf"] + NMk, w=[K0])
                                P.act(lambda e, d=d, hb=hb, hsl=hsl: e.activation(out=dec[d][:, hsl, :].rearrange("p r l -> p (r l)"), in_=X0[:], func=AF.Exp),
                                      r=[K0], w=[("dec", d, hb)])
                            yield
                            for g in range(2):
                                P.pe(lambda e, g=g, tsl=tsl: e.matmul(X1[:, g * 128:(g + 1) * 128], lhsT=BTm[g][:, tsl],
                                                                      rhs=CT[:, tsl], start=True, stop=True),
                                     r=[("BTm", g, gi) for gi in range(9)] + CTALL, w=[K1])
                            for g in range(2):
                                P.dve(lambda e, d=d, g=g: e.tensor_tensor(
                                    out=MT[d][:, 4 * g:4 * g + 4, :], in0=dec[d][:, 4 * g:4 * g + 4, :],
                                    in1=X1[:, g * 128:(g + 1) * 128].unsqueeze(1).to_broadcast([128, 4, 128]), op=ALU.mult),
                                    r=[("dec", d, g), K1], w=[("MT", d, g)])
                            P.pe(lambda e, S__=S__, dA=dA: e.matmul(X1[:, 256:264], lhsT=S__[:], rhs=dA, start=True, stop=True),
                                 r=[Sk, ("dA_all", d)], w=[K1])
                            P.act(lambda e, d=d: e.activation(out=wdec[d][:], in_=X1[:, 256:264], func=AF.Exp), r=[K1], w=[("wdec", d)])
                            P.dve(lambda e, d=d, dtd=dtd: e.tensor_tensor(out=dtw[d][:], in0=wdec[d][:], in1=dtd, op=ALU.mult),
                                  r=[("wdec", d), ("dt_all", d)], w=[("dtw", d)])
                            xsv = xs_tm[:, t, :].rearrange("p (r x) -> p r x", x=64)
                            P.pool(lambda e, d=d, xsv=xsv, dtd=dtd: e.tensor_tensor(out=xdt[d][:], in0=xsv, in1=dtd.unsqueeze(2).to_broadcast([128, 8, 64]),
                                                                                   op=ALU.mult), r=XS(t) + [("dt_all", d)], w=[("xdt", d)])
                            P.dve(lambda e, d=d, xsv=xsv: e.tensor_tensor(out=xdtw[d][:], in0=xsv, in1=dtw[d][:].unsqueeze(2).to_broadcast([128, 8, 64]),
                                                                          op=ALU.mult), r=XS(t) + [("dtw", d)], w=[("xdtw", d)])
                            for g in range(2):
                                ps_ = slice(64 * g, 64 * g + 64)
                                P.pool(lambda e, d=d, g=g, ps_=ps_, tsl=tsl: e.tensor_tensor(
                                    out=CsT[d][ps_, g, :, :], in0=CT[ps_, tsl].unsqueeze(1).to_broadcast([64, 4, 128]),
                                    in1=Erow[d][ps_, 4 * g:4 * g + 4, :], op=ALU.mult),
                                    r=CTALL + [("Erow", d, g)], w=[("CsT", d, g)])
                            yield
                            for r8 in range(8):
                                g = r8 // 4
                                ps_ = slice(64 * g, 64 * g + 64)
                                ysl = slice(r8 * 64, (r8 + 1) * 64)
                                P.pe(lambda e, d=d, r8=r8, ysl=ysl: e.matmul(X2[:, ysl], lhsT=MT[d][:, r8, :], rhs=xdt[d][:, r8, :],
                                                                             start=True, stop=False),
                                     r=[("MT", d, g), ("xdt", d)], w=[K2])
                                P.pe(lambda e, d=d, r8=r8, ysl=ysl, g=g: e.matmul(X2[:, ysl], lhsT=CsT[d][:, g, r8 % 4, :], rhs=hSb[:, d, r8 % 4, :],
                                                                                 start=False, stop=True),
                                     r=[("CsT", d, g), ("hSb", d)], w=[K2])
                            if t not in seen:
                                seen.add(t)
                                P.act(lambda e, t=t: e.activation(out=Y[:, t, :], in_=X2[:], func=AF.Copy), r=[K2], w=[("Y", t)])
                            else:
                                P.dve(lambda e, t=t: e.tensor_tensor(out=Y[:, t, :], in0=X2[:], in1=Y[:, t, :], op=ALU.add),
                                      r=[K2, ("Y", t)], w=[("Y", t)])
                            yield
                            P.pe(lambda e, d=d, t=t: e.matmul(X1[:], lhsT=B_tm[:, t, :], rhs=xdtw[d][:].rearrange("p r x -> p (r x)"),
                                                              start=True, stop=True), r=[("B_tm", t), ("xdtw", d)], w=[K1])
                            for g in range(2):
                                ps_ = slice(64 * g, 64 * g + 64)
                                P.dve(lambda e, d=d, g=g, ps_=ps_, lastc=lastc: e.tensor_tensor(
                                    out=hS[ps_, d, :, :], in0=hS[ps_, d, :, :],
                                    in1=Erow[d][ps_, 4 * g:4 * g + 4, lastc:lastc + 1].to_broadcast([64, 4, 64]), op=ALU.mult),
                                    r=[("hS", d), ("Erow", d, g)], w=[("hS", d)])
                                P.dve(lambda e, d=d, g=g, ps_=ps_: e.tensor_tensor(
                                    out=hS[ps_, d, :, :], in0=hS[ps_, d, :, :],
                                    in1=X1[ps_, 256 * g:256 * g + 256].rearrange("p (r x) -> p r x", x=64), op=ALU.add),
                                    r=[("hS", d), K1], w=[("hS", d)])
                            P.pool(lambda e, d=d: e.tensor_copy(out=hSb[:, d, :, :], in_=hS[:, d, :, :]), r=[("hS", d)], w=[("hSb", d)])

                    def run_interleaved_s(fn):
                        for step in range(NT):
                            gens = [fn(d_, ORDER[d_][step]) for d_ in range(2)]
                            while gens:
                                for g_ in list(gens):
                                    try:
                                        next(g_)
                                    except StopIteration:
                                        gens.remove(g_)
                    run_interleaved_s(sproc)
                    zt_ = [sb(esC, f"zt_{i}", [128, 512], BF16) for i in range(2)]
                    yy = [sb(esC, f"yy{i}", [128, 512], F32) for i in range(2)]
                    ysq = sb(esC, "ysq", [128, 512], F32)
                    ssg = [sb(esC, f"ssg{i}", [128, 2], F32) for i in range(2)]
                    yo = [sb(esC, f"yo{i}", [128, 512], BF16) for i in range(2)]
                    for t in range(NT):
                        a = t % 2
                        rows = slice(t * 128, (t + 1) * 128)
                        P.dma("sp", lambda e, a=a, rows=rows: e.dma_start(out=zt_[a][:], in_=zs[rows, :]), r=[("zs", t)], w=[("zt_", a)])
                        P.dve(lambda e, a=a, t=t: e.tensor_tensor(out=yy[a][:].rearrange("p (r x) -> p r x", x=64),
                                                                  in0=xs_tm[:, t, :].rearrange("p (r x) -> p r x", x=64),
                                                                  in1=dsk[:].unsqueeze(2).to_broadcast([128, 8, 64]), op=ALU.mult),
                              r=XS(t) + ["dsk"], w=[("yy", a)])
                        P.pool(lambda e, a=a, t=t: e.tensor_tensor(out=yy[a][:], in0=yy[a][:], in1=Y[:, t, :], op=ALU.add),
                               r=[("yy", a), ("Y", t)], w=[("yy", a)])
                        P.dve(lambda e, a=a: e.tensor_tensor(out=yy[a][:], in0=yy[a][:], in1=zt_[a][:], op=ALU.mult),
                              r=[("yy", a), ("zt_", a)], w=[("yy", a)])
                        for g in range(2):
                            P.act(lambda e, a=a, g=g: e.activation(out=ysq[:, g * 256:(g + 1) * 256], in_=yy[a][:, g * 256:(g + 1) * 256],
                                                                   func=AF.Square, accum_out=ssg[a][:, g:g + 1]),
                                  r=[("yy", a)], w=["ysq", ("ssg", a)])
                        rsqrt_ops(ssg[a][:], ssg[a][:], 1.0 / 256, [("ssg", a)], ("ssg", a))
                        P.dve(lambda e, a=a: e.tensor_tensor(out=yy[a][:].rearrange("p (g x) -> p g x", x=256),
                                                             in0=yy[a][:].rearrange("p (g x) -> p g x", x=256),
                                                             in1=ssg[a][:].unsqueeze(2).to_broadcast([128, 2, 256]), op=ALU.mult),
                              r=[("yy", a), ("ssg", a)], w=[("yy", a)])
                        P.pool(lambda e, a=a: e.tensor_tensor(out=yo[a][:], in0=yy[a][:], in1=nw[:], op=ALU.mult),
                               r=[("yy", a), "snw"], w=[("yo", a)])
                        P.dma("pool", lambda e, a=a, rows=rows: e.dma_start(out=ml[rows, 256:768], in_=yo[a][:]), r=[("yo", a)], w=[("ml_s", t)])
            P.barrier()

        def phase_gdn(l):
            with contextlib.ExitStack() as esA:
                qT = sb(esA, "qT", [128, 2, T], BF16)
                kTm = [sb(esA, f"kTm{i}", [128, 2, T], BF16) for i in range(2)]
                P.pool(lambda e: e.memset(kTm[0][64:128, :, :], 0.0), w=["kTm0z"])
                P.pool(lambda e: e.memset(kTm[1][0:64, :, :], 0.0), w=["kTm1z"])
                k_tm = sb(esA, "k_tm", [128, NT, 256], BF16)
                v_tm = sb(esA, "v_tm", [128, NT, 256], BF16)
                O = sb(esA, "O", [128, NT, 256], BF16)
                beta_all = sb(esA, "beta_all", [128, NT, 8], F32)
                g_all = sb(esA, "g_all", [128, NT, 8], F32)
                S = sb(esA, "S", [128, 2, 2, 64], F32)
                Sb = sb(esA, "Sbf", [128, 2, 2, 2, 64], BF16)
                gdtb = bcast_load(esA, "gdtb", gdn_dt_bias[l].rearrange("a b -> (a b)"), 8)
                galg = bcast_load(esA, "galg", gdn_a_log[l].rearrange("a b -> (a b)"), 8)
                gnw = bcast_load(esA, "gnw", gdn_norm_w[l], 64)
                SM = [("small", t) for t in range(NT)]
                P.act(lambda e: e.activation(out=galg[:], in_=galg[:], func=AF.Exp), r=["galg"], w=["galg"])
                P.dve(lambda e: e.tensor_scalar(out=galg[:], in0=galg[:], scalar1=-1.0, scalar2=None, op0=ALU.mult), r=["galg"], w=["galg"])
                P.act(lambda e: e.activation(out=beta_all[:], in_=small_all[:, :, 8:16], func=AF.Sigmoid), r=SM, w=["beta_all"])
                P.dve(lambda e: e.tensor_tensor(out=g_all[:], in0=small_all[:, :, 16:24], in1=gdtb[:].unsqueeze(1).to_broadcast([128, NT, 8]),
                                                op=ALU.add), r=SM + ["gdtb"], w=["g_all"])
                P.act(lambda e: e.activation(out=g_all[:], in_=g_all[:], func=AF.Exp), r=["g_all"], w=["g_all"])
                P.act(lambda e: e.activation(out=g_all[:], in_=g_all[:], func=AF.Ln, bias=onesf[:, 0:1], scale=1.0), r=["g_all", "onesf"], w=["g_all"])
                P.dve(lambda e: e.tensor_tensor(out=g_all[:], in0=g_all[:], in1=galg[:].unsqueeze(1).to_broadcast([128, NT, 8]), op=ALU.mult),
                      r=["g_all", "galg"], w=["g_all"])
                P.pool(lambda e: e.memset(S[:], 0.0), w=[("S", 0), ("S", 1)])
                P.pool(lambda e: e.memset(Sb[:], 0.0), w=[("Sb", 0), ("Sb", 1)])
                with contextlib.ExitStack() as esB:
                    cw = sb(esB, "gcw", [128, 6, 5], F32)
                    dg = sb(esB, "gdg", [128, 30, 128], BF16)
                    hc = [sb(esB, f"ghc{i}", [128, T], BF16) for i in range(2)]
                    cst = [sb(esB, f"gcst{i}", [128, 512], BF16) for i in range(2)]
                    xf = [sb(esB, f"gxf{i}", [128, 512], F32) for i in range(2)]
                    sq = [sb(esB, f"gsq{i}", [128, 512], F32) for i in range(2)]
                    rs = [sb(esB, f"grs{i}", [128, 512], F32) for i in range(2)]
                    xnst = [sb(esB, f"gxnst{i}", [128, 512], BF16) for i in range(2)]
                    P.dma("sp", lambda e: e.dma_start(out=cw[:], in_=gdn_conv_wT[l]), w=["gcw"])
                    for ch in range(6):
                        for jt in range(5):
                            P.dve(lambda e, ch=ch, jt=jt: e.tensor_scalar(out=dg[:, ch * 5 + jt, :], in0=identf[:], scalar1=cw[:, ch, jt:jt + 1],
                                                                          scalar2=None, op0=ALU.mult), r=["identf", "gcw"], w=[("gdg", ch)])
                    cnt = 0
                    for ch in range(6):
                        hs_ = ch % 2
                        P.dma("sp", lambda e, hs_=hs_, ch=ch: e.dma_start(out=hc[hs_][:], in_=hT[6 + ch]),
                              r=[("hT", 6 + ch, gi) for gi in range(9)], w=[("ghc", hs_)])
                        for gidx, (t0, nt) in enumerate(GROUPS):
                            N = nt * 128
                            tok0 = t0 * 128
                            seg_lo, seg_hi = (0, 256) if gidx == 0 else (256, T)
                            bk = cnt % 2
                            cnt += 1
                            for ii, jt in enumerate([2, 0, 1, 3, 4]):
                                o = jt - 2
                                a_ = max(0, seg_lo - tok0 - o)
                                b_ = min(N, seg_hi - tok0 - o)
                                P.pe(lambda e, bk=bk, ch=ch, jt=jt, a_=a_, b_=b_, o=o, tok0=tok0, hs_=hs_, ii=ii: e.matmul(
                                    banks[bk][:, a_:b_], lhsT=dg[:, ch * 5 + jt, :], rhs=hc[hs_][:, tok0 + a_ + o:tok0 + b_ + o],
                                    start=(ii == 0), stop=(ii == 4)), r=[("gdg", ch), ("ghc", hs_)], w=[BK(bk)])
                            if ch < 4:
                                P.act(lambda e, bk=bk, N=N: e.activation(out=xf[bk][:, 0:N], in_=banks[bk][:, 0:N], func=AF.Silu),
                                      r=[BK(bk)], w=[("gxf", bk)])
                                P.pool(lambda e, bk=bk, N=N: e.tensor_tensor(out=sq[bk][:, 0:N], in0=xf[bk][:, 0:N], in1=xf[bk][:, 0:N], op=ALU.mult),
                                       r=[("gxf", bk)], w=[("gsq", bk)])
                                b2 = 2 + bk
                                P.pe(lambda e, bk=bk, b2=b2, N=N: e.matmul(banks[b2][:, 0:N], lhsT=blkf[:], rhs=sq[bk][:, 0:N], start=True, stop=True),
                                     r=["blkf", ("gsq", bk)], w=[BK(b2)])
                                P.dve(lambda e, bk=bk, b2=b2, N=N: e.tensor_scalar(out=rs[bk][:, 0:N], in0=banks[b2][:, 0:N], scalar1=EPS, scalar2=None,
                                                                                  op0=ALU.add), r=[BK(b2)], w=[("grs", bk)])
                                P.act(lambda e, bk=bk, N=N: e.activation(out=rs[bk][:, 0:N], in_=rs[bk][:, 0:N], func=AF.Sqrt), r=[("grs", bk)], w=[("grs", bk)])
                                P.dve(lambda e, bk=bk, N=N: e.reciprocal(out=rs[bk][:, 0:N], in_=rs[bk][:, 0:N]), r=[("grs", bk)], w=[("grs", bk)])
                                dstT = (qT[:, ch, tok0:tok0 + N] if ch < 2 else xnst[bk][:, 0:N])
                                dkey = ("qT", ch, gidx) if ch < 2 else ("gxnst", bk)
                                sc_ = 0.125 if ch < 2 else 1.0
                                P.dve(lambda e, bk=bk, N=N, dstT=dstT, sc_=sc_: e.scalar_tensor_tensor(
                                    out=dstT, in0=xf[bk][:, 0:N], scalar=sc_, in1=rs[bk][:, 0:N], op0=ALU.mult, op1=ALU.mult),
                                    r=[("gxf", bk), ("grs", bk)], w=[dkey])
                                if ch >= 2:
                                    for g_ in range(2):
                                        pg = slice(64 * g_, 64 * g_ + 64)
                                        P.pool(lambda e, g_=g_, pg=pg, bk=bk, ch=ch, tok0=tok0, N=N: e.tensor_copy(
                                            out=kTm[g_][pg, ch - 2, tok0:tok0 + N], in_=xnst[bk][pg, 0:N]),
                                            r=[dkey, f"kTm{g_}z"], w=[("kTm", g_, ch - 2, gidx)])
                                    for ti in range(nt):
                                        t = t0 + ti
                                        tb_ = 6 + (t % 2)
                                        P.pe(lambda e, tb_=tb_, ti=ti, bk=bk: e.transpose(out=tbanks[tb_][:, 0:128], in_=xnst[bk][:, ti * 128:(ti + 1) * 128],
                                                                                         identity=identb[:]), r=[dkey, "identb"], w=[BK(tb_)])
                                        P.dve(lambda e, tb_=tb_, t=t, ch=ch: e.tensor_copy(out=k_tm[:, t, (ch - 2) * 128:(ch - 1) * 128], in_=tbanks[tb_][:, 0:128]),
                                              r=[BK(tb_)], w=[("k_tm", t, ch - 2)])
                            else:
                                P.act(lambda e, bk=bk, N=N: e.activation(out=cst[bk][:, 0:N], in_=banks[bk][:, 0:N], func=AF.Silu),
                                      r=[BK(bk)], w=[("gcst", bk)])
                                for ti in range(nt):
                                    t = t0 + ti
                                    tb_ = 6 + (t % 2)
                                    P.pe(lambda e, tb_=tb_, bk=bk, ti=ti: e.transpose(out=tbanks[tb_][:, 0:128], in_=cst[bk][:, ti * 128:(ti + 1) * 128],
                                                                                     identity=identb[:]), r=[("gcst", bk), "identb"], w=[BK(tb_)])
                                    P.dve(lambda e, tb_=tb_, t=t, ch=ch: e.tensor_copy(out=v_tm[:, t, (ch - 4) * 128:(ch - 3) * 128], in_=tbanks[tb_][:, 0:128]),
                                          r=[BK(tb_)], w=[("v_tm", t, ch - 4)])
                P.barrier()
                if stop_after == "gconv":
                    return
                with contextlib.ExitStack() as esC:
                    def mk(name, shape, dt):
                        return [sb(esC, f"{name}{i}", shape, dt) for i in range(2)]
                    rhs1 = mk("g_rhs1", [128, 4, 128], F32)
                    rhs2 = mk("g_rhs2", [128, 4, 128], F32)
                    gmask = mk("g_gmask", [128, 4, 2], F32)
                    decp = mk("g_decp", [128, 4, 128], F32)
                    esm = mk("g_esm", [128, 16], F32)
                    tmpA = rhs1
                    nbo = rhs2
                    nb4 = mk("g_nb4", [128, 4], F32)
                    beg = mk("g_beg", [128, 4], F32)
                    Nm = [mk("g_Nm_a", [128, 4, 128], BF16)]
                    Ym = [mk("g_Ym_a", [128, 4, 128], BF16)]
                    Wm = [mk("g_Wm_a", [128, 4, 128], BF16), mk("g_Wm_b", [128, 4, 128], BF16)]
                    Tm = [mk("g_Tm_a", [128, 4, 128], BF16), mk("g_Tm_b", [128, 4, 128], BF16)]
                    NMc = mk("g_NMc", [128, 4, 128], BF16)
                    YMc = mk("g_YMc", [128, 4, 128], BF16)
                    P1s = mk("g_P1s", [128, 4, 128], BF16)
                    P2s = mk("g_P2s", [128, 4, 128], BF16)
                    M4 = [sb(esC, f"g_M4_{j}", [128, 4, 128], BF16) for j in range(6)]
                    I4 = sb(esC, "g_I4", [128, 4, 128], BF16)
                    blk_a = sb(esC, "g_blka", [128, 128], F32)
                    blk_b = sb(esC, "g_blkb", [128, 128], F32)
                    mtmp = sb(esC, "g_mtmp", [128, 128], F32)

                    def mk_blk(t_, n):
                        v_ = t_[:].rearrange("p (a b) -> p a b", b=n)
                        P.pool(lambda e: e.memset(t_[:], 1.0), w=[id(t_)])
                        P.pool(lambda e: e.affine_select(out=v_, in_=v_, compare_op=ALU.is_ge, fill=0.0, base=0,
                                                         pattern=[[-n, 128 // n], [0, n]], channel_multiplier=1), r=[id(t_)], w=[id(t_)])
                        P.pool(lambda e: e.affine_select(out=v_, in_=v_, compare_op=ALU.is_ge, fill=0.0, base=n - 1,
                                                         pattern=[[n, 128 // n], [0, n]], channel_multiplier=-1), r=[id(t_)], w=[id(t_)])
                    prev, cur = blk_a, blk_b
                    mk_blk(prev, 1)
                    for j in range(6):
                        mk_blk(cur, 2 << j)
                        P.pool(lambda e, prev=prev, cur=cur: e.tensor_tensor(out=mtmp[:], in0=cur[:], in1=prev[:], op=ALU.subtract),
                               r=[id(prev), id(cur)], w=["g_mtmp"])
                        for h in range(4):
                            P.pool(lambda e, j=j, h=h: e.tensor_copy(out=M4[j][:, h, :], in_=mtmp[:]), r=["g_mtmp"], w=["M4"])
                        prev, cur = cur, prev
                    for h in range(4):
                        P.pool(lambda e, h=h: e.tensor_copy(out=I4[:, h, :], in_=identb[:]), r=["identb"], w=["I4"])
                    QKm = mk("g_QKm", [128, 4, 128], BF16)
                    QKmT = mk("g_QKmT", [128, 4, 128], BF16)
                    kbg = mk("g_kbg", [128, 4, 64], BF16)
                    kend = mk("g_kend", [128, 4, 64], BF16)
                    vb = mk("g_vb", [128, 4, 64], BF16)
                    u_sb = mk("g_u", [128, 4, 64], F32)
                    wT_sb = mk("g_wT", [128, 2, 128], BF16)
                    vnewc = [mk("g_vnew_a", [128, 4, 64], BF16), mk("g_vnew_b", [128, 4, 64], BF16)]
                    for ci_ in range(2):
                        for d_ in range(2):
                            P.pool(lambda e, ci_=ci_, d_=d_: e.memset(vnewc[ci_][d_][:], 0.0), w=[("vnewc", ci_, d_)])
                    o1 = mk("g_o1", [128, 4, 64], F32)
                    QT_ALL = [("qT", c, gi) for c in range(2) for gi in range(9)]
                    KT_ALL = [("kTm", g_, c, gi) for g_ in range(2) for c in range(2) for gi in range(9)]
                    seen = set()
                    v4 = lambda bk: bk[:].rearrange("p (h s) -> p h s", s=128)

                    def gproc(d, t):
                        if True:
                            b0, b1, b2_ = banks[3 * d], banks[3 * d + 1], banks[3 * d + 2]
                            b3, b4, b5 = b0, b1, b2_
                            BKd = lambda i: BK(3 * d + (i % 3)) if i < 6 else BK(6 + d)
                            tsl = slice(t * 128, (t + 1) * 128)
                            UB_, nUB_, SB_, NMB_ = (UBf, nUBf, SBf, NMBf) if d == 0 else (UBb, nUBb, SBb, NMBb)
                            UBk, nUBk, SBk, NMBk = ("UBf", "nUBf", "SBf", NMBf_keys) if d == 0 else ("UBb", "nUBb", "SBb", NMBb_keys)
                            g4 = g_all[:, t, 4 * d:4 * d + 4]
                            bt4 = beta_all[:, t, 4 * d:4 * d + 4]
                            KD = lambda n: (n, d)
                            P.dve(lambda e, d=d, nUB_=nUB_, g4=g4: e.tensor_tensor(
                                out=rhs1[d][:], in0=nUB_[:].unsqueeze(1).to_broadcast([128, 4, 128]),
                                in1=g4.unsqueeze(2).to_broadcast([128, 4, 128]), op=ALU.mult), r=[nUBk, "g_all"], w=[KD("rhs1")])
                            P.dve(lambda e, d=d, g4=g4: e.tensor_copy(out=rhs2[d][:], in_=g4.unsqueeze(2).to_broadcast([128, 4, 128])),
                                  r=["g_all"], w=[KD("rhs2")])
                            P.dve(lambda e, d=d, g4=g4: e.tensor_tensor(
                                out=gmask[d][:], in0=g4.unsqueeze(2).to_broadcast([128, 4, 2]),
                                in1=chunkind[:].unsqueeze(1).to_broadcast([128, 4, 2]), op=ALU.mult), r=["g_all", "chunkind"], w=[KD("gmask")])
                            P.pe(lambda e, d=d: e.matmul(b0[:], lhsT=onesf[:], rhs=rhs1[d][:].rearrange("p h s -> p (h s)"), start=True, stop=False),
                                 r=["onesf", KD("rhs1")], w=[BKd(0)])
                            P.pe(lambda e, d=d, UB_=UB_: e.matmul(b0[:], lhsT=UB_[:], rhs=rhs2[d][:].rearrange("p h s -> p (h s)"), start=False, stop=False),
                                 r=[UBk, KD("rhs2")], w=[BKd(0)])
                            P.pe(lambda e, NMB_=NMB_: e.matmul(b0[:], lhsT=identf[:], rhs=NMB_[:].rearrange("p h s -> p (h s)"), start=False, stop=True),
                                 r=["identf"] + NMBk, w=[BKd(0)])
                            for h in range(4):
                                c, hh = h // 2, h % 2
                                hp = slice(64 * hh, 64 * hh + 64)
                                P.pe(lambda e, h=h, c=c, hh=hh, tsl=tsl: e.matmul(b1[:, h * 128:(h + 1) * 128], lhsT=kTm[hh][:, c, tsl], rhs=kTm[hh][:, c, tsl],
                                                                                 start=True, stop=True), r=KT_ALL, w=[BKd(1)])
                            for h in range(4):
                                c, hh = h // 2, h % 2
                                hp = slice(64 * hh, 64 * hh + 64)
                                P.pe(lambda e, h=h, c=c, hh=hh, tsl=tsl: e.matmul(b2_[:, h * 128:(h + 1) * 128], lhsT=qT[:, c, tsl], rhs=kTm[hh][:, c, tsl],
                                                                                 start=True, stop=True), r=KT_ALL + QT_ALL, w=[BKd(2)])
                            P.act(lambda e, d=d: e.activation(out=decp[d][:].rearrange("p h s -> p (h s)"), in_=b0[:], func=AF.Exp), r=[BKd(0)], w=[KD("decp")])
                            P.pe(lambda e, UB_=UB_, g4=g4: e.matmul(b3[:, 0:4], lhsT=UB_[:], rhs=g4, start=True, stop=True), r=[UBk, "g_all"], w=[BKd(3)])
                            P.pe(lambda e, SB_=SB_, g4=g4: e.matmul(b3[:, 4:8], lhsT=SB_[:], rhs=g4, start=True, stop=True), r=[SBk, "g_all"], w=[BKd(3)])
                            P.pe(lambda e, d=d: e.matmul(b3[:, 8:16], lhsT=onesf[:], rhs=gmask[d][:].rearrange("p h c -> p (h c)"), start=True, stop=True),
                                 r=["onesf", KD("gmask")], w=[BKd(3)])
                            P.act(lambda e, d=d: e.activation(out=esm[d][:], in_=b3[:, 0:16], func=AF.Exp), r=[BKd(3)], w=[KD("esm")])
                            P.dve(lambda e, d=d: e.tensor_tensor(out=tmpA[d][:], in0=v4(b1), in1=decp[d][:], op=ALU.mult), r=[BKd(1), KD("decp")], w=[KD("rhs1")])
                            P.dve(lambda e, d=d, bt4=bt4: e.tensor_scalar(out=nb4[d][:], in0=bt4, scalar1=-1.0, scalar2=None, op0=ALU.mult),
                                  r=["beta_all"], w=[KD("nb4")])
                            P.dve(lambda e, d=d: e.tensor_tensor(out=nbo[d][:], in0=offd[:].unsqueeze(1).to_broadcast([128, 4, 128]),
                                                                 in1=nb4[d][:].unsqueeze(2).to_broadcast([128, 4, 128]), op=ALU.mult),
                                  r=["offd", KD("nb4")], w=[KD("rhs2")])
                            P.dve(lambda e, d=d: e.tensor_tensor(out=Nm[0][d][:], in0=tmpA[d][:], in1=nbo[d][:], op=ALU.mult),
                                  r=[KD("rhs1"), KD("rhs2")], w=[("Nm", 0, d)])
                            P.dve(lambda e, d=d: e.tensor_tensor(out=QKm[d][:], in0=v4(b2_), in1=decp[d][:], op=ALU.mult), r=[BKd(2), KD("decp")], w=[KD("QKm")])
                            P.dve(lambda e, d=d, bt4=bt4: e.tensor_tensor(out=beg[d][:], in0=bt4, in1=esm[d][:, 0:4], op=ALU.mult),
                                  r=["beta_all", KD("esm")], w=[KD("beg")])
                            ktv = k_tm[:, t, :].rearrange("p (h x) -> p h x", x=64)
                            vtv = v_tm[:, t, :].rearrange("p (h x) -> p h x", x=64)
                            KTM = [("k_tm", t, 0), ("k_tm", t, 1)]
                            VTM = [("v_tm", t, 0), ("v_tm", t, 1)]
                            P.dve(lambda e, d=d, ktv=ktv: e.tensor_tensor(out=kbg[d][:], in0=ktv, in1=beg[d][:].unsqueeze(2).to_broadcast([128, 4, 64]),
                                                                          op=ALU.mult), r=KTM + [KD("beg")], w=[KD("kbg")])
                            P.dve(lambda e, d=d, ktv=ktv: e.tensor_tensor(out=kend[d][:], in0=ktv, in1=esm[d][:, 4:8].unsqueeze(2).to_broadcast([128, 4, 64]),
                                                                          op=ALU.mult), r=KTM + [KD("esm")], w=[KD("kend")])
                            P.dve(lambda e, d=d, vtv=vtv, bt4=bt4: e.tensor_tensor(out=vb[d][:], in0=vtv, in1=bt4.unsqueeze(2).to_broadcast([128, 4, 64]),
                                                                                  op=ALU.mult), r=VTM + ["beta_all"], w=[KD("vb")])
                            tv6 = tbanks[6 + d][:, 0:512].rearrange("p (h s) -> p h s", s=128)
                            tv7 = tbanks[6 + d][:, 512:1024].rearrange("p (h s) -> p h s", s=128)
                            for h in range(4):
                                P.pe(lambda e, d=d, h=h, tv6=tv6: e.transpose(out=tv6[:, h, :], in_=Nm[0][d][:, h, :], identity=identb[:]),
                                     r=[("Nm", 0, d), "identb"], w=[BKd(6)])
                            for h in range(4):
                                P.pe(lambda e, d=d, h=h, tv7=tv7: e.transpose(out=tv7[:, h, :], in_=QKm[d][:, h, :], identity=identb[:]),
                                     r=[KD("QKm"), "identb"], w=[BKd(7)])
                            P.act(lambda e, d=d, tv6=tv6: e.activation(out=Ym[0][d][:], in_=tv6, func=AF.Copy), r=[BKd(6)], w=[("Ym", 0, d)])
                            P.act(lambda e, d=d, tv7=tv7: e.activation(out=QKmT[d][:], in_=tv7, func=AF.Copy), r=[BKd(7)], w=[KD("QKmT")])
                            yield
                            P.dve(lambda e, d=d: e.tensor_tensor(out=NMc[d][:], in0=Nm[0][d][:], in1=M4[0][:], op=ALU.mult),
                                  r=[("Nm", 0, d), "M4"], w=[KD("NMc")])
                            P.pool(lambda e, d=d: e.tensor_tensor(out=YMc[d][:], in0=Ym[0][d][:], in1=M4[0][:], op=ALU.mult),
                                   r=[("Ym", 0, d), "M4"], w=[KD("YMc")])
                            P.dve(lambda e, d=d: e.tensor_tensor(out=Wm[1][d][:], in0=YMc[d][:], in1=I4[:], op=ALU.add), r=[KD("YMc"), "I4"], w=[("Wm", 1, d)])
                            P.pool(lambda e, d=d: e.tensor_tensor(out=Tm[1][d][:], in0=NMc[d][:], in1=I4[:], op=ALU.add), r=[KD("NMc"), "I4"], w=[("Tm", 1, d)])
                            for j in range(2, 7):
                                pp, pn = (j - 1) % 2, j % 2
                                lastj = (j == 6)
                                P.dve(lambda e, d=d, j=j: e.tensor_tensor(out=NMc[d][:], in0=Nm[0][d][:], in1=M4[j - 1][:], op=ALU.mult),
                                      r=[("Nm", 0, d), "M4"], w=[KD("NMc")])
                                if not lastj:
                                    P.pool(lambda e, d=d, j=j: e.tensor_tensor(out=YMc[d][:], in0=Ym[0][d][:], in1=M4[j - 1][:], op=ALU.mult),
                                           r=[("Ym", 0, d), "M4"], w=[KD("YMc")])
                                for h in range(4):
                                    P.pe(lambda e, d=d, h=h, pp=pp: e.matmul(b0[:, h * 128:(h + 1) * 128], lhsT=NMc[d][:, h, :], rhs=Wm[pp][d][:, h, :],
                                                                            start=True, stop=True), r=[KD("NMc"), ("Wm", pp, d)], w=[BKd(0)])
                                P.act(lambda e, d=d: e.activation(out=P1s[d][:], in_=v4(b0), func=AF.Copy), r=[BKd(0)], w=[KD("P1s")])
                                if not lastj:
                                    for h in range(4):
                                        P.pe(lambda e, d=d, h=h, pp=pp: e.matmul(b1[:, h * 128:(h + 1) * 128], lhsT=YMc[d][:, h, :], rhs=Tm[pp][d][:, h, :],
                                                                                start=True, stop=True), r=[KD("YMc"), ("Tm", pp, d)], w=[BKd(1)])
                                    P.dve(lambda e, d=d: e.tensor_copy(out=P2s[d][:], in_=v4(b1)), r=[BKd(1)], w=[KD("P2s")])
                                for h in range(4):
                                    P.pe(lambda e, d=d, h=h, pp=pp: e.matmul(b2_[:, h * 128:(h + 1) * 128], lhsT=identb[:], rhs=Wm[pp][d][:, h, :],
                                                                            start=True, stop=False), r=["identb", ("Wm", pp, d)], w=[BKd(2)])
                                    P.pe(lambda e, d=d, h=h, pp=pp: e.matmul(b2_[:, h * 128:(h + 1) * 128], lhsT=Tm[pp][d][:, h, :], rhs=P1s[d][:, h, :],
                                                                            start=False, stop=True), r=[("Tm", pp, d), KD("P1s")], w=[BKd(2)])
                                P.act(lambda e, d=d, pn=pn: e.activation(out=Wm[pn][d][:], in_=v4(b2_), func=AF.Copy), r=[BKd(2)], w=[("Wm", pn, d)])
                                if not lastj:
                                    for h in range(4):
                                        P.pe(lambda e, d=d, h=h, pp=pp: e.matmul(b0[:, h * 128:(h + 1) * 128], lhsT=identb[:], rhs=Tm[pp][d][:, h, :],
                                                                                start=True, stop=False), r=["identb", ("Tm", pp, d)], w=[BKd(0)])
                                        P.pe(lambda e, d=d, h=h, pp=pp: e.matmul(b0[:, h * 128:(h + 1) * 128], lhsT=Wm[pp][d][:, h, :], rhs=P2s[d][:, h, :],
                                                                                start=False, stop=True), r=[("Wm", pp, d), KD("P2s")], w=[BKd(0)])
                                    P.dve(lambda e, d=d, pn=pn: e.tensor_copy(out=Tm[pn][d][:], in_=v4(b0)), r=[BKd(0)], w=[("Tm", pn, d)])
                                yield
                            TT = Wm[0][d]
                            TTk = ("Wm", 0, d)
                            yield
                            for h in range(4):
                                P.pe(lambda e, d=d, h=h, TT=TT: e.matmul(b3[:, 256 + h * 64:256 + (h + 1) * 64], lhsT=TT[:, h, :], rhs=vb[d][:, h, :],
                                                                        start=True, stop=True), r=[TTk, KD("vb")], w=[BKd(3)])
                            for h in range(4):
                                c = h // 2
                                P.pe(lambda e, d=d, h=h, c=c, TT=TT: e.matmul(
                                    b4[:, h * 128:(h + 1) * 128], lhsT=kbg[d][:, 2 * c:2 * c + 2, :].rearrange("p a x -> p (a x)"), rhs=TT[:, h, :],
                                    start=True, stop=True), r=[TTk, KD("kbg")], w=[BKd(4)])
                            P.act(lambda e, d=d: e.activation(out=u_sb[d][:].rearrange("p h x -> p (h x)"), in_=b3[:, 256:512], func=AF.Copy),
                                  r=[BKd(3)], w=[KD("u")])
                            for hh in range(2):
                                hp = slice(64 * hh, 64 * hh + 64)
                                P.dve(lambda e, d=d, hh=hh, hp=hp: e.tensor_copy(out=wT_sb[d][hp, :, :], in_=v4(b4)[hp, hh::2, :]), r=[BKd(4)], w=[KD("wT")])
                            yield
                            for ci in ((0, 1) if d == 0 else (1, 0)):
                                rows = slice(64 * ci, 64 * ci + 64)
                                for h in range(4):
                                    c, hh = h // 2, h % 2
                                    hp = slice(64 * hh, 64 * hh + 64)
                                    P.pe(lambda e, d=d, h=h, c=c, hh=hh: e.matmul(b5[:, h * 64:(h + 1) * 64], lhsT=wT_sb[d][:, c, :], rhs=Sb[:, hh, d, c, :],
                                                                                 start=True, stop=True), r=[KD("wT"), ("Sb", d)], w=[BKd(5)])
                                for h in range(4):
                                    c, hh = h // 2, h % 2
                                    hp = slice(64 * hh, 64 * hh + 64)
                                    P.pe(lambda e, d=d, h=h, c=c, hh=hh, tsl=tsl: e.matmul(b5[:, 256 + h * 64:256 + (h + 1) * 64], lhsT=qT[:, c, tsl],
                                                                                          rhs=Sb[:, hh, d, c, :], start=True, stop=True),
                                         r=QT_ALL + [("Sb", d)], w=[BKd(5)])
                                P.dve(lambda e, d=d, rows=rows, ci=ci: e.tensor_tensor(
                                    out=vnewc[ci][d][rows, :, :], in0=u_sb[d][rows, :, :], in1=b5[rows, 0:256].rearrange("p (h x) -> p h x", x=64),
                                    op=ALU.subtract), r=[KD("u"), BKd(5)], w=[("vnewc", ci, d)])
                                P.dve(lambda e, d=d, rows=rows: e.tensor_tensor(
                                    out=o1[d][rows, :, :], in0=b5[rows, 256:512].rearrange("p (h x) -> p h x", x=64),
                                    in1=esm[d][rows, 0:4].unsqueeze(2).to_broadcast([64, 4, 64]), op=ALU.mult),
                                    r=[KD("esm"), BKd(5)], w=[KD("o1")])
                                for h in range(4):
                                    c = h // 2
                                    P.pe(lambda e, d=d, h=h, c=c, ci=ci: e.matmul(
                                        b4[:, h * 64:(h + 1) * 64], lhsT=kend[d][:, 2 * c:2 * c + 2, :].rearrange("p a x -> p (a x)"),
                                        rhs=vnewc[ci][d][:, h, :], start=True, stop=True), r=[KD("kend"), ("vnewc", ci, d)], w=[BKd(4)])
                                for hh in range(2):
                                    hp = slice(64 * hh, 64 * hh + 64)
                                    gv = esm[d][hp, 8:16].rearrange("p (h c) -> p h c", c=2)[:, hh::2, ci:ci + 1]
                                    P.dve(lambda e, d=d, hp=hp, gv=gv: e.tensor_tensor(out=S[hp, d, :, :], in0=S[hp, d, :, :],
                                                                                      in1=gv.to_broadcast([64, 2, 64]), op=ALU.mult),
                                          r=[("S", d), KD("esm")], w=[("S", d)])
                                    P.dve(lambda e, d=d, hp=hp, hh=hh: e.tensor_tensor(
                                        out=S[hp, d, :, :], in0=S[hp, d, :, :],
                                        in1=b4[hp, 0:256].rearrange("p (h x) -> p h x", x=64)[:, hh::2, :], op=ALU.add),
                                        r=[("S", d), BKd(4)], w=[("S", d)])
                                for hh in range(2):
                                    hp = slice(64 * hh, 64 * hh + 64)
                                    P.pool(lambda e, d=d, hh=hh, hp=hp: e.tensor_copy(out=Sb[hp, hh, d, :, :], in_=S[hp, d, :, :]), r=[("S", d)], w=[("Sb", d)])
                                yield
                            yield
                            for h in range(4):
                                for ci_ in range(2):
                                    P.pe(lambda e, d=d, h=h, ci_=ci_: e.matmul(b3[:, h * 64:(h + 1) * 64], lhsT=QKmT[d][:, h, :], rhs=vnewc[ci_][d][:, h, :],
                                                                              start=(ci_ == 0), stop=(ci_ == 1)),
                                         r=[KD("QKmT"), ("vnewc", 0, d), ("vnewc", 1, d)], w=[BKd(3)])
                            Ov = O[:, t, :]
                            if t not in seen:
                                seen.add(t)
                                P.dve(lambda e, d=d, Ov=Ov: e.tensor_tensor(out=Ov, in0=b3[:, 0:256], in1=o1[d][:].rearrange("p h x -> p (h x)"), op=ALU.add),
                                      r=[BKd(3), KD("o1")], w=[("O", t)])
                            else:
                                P.dve(lambda e, d=d: e.tensor_tensor(out=o1[d][:].rearrange("p h x -> p (h x)"), in0=b3[:, 0:256],
                                                                     in1=o1[d][:].rearrange("p h x -> p (h x)"), op=ALU.add),
                                      r=[BKd(3), KD("o1")], w=[KD("o1")])
                                P.dve(lambda e, d=d, Ov=Ov: e.tensor_tensor(out=Ov, in0=o1[d][:].rearrange("p h x -> p (h x)"), in1=Ov, op=ALU.add),
                                      r=[KD("o1"), ("O", t)], w=[("O", t)])

                    def run_interleaved(fn):
                        for step in range(NT):
                            gens = [fn(d_, ORDER[d_][step]) for d_ in range(2)]
                            while gens:
                                for g_ in list(gens):
                                    try:
                                        next(g_)
                                    except StopIteration:
                                        gens.remove(g_)
                    run_interleaved(gproc)
                    gt_ = mk("g_gt", [128, 256], BF16)
                    of = mk("g_of", [128, 4, 64], F32)
                    osq = sb(esC, "g_osq", [128, 4, 64], F32)
                    oss = mk("g_oss", [128, 4], F32)
                    oo = mk("g_oo", [128, 256], BF16)
                    for t in range(NT):
                        a = t % 2
                        rows = slice(t * 128, (t + 1) * 128)
                        P.dma("sp", lambda e, a=a, rows=rows: e.dma_start(out=gt_[a][:], in_=gs[rows, :]), r=[("gs", t)], w=[("g_gt", a)])
                        P.pool(lambda e, a=a, t=t: e.tensor_tensor(out=osq[:].rearrange("p h x -> p (h x)"), in0=O[:, t, :], in1=O[:, t, :], op=ALU.mult),
                               r=[("O", t)], w=["g_osq"])
                        P.dve(lambda e, a=a: e.tensor_reduce(out=oss[a][:], in_=osq[:], axis=AX.X, op=ALU.add), r=["g_osq"], w=[("g_oss", a)])
                        rsqrt_ops(oss[a][:], oss[a][:], 1.0 / 64, [("g_oss", a)], ("g_oss", a))
                        P.dve(lambda e, a=a, t=t: e.tensor_tensor(out=of[a][:], in0=O[:, t, :].rearrange("p (h x) -> p h x", x=64),
                                                                  in1=oss[a][:].unsqueeze(2).to_broadcast([128, 4, 64]), op=ALU.mult),
                              r=[("O", t), ("g_oss", a)], w=[("g_of", a)])
                        P.dve(lambda e, a=a: e.tensor_tensor(out=of[a][:], in0=of[a][:], in1=gnw[:].unsqueeze(1).to_broadcast([128, 4, 64]), op=ALU.mult),
                              r=[("g_of", a), "gnw"], w=[("g_of", a)])
                        P.pool(lambda e, a=a: e.tensor_tensor(out=oo[a][:], in0=of[a][:].rearrange("p h x -> p (h x)"), in1=gt_[a][:], op=ALU.mult),
                               r=[("g_of", a), ("g_gt", a)], w=[("g_oo", a)])
                        P.dma("pool", lambda e, a=a, rows=rows: e.dma_start(out=ml[rows, 768:1024], in_=oo[a][:]), r=[("g_oo", a)], w=[("ml_g", t)])
            P.barrier()

        HALVES = [[(0, 2)] + [(2 + 4 * i, 4) for i in range(4)], [(18 + 4 * i, 4) for i in range(4)]]

        def phase_out_moe(l, stream_in, stream_out, last):
            with contextlib.ExitStack() as esA:
                G = sb(esA, "G", [128, NT, 32], F32)
                rb = bcast_load(esA, "rb", router_b[l], 36)
                def do_half(hi, groups):
                    tiles = [t0 + i for (t0, nt) in groups for i in range(nt)]
                    tb = tiles[0]
                    nth = len(tiles)
                    with contextlib.ExitStack() as esH:
                        flT = sb(esH, "flT", [128, 8, nth * 128], BF16)
                        with contextlib.ExitStack() as esB:
                            wob = sb(esB, "wob", [128, 8, D], BF16)
                            wst = [sb(esB, f"wost{i}", [128, D], F32) for i in range(2)]
                            rw = sb(esB, "rw", [128, 8, 36], F32)
                            mlt = [sb(esB, f"mlt{i}", [128, D], BF16) for i in range(2)]
                            mlT = [sb(esB, f"mlT{i}", [128, 8, 128], BF16) for i in range(2)]
                            xt = [sb(esB, f"xto{i}", [128, D], F32) for i in range(2)]
                            x1 = [sb(esB, f"x1{i}", [128, D], F32) for i in range(2)]
                            junk = sb(esB, "junko", [128, D], BF16)
                            ss = [sb(esB, f"sso{i}", [128, 1], F32) for i in range(2)]
                            xn = [sb(esB, f"xno{i}", [128, D], F32) for i in range(2)]
                            fl32 = [sb(esB, f"fl32{i}", [128, 8, 128], F32) for i in range(2)]
                            rl = sb(esB, "rl", [128, 36], F32)
                            rt = sb(esB, "rt", [128, 64], F32)
                            tmp48 = sb(esB, "tmp48", [128, 4, 8], F32)
                            for k in range(8):
                                s_ = k % 2
                                P.dma("sp", lambda e, s_=s_, k=k: e.dma_start(out=wst[s_][:], in_=w_out[l][k * 128:(k + 1) * 128, :]),
                                      w=[("wost", s_)])
                                P.pool(lambda e, s_=s_, k=k: e.tensor_copy(out=wob[:, k, :], in_=wst[s_][:]),
                                       r=[("wost", s_)], w=[("wob", k)])
                            WOB = [("wob", k) for k in range(8)]
                            P.dma("sp", lambda e: e.dma_start(out=rw[:], in_=router_w[l].rearrange("(k p) n -> p k n", p=128)), w=["rw"])
                            for t in tiles:
                                if last and t < NCTX_T:
                                    continue
                                j = 1 if t < NCTX_T else 0
                                a = t % 2
                                tl = t - tb
                                rows = slice(t * 128, (t + 1) * 128)
                                P.dma("sp", lambda e, a=a, rows=rows: e.dma_start(out=mlt[a][:], in_=ml[rows, :]),
                                      r=[("ml_a", t), ("ml_s", t), ("ml_g", t)], w=[("mlt", a)])
                                P.dma("sp", lambda e, a=a, rows=rows: e.dma_start(out=xt[a][:], in_=stream_in[rows, :]), w=[("xto", a)])
                                bk = 6 + a
                                ptv = tbanks[bk][:].rearrange("p (k n) -> p k n", n=128)
                                for k in range(8):
                                    P.pe(lambda e, a=a, k=k, ptv=ptv: e.transpose(out=ptv[:, k, :], in_=mlt[a][:, k * 128:(k + 1) * 128],
                                                                              identity=identb[:]),
                                         r=[("mlt", a), "identb"], w=[BK(bk)])
                                P.act(lambda e, a=a, ptv=ptv: e.activation(out=mlT[a][:], in_=ptv, func=AF.Copy), r=[BK(bk)], w=[("mlT", a)])
                                for c2 in range(2):
                                    for k in range(8):
                                        P.pe(lambda e, a=a, k=k, c2=c2: e.matmul(
                                            banks[c2][:], lhsT=mlT[a][:, k, :], rhs=wob[:, k, c2 * 512:(c2 + 1) * 512],
                                            start=(k == 0), stop=(k == 7)), r=[("mlT", a)] + WOB, w=[BK(c2)])
                                    cs = slice(c2 * 512, (c2 + 1) * 512)
                                    P.dve(lambda e, a=a, c2=c2, cs=cs, j=j: e.tensor_tensor(
                                        out=x1[a][:, cs], in0=banks[c2][:], in1=g1row[:, j, c2 * 4:(c2 + 1) * 4, :].rearrange("p k n -> p (k n)"),
                                        op=ALU.mult), r=[BK(c2), "g1row"], w=[("x1", a, c2)])
                                    P.pool(lambda e, a=a, cs=cs: e.tensor_tensor(out=x1[a][:, cs], in0=x1[a][:, cs], in1=xt[a][:, cs], op=ALU.add),
                                           r=[("x1", a, c2), ("xto", a)], w=[("x1", a, c2)])
                                X1K = [("x1", a, 0), ("x1", a, 1)]
                                P.dma("pool", lambda e, a=a, rows=rows: e.dma_start(out=xs1[rows, :], in_=x1[a][:]), r=X1K, w=[("xs1", t)])
                                P.act(lambda e, a=a: e.activation(out=junk[:], in_=x1[a][:], func=AF.Square, accum_out=ss[a][:]),
                                      r=X1K, w=["junko", ("sso", a)])
                                rsqrt_ops(ss[a][:], ss[a][:], 1.0 / D, [("sso", a)], ("sso", a))
                                P.dve(lambda e, a=a: e.tensor_scalar(out=xn[a][:], in0=x1[a][:], scalar1=ss[a][:, 0:1], scalar2=None,
                                                                     op0=ALU.mult), r=X1K + [("sso", a)], w=[("xno", a)])
                                for hf in range(2):
                                    bkf = 2 + hf
                                    pf = banks[bkf][:].rearrange("p (k n) -> p k n", n=128)
                                    for kk in range(4):
                                        k = hf * 4 + kk
                                        P.pe(lambda e, a=a, k=k, kk=kk, pf=pf: e.transpose(out=pf[:, kk, :], in_=xn[a][:, k * 128:(k + 1) * 128],
                                                                                       identity=identf[:]),
                                             r=[("xno", a), "identf"], w=[BK(bkf)])
                                    for kk in range(4):
                                        k = hf * 4 + kk
                                        P.act(lambda e, a=a, k=k, kk=kk, pf=pf, j=j: e.activation(
                                            out=fl32[a][:, k, :], in_=pf[:, kk, :], func=AF.Identity,
                                            scale=s2[:, k, j:j + 1], bias=modT[:, 24 + k, j:j + 1]),
                                            r=[BK(bkf), "s2k", "modT"], w=[("fl32", a, k)])
                                FLK = [("fl32", a, k) for k in range(8)]
                                P.pool(lambda e, a=a, tl=tl: e.tensor_copy(out=flT[:, :, tl * 128:(tl + 1) * 128], in_=fl32[a][:]),
                                       r=FLK, w=[("flT", tl)])
                                for k in range(8):
                                    P.pe(lambda e, a=a, k=k: e.matmul(banks[4][:, 0:36], lhsT=fl32[a][:, k, :], rhs=rw[:, k, :],
                                                                      start=(k == 0), stop=(k == 7)), r=FLK + ["rw"], w=[BK(4)])
                                RT = "rt"
                                P.dve(lambda e: e.tensor_tensor(out=rl[:], in0=banks[4][:, 0:36], in1=rb[:], op=ALU.add), r=[BK(4), "rb"], w=["rl"])
                                gmax, ngmax, gsum, m1, m2, dd, e2, g1_, g2_ = [rt[:, i:i + 1] for i in range(9)]
                                ohg, ge = rt[:, 12:16], rt[:, 16:20]
                                sel, oh1, sel2, oh2, gate8 = [rt[:, 24 + 8 * i:32 + 8 * i] for i in range(5)]
                                P.dve(lambda e: e.tensor_reduce(out=gmax, in_=rl[:, 0:4], axis=AX.X, op=ALU.max), r=["rl"], w=[RT])
                                P.dve(lambda e: e.tensor_scalar(out=ohg, in0=rl[:, 0:4], scalar1=gmax, scalar2=None, op0=ALU.is_equal), r=["rl", RT], w=[RT])
                                P.dve(lambda e: e.tensor_scalar(out=ngmax, in0=gmax, scalar1=-1.0, scalar2=None, op0=ALU.mult), r=[RT], w=[RT])
                                P.act(lambda e: e.activation(out=ge, in_=rl[:, 0:4], func=AF.Exp, bias=ngmax, scale=1.0, accum_out=gsum), r=["rl", RT], w=[RT])
                                P.dve(lambda e: e.reciprocal(out=gsum, in_=gsum), r=[RT], w=[RT])
                                P.dve(lambda e: e.tensor_tensor(out=tmp48[:], in0=rl[:, 4:36].rearrange("p (g x) -> p g x", x=8),
                                                                in1=ohg.unsqueeze(2).to_broadcast([128, 4, 8]), op=ALU.mult), r=["rl", RT], w=["tmp48"])
                                P.dve(lambda e: e.tensor_reduce(out=sel, in_=tmp48[:].rearrange("p g x -> p x g"), axis=AX.X, op=ALU.add), r=["tmp48"], w=[RT])
                                P.dve(lambda e: e.tensor_reduce(out=m1, in_=sel, axis=AX.X, op=ALU.max), r=[RT], w=[RT])
                                P.dve(lambda e: e.tensor_scalar(out=oh1, in0=sel, scalar1=m1, scalar2=None, op0=ALU.is_equal), r=[RT], w=[RT])
                                P.dve(lambda e: e.scalar_tensor_tensor(out=sel2, in0=oh1, scalar=-1e30, in1=sel, op0=ALU.mult, op1=ALU.add), r=[RT], w=[RT])
                                P.dve(lambda e: e.tensor_reduce(out=m2, in_=sel2, axis=AX.X, op=ALU.max), r=[RT], w=[RT])
                                P.dve(lambda e: e.tensor_scalar(out=oh2, in0=sel2, scalar1=m2, scalar2=None, op0=ALU.is_equal), r=[RT], w=[RT])
                                P.dve(lambda e: e.tensor_tensor(out=dd, in0=m2, in1=m1, op=ALU.subtract), r=[RT], w=[RT])
                                P.act(lambda e: e.activation(out=e2, in_=dd, func=AF.Exp), r=[RT], w=[RT])
                                P.dve(lambda e: e.tensor_scalar(out=dd, in0=e2, scalar1=1.0, scalar2=None, op0=ALU.add), r=[RT], w=[RT])
                                P.dve(lambda e: e.reciprocal(out=dd, in_=dd), r=[RT], w=[RT])
                                P.dve(lambda e: e.tensor_tensor(out=g1_, in0=dd, in1=gsum, op=ALU.mult), r=[RT], w=[RT])
                                P.dve(lambda e: e.tensor_tensor(out=g2_, in0=g1_, in1=e2, op=ALU.mult), r=[RT], w=[RT])
                                P.dve(lambda e: e.tensor_scalar(out=gate8, in0=oh1, scalar1=g1_, scalar2=None, op0=ALU.mult), r=[RT], w=[RT])
                                P.dve(lambda e: e.scalar_tensor_tensor(out=gate8, in0=oh2, scalar=g2_, in1=gate8, op0=ALU.mult, op1=ALU.add), r=[RT], w=[RT])
                                P.dve(lambda e, t=t: e.tensor_tensor(out=G[:, t, :].rearrange("p (g x) -> p g x", x=8),
                                                                     in0=ohg.unsqueeze(2).to_broadcast([128, 4, 8]),
                                                                     in1=gate8.unsqueeze(1).to_broadcast([128, 4, 8]), op=ALU.mult),
                                      r=[RT], w=[("G", t)])
                        P.barrier()
                        yacc = sb(esH, "yacc", [128, nth, D], BF16)
                        with contextlib.ExitStack() as esC:
                            est = [sb(esC, f"est{i}", [128, 2048], F32) for i in range(3)]
                            wgb = [sb(esC, f"wgb{i}", [128, 8, 512], BF16) for i in range(2)]
                            wub = [sb(esC, f"wub{i}", [128, 8, 512], BF16) for i in range(2)]
                            wdb = [sb(esC, f"wdb{i}", [128, 4, D], BF16) for i in range(2)]
                            sg = [sb(esC, f"sg{i}", [128, 512], F32) for i in range(2)]
                            hh = [sb(esC, f"hh{i}", [128, 4, 512], BF16) for i in range(2)]
                            FLALL = [("flT", i) for i in range(nth)]
                            nst = 0
                            gi = 0
                            for ex in range(32):
                                ws = ex % 2
                                for (dst, src, key) in ((wgb, exp_w_gate, "wgb"), (wub, exp_w_up, "wub")):
                                    for pc in range(2):
                                        s_ = nst % 3
                                        nst += 1
                                        P.dma("sp", lambda e, s_=s_, src=src, pc=pc, ex=ex: e.dma_start(
                                            out=est[s_][:].rearrange("p (k n) -> p k n", n=512),
                                            in_=src[l, ex, pc * 512:(pc + 1) * 512, :].rearrange("(k p) n -> p k n", p=128)), w=[("est", s_)])
                                        P.pool(lambda e, s_=s_, dst=dst, pc=pc, ws=ws: e.tensor_copy(
                                            out=dst[ws][:, pc * 4:(pc + 1) * 4, :], in_=est[s_][:].rearrange("p (k n) -> p k n", n=512)),
                                            r=[("est", s_)], w=[(key, ws, pc)])
                                for pc in range(2):
                                    s_ = nst % 3
                                    nst += 1
                                    P.dma("sp", lambda e, s_=s_, pc=pc, ex=ex: e.dma_start(
                                        out=est[s_][:].rearrange("p (k n) -> p k n", n=D),
                                        in_=exp_w_down[l, ex, pc * 256:(pc + 1) * 256, :].rearrange("(k p) n -> p k n", p=128)), w=[("est", s_)])
                                    P.pool(lambda e, s_=s_, pc=pc, ws=ws: e.tensor_copy(
                                        out=wdb[ws][:, pc * 2:(pc + 1) * 2, :], in_=est[s_][:].rearrange("p (k n) -> p k n", n=D)),
                                        r=[("est", s_)], w=[("wdb", ws, pc)])
                                WG = [("wgb", ws, 0), ("wgb", ws, 1)]
                                WU = [("wub", ws, 0), ("wub", ws, 1)]
                                WD = [("wdb", ws, 0), ("wdb", ws, 1)]
                                for (t0, nt) in groups:
                                    if last and t0 < NCTX_T:
                                        continue
                                    N = nt * 128
                                    o0 = (t0 - tb) * 128
                                    hs = gi % 2
                                    gi += 1
                                    for jx in range(4):
                                        ba = (jx % 2) * 2
                                        for (bk_, wsrc, wk) in ((ba, wgb, WG), (ba + 1, wub, WU)):
                                            for k in range(8):
                                                P.pe(lambda e, bk_=bk_, wsrc=wsrc, k=k, jx=jx, o0=o0, N=N, ws=ws: e.matmul(
                                                    banks[bk_][:, 0:N], lhsT=wsrc[ws][:, k, jx * 128:(jx + 1) * 128], rhs=flT[:, k, o0:o0 + N],
                                                    start=(k == 0), stop=(k == 7)), r=wk + FLALL, w=[BK(bk_)])
                                        sgs = jx % 2
                                        P.act(lambda e, ba=ba, sgs=sgs, N=N: e.activation(out=sg[sgs][:, 0:N], in_=banks[ba][:, 0:N], func=AF.Silu),
                                              r=[BK(ba)], w=[("sg", sgs)])
                                        P.dve(lambda e, ba=ba, sgs=sgs, N=N, hs=hs, jx=jx: e.tensor_tensor(
                                            out=hh[hs][:, jx, 0:N], in0=sg[sgs][:, 0:N], in1=banks[ba + 1][:, 0:N], op=ALU.mult),
                                            r=[("sg", sgs), BK(ba + 1)], w=[("hh", hs, jx)])
                                    HK = [("hh", hs, jx) for jx in range(4)]
                                    for ti in range(nt):
                                        t = t0 + ti
                                        tl = t - tb
                                        for c2 in range(2):
                                            bk_ = 4 + c2
                                            for jx in range(4):
                                                P.pe(lambda e, bk_=bk_, jx=jx, hs=hs, ti=ti, c2=c2, ws=ws: e.matmul(
                                                    banks[bk_][:], lhsT=hh[hs][:, jx, ti * 128:(ti + 1) * 128], rhs=wdb[ws][:, jx, c2 * 512:(c2 + 1) * 512],
                                                    start=(jx == 0), stop=(jx == 3)), r=HK + WD, w=[BK(bk_)])
                                            ya = yacc[:, tl, c2 * 512:(c2 + 1) * 512]
                                            if ex == 0:
                                                P.dve(lambda e, bk_=bk_, ya=ya, t=t, ex=ex: e.tensor_scalar(
                                                    out=ya, in0=banks[bk_][:], scalar1=G[:, t, ex:ex + 1], scalar2=None, op0=ALU.mult),
                                                    r=[BK(bk_), ("G", t)], w=[("yacc", tl, c2)])
                                            else:
                                                P.dve(lambda e, bk_=bk_, ya=ya, t=t, ex=ex: e.scalar_tensor_tensor(
                                                    out=ya, in0=banks[bk_][:], scalar=G[:, t, ex:ex + 1], in1=ya, op0=ALU.mult, op1=ALU.add),
                                                    r=[BK(bk_), ("G", t), ("yacc", tl, c2)], w=[("yacc", tl, c2)])
                        P.barrier()
                        with contextlib.ExitStack() as esC:
                            xr = [sb(esC, f"xr{i}", [128, D], F32) for i in range(2)]
                            xo = [sb(esC, f"xo{i}", [128, D], F32) for i in range(2)]
                            for t in tiles:
                                if last and t < NCTX_T:
                                    continue
                                j = 1 if t < NCTX_T else 0
                                a = t % 2
                                tl = t - tb
                                rows = slice(t * 128, (t + 1) * 128)
                                P.dma("sp", lambda e, a=a, rows=rows: e.dma_start(out=xr[a][:], in_=xs1[rows, :]), r=[("xs1", t)], w=[("xr", a)])
                                P.dve(lambda e, a=a, tl=tl, j=j: e.tensor_tensor(
                                    out=xo[a][:], in0=yacc[:, tl, :], in1=g2row[:, j, :, :].rearrange("p k n -> p (k n)"), op=ALU.mult),
                                    r=[("yacc", tl, 0), ("yacc", tl, 1), "g2row"], w=[("xo", a)])
                                P.pool(lambda e, a=a: e.tensor_tensor(out=xo[a][:], in0=xo[a][:], in1=xr[a][:], op=ALU.add),
                                       r=[("xo", a), ("xr", a)], w=[("xo", a)])
                                if last:
                                    lt = t - NCTX_T
                                    P.dma("pool", lambda e, a=a, lt=lt: e.dma_start(out=y_out[lt * 128:(lt + 1) * 128, :], in_=xo[a][:]),
                                          r=[("xo", a)], w=[("yout", t)])
                                else:
                                    P.dma("pool", lambda e, a=a, rows=rows: e.dma_start(out=stream_out[rows, :], in_=xo[a][:]),
                                          r=[("xo", a)], w=[("sout", t)])
                        P.barrier()

                for hi_, groups_ in enumerate(HALVES):
                    do_half(hi_, groups_)

        cur_in = xin
        if zero_ml:
            with contextlib.ExitStack() as esZ:
                zt = sb(esZ, "zt", [128, 256], BF16)
                P.pool(lambda e: e.memset(zt[:], 0.0), w=["zt"])
                for t in range(NT):
                    P.dma("pool", lambda e, t=t: e.dma_start(out=ml[t * 128:(t + 1) * 128, 768:1024], in_=zt[:]), r=["zt"],
                          w=[("ml_g", t)])
            P.barrier()
        for l in range(NL):
            with contextlib.ExitStack() as esM:
                phase_mod(l, esM)
            P.barrier()
            if stop_after == "mod":
                break
            phase_proj_attn(l, cur_in)
            if stop_after in ("proj", "attn", "p1a", "p1b", "p1c", "p1d", "p1e", "p1f", "p1g"):
                break
            phase_ssm(l)
            if stop_after in ("conv", "ssm", "ssd1", "ssd2", "ssd3"):
                break
            phase_gdn(l)
            if stop_after in ("gconv", "gdn"):
                break
            phase_out_moe(l, cur_in, xs2, last=(l == NL - 1))
            cur_in = xs2

        P.emit()
    return nc


def _rope_tables():
    rows = 4096 // 64
    row = np.repeat(np.arange(rows, dtype=np.float32), 64)
    col = np.tile(np.arange(64, dtype=np.float32), rows)
    half = 16
    inv = (10000.0 ** (-np.arange(0, half, 2, dtype=np.float32) / half)).astype(np.float32)
    ang_r = row[:, None] * inv
    ang_c = col[:, None] * inv
    ang = np.concatenate([ang_r, ang_r, ang_c, ang_c], axis=-1).astype(np.float32)
    cos, sin = np.cos(ang), np.sin(ang)
    sgn = np.concatenate([-np.ones(8), np.ones(8), -np.ones(8), np.ones(8)]).astype(np.float32)
    tb = np.stack([cos, sin * sgn]).astype(np.float32)
    return np.ascontiguousarray(tb.reshape(2, 32, 128, 32).transpose(0, 2, 1, 3))


def _chunkT(v, n):
    return np.ascontiguousarray(np.swapaxes(v.reshape(v.shape[:-1] + (n, 128)), -1, -2))


def prep_inputs(inp, cores=range(8)):
    f = lambda a: np.ascontiguousarray(np.asarray(a, dtype=np.float32))
    shared = {
        "rope": _rope_tables(),
        "w_mod": f(inp["w_mod"]), "b_modT": _chunkT(f(inp["b_mod"]), 48),
        "n1T": _chunkT(f(inp["norm1_w"]), 8), "n2T": _chunkT(f(inp["norm2_w"]), 8),
        "w_in": f(inp["w_in"]), "w_out": f(inp["w_out"]),
        "ssm_conv_wT": np.ascontiguousarray(f(inp["ssm_conv_w"]).reshape(L_DEPTH, 5, 6, 128).transpose(0, 3, 2, 1)),
        "ssm_conv_bT": _chunkT(f(inp["ssm_conv_b"]), 6),
        "gdn_conv_wT": np.ascontiguousarray(f(inp["gdn_conv_w"]).reshape(L_DEPTH, 5, 6, 128).transpose(0, 3, 2, 1)),
        "router_w": np.ascontiguousarray(np.concatenate([f(inp["router_g_w"]), f(inp["router_e_w"])], axis=-1)),
        "router_b": np.ascontiguousarray(np.concatenate([f(inp["router_g_b"]), f(inp["router_e_b"])], axis=-1)),
    }
    for k in ("diff_qn_w", "diff_kn_w", "diff_lq1", "diff_lk1", "diff_lq2", "diff_lk2", "diff_norm_w",
              "ssm_dt_bias", "ssm_a_log", "ssm_d", "ssm_norm_w", "gdn_dt_bias", "gdn_a_log", "gdn_norm_w",
              "exp_w_gate", "exp_w_up", "exp_w_down"):
        shared[k] = f(inp[k])
    x, c, ctx, c_ctx = f(inp["x"]), f(inp["c"]), f(inp["ctx"]), f(inp["c_ctx"])
    maps = []
    for b in cores:
        m = dict(shared)
        m["xin"] = np.ascontiguousarray(np.concatenate([ctx[b], x[b]], axis=0))
        cc = np.stack([c[b], c_ctx], axis=-1)
        m["ccT"] = np.ascontiguousarray(cc.reshape(8, 128, 2).transpose(1, 0, 2))
        maps.append(m)
    return maps


_NC_CACHE = {}


def kernel(**inputs):
    if "nc" not in _NC_CACHE:
        _NC_CACHE["nc"] = build()
    nc = _NC_CACHE["nc"]
    maps = prep_inputs(inputs)
    res = run_bass_kernel_spmd(nc, maps, core_ids=list(range(8)))
    return np.stack([np.asarray(r["y"], dtype=np.float32) for r in res.results], axis=0)
```
